# Optimizing a Trainium2 kernel written in Bass

```python
import jax
import jax.numpy as jnp
from jax import lax
import numpy as np

D_MODEL = 1024
BATCH = 4
SEQ = 4096
DEPTH = 4

N_MIXERS = 3
DN_ALPHA = (2.0 * DEPTH) ** 0.25
DN_BETA = (8.0 * DEPTH) ** -0.25
LN_EPS = 1e-5
RMS_EPS = 1e-6

MLA_HEADS = 8
MLA_Q_RANK = 384
MLA_KV_RANK = 256
MLA_NOPE = 128
MLA_ROPE = 64
MLA_V = 128
ROPE_THETA = 10000.0
Q_BLOCK = 128
MLA_IN = MLA_Q_RANK + MLA_KV_RANK + MLA_ROPE

POOL_WINDOWS = (2, 4, 8, 16)
POOL_GROUPS = len(POOL_WINDOWS)
POOL_GROUP_DIM = D_MODEL // POOL_GROUPS

MLSTM_HEADS = 4
MLSTM_QK = D_MODEL // (2 * MLSTM_HEADS)
MLSTM_V = D_MODEL // MLSTM_HEADS
MLSTM_CONV = 5
MLSTM_CHUNK = 64
MLSTM_IN = 2 * MLSTM_HEADS * MLSTM_QK + 2 * MLSTM_HEADS * MLSTM_V + 4 * MLSTM_HEADS

D_FF = 2816
N_EXPERTS = 8
TOP_K = 2
D_FF_EXPERT = 3584

N_MLA = (DEPTH + 2) // 3
N_POOL = (DEPTH + 1) // 3
N_MLSTM = DEPTH // 3
N_DENSE = (DEPTH + 1) // 2
N_MOE = DEPTH // 2

kernel_name = "hybrid_mla_pool_mlstm_moe_encoder"


def _layernorm(x, g, b):
    xf = x.astype(jnp.float32)
    mu = jnp.mean(xf, axis=-1, keepdims=True)
    var = jnp.mean(jnp.square(xf - mu), axis=-1, keepdims=True)
    y = (xf - mu) * lax.rsqrt(var + LN_EPS)
    return (y * g + b).astype(x.dtype)


def _rmsnorm(x, g):
    xf = x.astype(jnp.float32)
    y = xf * lax.rsqrt(jnp.mean(jnp.square(xf), axis=-1, keepdims=True) + RMS_EPS)
    return (y * g).astype(x.dtype)


def _rope(x, cos, sin):
    half = x.shape[-1] // 2
    xf = x.astype(jnp.float32)
    x1, x2 = xf[..., :half], xf[..., half:]
    out = jnp.concatenate([x1 * cos - x2 * sin, x2 * cos + x1 * sin], axis=-1)
    return out.astype(x.dtype)


def _mla(x, positions, w_dqkv, q_norm, w_uq, kv_norm, w_ukv, w_o):
    B, S, _ = x.shape
    H = MLA_HEADS
    lat = x @ w_dqkv
    cq = lat[..., :MLA_Q_RANK]
    ckv = lat[..., MLA_Q_RANK:MLA_Q_RANK + MLA_KV_RANK]
    kr = lat[..., MLA_Q_RANK + MLA_KV_RANK:]
    q = (_rmsnorm(cq, q_norm) @ w_uq).reshape(B, S, H, MLA_NOPE + MLA_ROPE)
    qn, qr = q[..., :MLA_NOPE], q[..., MLA_NOPE:]
    kv = (_rmsnorm(ckv, kv_norm) @ w_ukv).reshape(B, S, H, MLA_NOPE + MLA_V)
    kn, v = kv[..., :MLA_NOPE], kv[..., MLA_NOPE:]
    inv_freq = ROPE_THETA ** (-jnp.arange(0, MLA_ROPE, 2, dtype=jnp.float32) / MLA_ROPE)
    ang = positions.astype(jnp.float32)[..., None] * inv_freq
    cos, sin = jnp.cos(ang), jnp.sin(ang)
    qr = _rope(qr, cos[:, :, None, :], sin[:, :, None, :])
    kr = _rope(kr, cos, sin)
    scale = (MLA_NOPE + MLA_ROPE) ** -0.5
    nb = S // Q_BLOCK
    qn_b = qn.reshape(B, nb, Q_BLOCK, H, MLA_NOPE).transpose(1, 0, 2, 3, 4)
    qr_b = qr.reshape(B, nb, Q_BLOCK, H, MLA_ROPE).transpose(1, 0, 2, 3, 4)

    def attend(blk):
        qn_i, qr_i = blk
        s = (jnp.einsum('bqhd,bkhd->bhqk', qn_i, kn)
             + jnp.einsum('bqhr,bkr->bhqk', qr_i, kr))
        p = jax.nn.softmax(s.astype(jnp.float32) * scale, axis=-1).astype(v.dtype)
        return jnp.einsum('bhqk,bkhd->bqhd', p, v)

    o = lax.map(attend, (qn_b, qr_b))
    o = o.transpose(1, 0, 2, 3, 4).reshape(B, S, H * MLA_V)
    return o @ w_o


def _pool_mixer(x, w_grp, scale):
    B, S, D = x.shape
    xf = x.astype(jnp.float32)
    cs = jnp.concatenate([jnp.zeros((B, 1, D), jnp.float32), jnp.cumsum(xf, axis=1)], axis=1)
    t = jnp.arange(S)
    outs = []
    for g, w in enumerate(POOL_WINDOWS):
        lo = jnp.clip(t - w // 2, 0, S)
        hi = jnp.clip(t + w - w // 2, 0, S)
        sl = slice(g * POOL_GROUP_DIM, (g + 1) * POOL_GROUP_DIM)
        csg = cs[:, :, sl]
        mean = (csg[:, hi] - csg[:, lo]) / (hi - lo).astype(jnp.float32)[None, :, None]
        outs.append(mean - xf[:, :, sl])
    z = jnp.stack(outs, axis=2).astype(x.dtype)
    y = jnp.einsum('bsgc,gcd->bsgd', z, w_grp).reshape(B, S, D)
    return y * scale


def _mlstm_chunk(carry, inp):
    C, n, m = carry
    q, k, v, li, lf = inp
    L = q.shape[-2]
    causal_in_chunk = jnp.tril(jnp.ones((L, L), dtype=bool))
    b = jnp.cumsum(lf, axis=-1)
    Dm = b[..., :, None] - b[..., None, :] + li[..., None, :]
    Dm = jnp.where(causal_in_chunk, Dm, -jnp.inf)
    g = b + m[..., None]
    m_t = jnp.maximum(g, jnp.max(Dm, axis=-1))
    Dw = jnp.exp(Dm - m_t[..., None])
    gw = jnp.exp(g - m_t)
    s = jnp.einsum('...td,...sd->...ts', q, k) * Dw
    num = gw[..., None] * jnp.einsum('...td,...dv->...tv', q, C) + jnp.einsum('...ts,...sv->...tv', s, v)
    den = gw * jnp.einsum('...td,...d->...t', q, n) + jnp.sum(s, axis=-1)
    h = num / jnp.maximum(jnp.abs(den), jnp.exp(-m_t))[..., None]
    bL = b[..., -1]
    a = bL[..., None] - b + li
    m_new = jnp.maximum(bL + m, jnp.max(a, axis=-1))
    aw = jnp.exp(a - m_new[..., None])
    cw = jnp.exp(bL + m - m_new)
    C_new = cw[..., None, None] * C + jnp.einsum('...sd,...sv->...dv', k * aw[..., None], v)
    n_new = cw[..., None] * n + jnp.einsum('...s,...sd->...d', aw, k)
    return (C_new, n_new, m_new), h


def _mlstm_mixer(x, w_in, conv_w, b_gate, norm_g, w_out):
    B, S, D = x.shape
    H, DK, DV, L = MLSTM_HEADS, MLSTM_QK, MLSTM_V, MLSTM_CHUNK
    NC = S // L
    proj = x @ w_in
    n_qk = 2 * H * DK
    qk = proj[..., :n_qk]
    v = proj[..., n_qk:n_qk + H * DV]
    o = proj[..., n_qk + H * DV:n_qk + 2 * H * DV]
    gates = proj[..., n_qk + 2 * H * DV:]
    qk = jax.nn.silu(lax.conv_general_dilated(
        qk, conv_w[:, None, :], window_strides=(1,),
        padding=[(MLSTM_CONV // 2, MLSTM_CONV // 2)],
        dimension_numbers=('NWC', 'WIO', 'NWC'), feature_group_count=n_qk))
    q = qk[..., :H * DK].reshape(B, S, H, DK).astype(jnp.float32)
    k = qk[..., H * DK:].reshape(B, S, H, DK).astype(jnp.float32) * (DK ** -0.5)
    v = v.reshape(B, S, H, DV).astype(jnp.float32)
    gates = (gates + b_gate).astype(jnp.float32).reshape(B, S, 2, 2, H)
    li = gates[:, :, 0]
    lf = jax.nn.log_sigmoid(gates[:, :, 1])

    def two_dir(a):
        return jnp.stack([a, jnp.flip(a, axis=1)], axis=0)

    def gate_dir(a):
        return jnp.stack([a[:, :, 0], jnp.flip(a[:, :, 1], axis=1)], axis=0)

    def to_chunks(a):
        return a.reshape(2, B, NC, L, H, a.shape[-1]).transpose(2, 0, 1, 4, 3, 5)

    qc = to_chunks(two_dir(q))
    kc = to_chunks(two_dir(k))
    vc = to_chunks(two_dir(v))
    lic = to_chunks(gate_dir(li)[..., None])[..., 0]
    lfc = to_chunks(gate_dir(lf)[..., None])[..., 0]
    init = (jnp.zeros((2, B, H, DK, DV), jnp.float32),
            jnp.zeros((2, B, H, DK), jnp.float32),
            jnp.zeros((2, B, H), jnp.float32))
    _, hs = lax.scan(_mlstm_chunk, init, (qc, kc, vc, lic, lfc))
    hs = hs.transpose(1, 2, 0, 4, 3, 5).reshape(2, B, S, H, DV)
    h = hs[0] + jnp.flip(hs[1], axis=1)
    mu = jnp.mean(h, axis=-1, keepdims=True)
    var = jnp.mean(jnp.square(h - mu), axis=-1, keepdims=True)
    hn = ((h - mu) * lax.rsqrt(var + LN_EPS)).reshape(B, S, D) * norm_g
    y = (hn * jax.nn.sigmoid(o.astype(jnp.float32))).astype(x.dtype)
    return y @ w_out


def _swiglu(x, w_gate, w_up, w_down):
    return (jax.nn.silu(x @ w_gate) * (x @ w_up)) @ w_down


def _moe(x, w_router, w_gate, w_up, w_down):
    B, S, D = x.shape
    xt = x.reshape(B * S, D)
    logits = (xt @ w_router).astype(jnp.float32)
    top_v, top_i = lax.top_k(logits, TOP_K)
    top_w = jax.nn.softmax(top_v, axis=-1)
    combine = jnp.sum(jax.nn.one_hot(top_i, N_EXPERTS, dtype=jnp.float32) * top_w[..., None], axis=1)
    y = jnp.zeros_like(xt)
    for e in range(N_EXPERTS):
        y = y + combine[:, e:e + 1].astype(x.dtype) * _swiglu(xt, w_gate[e], w_up[e], w_down[e])
    return y.reshape(B, S, D)


def setup_inputs(seed: int = 0) -> dict:
    key = jax.random.key(seed)
    ks = iter(jax.random.split(key, 32))
    f32 = jnp.float32

    def nrm(shape, fan_in, scale=1.0):
        return jax.random.normal(next(ks), shape, f32) * (scale * fan_in ** -0.5)

    def gain(shape):
        return 1.0 + 0.02 * jax.random.normal(next(ks), shape, f32)

    x = jax.random.normal(next(ks), (BATCH, SEQ, D_MODEL), f32)
    offset = jax.random.randint(next(ks), (BATCH, 1), 0, 1024, dtype=jnp.int32)
    positions = jnp.arange(SEQ, dtype=jnp.int32)[None, :] + offset

    H = MLA_HEADS
    mla_w_dqkv = nrm((N_MLA, D_MODEL, MLA_IN), D_MODEL)
    mla_q_norm = gain((N_MLA, MLA_Q_RANK))
    mla_w_uq = nrm((N_MLA, MLA_Q_RANK, H * (MLA_NOPE + MLA_ROPE)), MLA_Q_RANK)
    mla_kv_norm = gain((N_MLA, MLA_KV_RANK))
    mla_w_ukv = nrm((N_MLA, MLA_KV_RANK, H * (MLA_NOPE + MLA_V)), MLA_KV_RANK)
    mla_w_o = nrm((N_MLA, H * MLA_V, D_MODEL), H * MLA_V, DN_BETA)

    pool_w = nrm((N_POOL, POOL_GROUPS, POOL_GROUP_DIM, POOL_GROUP_DIM), POOL_GROUP_DIM, DN_BETA)
    pool_scale = gain((N_POOL, D_MODEL))

    HM = MLSTM_HEADS
    mlstm_w_in = nrm((N_MLSTM, D_MODEL, MLSTM_IN), D_MODEL)
    mlstm_conv = nrm((N_MLSTM, MLSTM_CONV, 2 * HM * MLSTM_QK), MLSTM_CONV)
    b_i = 0.1 * jax.random.normal(next(ks), (N_MLSTM, 2 * HM), f32)
    b_f = 3.0 + 3.0 * jax.random.uniform(next(ks), (N_MLSTM, 2 * HM), f32)
    mlstm_b_gate = jnp.concatenate([b_i, b_f], axis=-1)
    mlstm_norm = gain((N_MLSTM, D_MODEL))
    mlstm_w_out = nrm((N_MLSTM, HM * MLSTM_V, D_MODEL), HM * MLSTM_V, DN_BETA)

    ffn_w_gate = nrm((N_DENSE, D_MODEL, D_FF), D_MODEL)
    ffn_w_up = nrm((N_DENSE, D_MODEL, D_FF), D_MODEL)
    ffn_w_down = nrm((N_DENSE, D_FF, D_MODEL), D_FF, DN_BETA)

    moe_router = nrm((N_MOE, D_MODEL, N_EXPERTS), D_MODEL)
    moe_w_gate = nrm((N_MOE, N_EXPERTS, D_MODEL, D_FF_EXPERT), D_MODEL)
    moe_w_up = nrm((N_MOE, N_EXPERTS, D_MODEL, D_FF_EXPERT), D_MODEL)
    moe_w_down = nrm((N_MOE, N_EXPERTS, D_FF_EXPERT, D_MODEL), D_FF_EXPERT, DN_BETA)

    ln_g = gain((DEPTH, 2, D_MODEL))
    ln_b = 0.02 * jax.random.normal(next(ks), (DEPTH, 2, D_MODEL), f32)

    return {
        "x": x, "positions": positions,
        "mla_w_dqkv": mla_w_dqkv, "mla_q_norm": mla_q_norm, "mla_w_uq": mla_w_uq,
        "mla_kv_norm": mla_kv_norm, "mla_w_ukv": mla_w_ukv, "mla_w_o": mla_w_o,
        "pool_w": pool_w, "pool_scale": pool_scale,
        "mlstm_w_in": mlstm_w_in, "mlstm_conv": mlstm_conv, "mlstm_b_gate": mlstm_b_gate,
        "mlstm_norm": mlstm_norm, "mlstm_w_out": mlstm_w_out,
        "ffn_w_gate": ffn_w_gate, "ffn_w_up": ffn_w_up, "ffn_w_down": ffn_w_down,
        "moe_router": moe_router, "moe_w_gate": moe_w_gate, "moe_w_up": moe_w_up,
        "moe_w_down": moe_w_down,
        "ln_g": ln_g, "ln_b": ln_b,
    }


def reference(x, positions, mla_w_dqkv, mla_q_norm, mla_w_uq, mla_kv_norm, mla_w_ukv, mla_w_o,
              pool_w, pool_scale, mlstm_w_in, mlstm_conv, mlstm_b_gate, mlstm_norm, mlstm_w_out,
              ffn_w_gate, ffn_w_up, ffn_w_down, moe_router, moe_w_gate, moe_w_up, moe_w_down,
              ln_g, ln_b):
    for i in range(DEPTH):
        j = i // N_MIXERS
        if i % N_MIXERS == 0:
            y = _mla(x, positions, mla_w_dqkv[j], mla_q_norm[j], mla_w_uq[j],
                     mla_kv_norm[j], mla_w_ukv[j], mla_w_o[j])
        elif i % N_MIXERS == 1:
            y = _pool_mixer(x, pool_w[j], pool_scale[j])
        else:
            y = _mlstm_mixer(x, mlstm_w_in[j], mlstm_conv[j], mlstm_b_gate[j],
                             mlstm_norm[j], mlstm_w_out[j])
        x = _layernorm(DN_ALPHA * x + y, ln_g[i, 0], ln_b[i, 0])
        c = i // 2
        if i % 2 == 0:
            y = _swiglu(x, ffn_w_gate[c], ffn_w_up[c], ffn_w_down[c])
        else:
            y = _moe(x, moe_router[c], moe_w_gate[c], moe_w_up[c], moe_w_down[c])
        x = _layernorm(DN_ALPHA * x + y, ln_g[i, 1], ln_b[i, 1])
    return x
```

```python
import math
from contextlib import ExitStack

import numpy as np
import concourse.bass as bass
import concourse.mybir as mybir
from concourse.bass_utils import run_bass_kernel_spmd

F32 = mybir.dt.float32
BF16 = mybir.dt.bfloat16
I32 = mybir.dt.int32
AF = mybir.ActivationFunctionType
ALU = mybir.AluOpType
AX = mybir.AxisListType

D = 1024
NT = 2048
SEQ = 4096
DEPTH = 4
ALPHA = (2.0 * DEPTH) ** 0.25
LN_EPS = 1e-5
RMS_EPS = 1e-6
D_FF = 2816
D_FFE = 3584
NE = 8

ENGS = ("sync", "scalar", "vector", "gpsimd", "tensor")


class Op:
    __slots__ = ("eng", "fn", "waits", "dkey", "dval", "sig", "idx", "epoch", "dinc")

    def __init__(self, eng, fn):
        self.eng = eng
        self.fn = fn
        self.waits = []
        self.dkey = None
        self.dval = 0
        self.sig = None
        self.idx = None
        self.epoch = 0
        self.dinc = 16


class Prog:
    def __init__(self, nc):
        self.nc = nc
        self.ops = {e: [] for e in ENGS}
        self.last_write = {}
        self.readers = {}
        self.dcount = {}
        self.epoch = 0
        self.waited_e = {}
        self.waited_d = {}
        self.n_epochs = 1
        self.epoch_dkeys = set()

    def op(self, eng, fn, reads=(), writes=(), dkey=None, dinc=16):
        o = Op(eng, fn)
        o.dinc = dinc
        o.epoch = self.epoch
        o.idx = len(self.ops[eng])
        psr = [k for k in reads if isinstance(k, tuple) and k[0] == "ps"]
        if psr:
            writes = list(writes) + [k for k in psr if k not in writes]
        deps = []
        for k in reads:
            w = self.last_write.get(k)
            if w is not None:
                deps.append(w)
        for k in writes:
            w = self.last_write.get(k)
            if w is not None:
                deps.append(w)
            deps.extend(self.readers.get(k, ()))
        for d in deps:
            self._add_wait(o, d)
        if dkey is not None:
            key = dkey
            self.epoch_dkeys.add(key)
            self.dcount[key] = self.dcount.get(key, 0) + dinc
            o.dkey = key
            o.dval = self.dcount[key]
            tok = ("d", key, o.dval)
        else:
            tok = ("e", eng, o.idx)
        for k in writes:
            self.last_write[k] = tok
            self.readers[k] = []
        for k in reads:
            self.readers.setdefault(k, []).append(tok)
        self.ops[eng].append(o)
        return o

    def _add_wait(self, o, tok):
        eng = o.eng
        if tok[0] == "d":
            _, key, val = tok
            pk = (eng, key)
            if self.waited_d.get(pk, 0) >= val:
                return
            self.waited_d[pk] = val
            o.waits.append(tok)
        else:
            _, peng, pidx = tok
            if peng == eng and eng in ("tensor", "sync"):
                return
            pk = (eng, peng)
            if self.waited_e.get(pk, -1) >= pidx:
                return
            self.waited_e[pk] = pidx
            o.waits.append(tok)

    def barrier(self):
        lasts = {}
        for e in ENGS:
            li = -1
            for o in reversed(self.ops[e]):
                if o.fn is not None and o.dkey is None:
                    li = o.idx
                    break
            lasts[e] = li
        dk = [(k, self.dcount[k]) for k in sorted(self.epoch_dkeys, key=str)]
        self.epoch_dkeys = set()
        for e in ENGS:
            o = Op(e, None)
            o.epoch = self.epoch
            o.idx = len(self.ops[e])
            for pe in ENGS:
                if pe != e and lasts[pe] >= 0:
                    self._add_wait(o, ("e", pe, lasts[pe]))
            for k, v in dk:
                self._add_wait(o, ("d", k, v))
            self.ops[e].append(o)
        self.last_write = {}
        self.readers = {}
        self.epoch += 1
        self.n_epochs = self.epoch + 1
        self.waited_e = {}
        self.waited_d = {}

    def emit(self):
        nc = self.nc
        self.barrier()
        need = {e: set() for e in ENGS}
        for e in ENGS:
            for o in self.ops[e]:
                for w in o.waits:
                    if w[0] == "e":
                        need[w[1]].add(w[2])
        K_ROT = 4
        sigval = {}
        self.maxsig = 0
        for e in ENGS:
            cnt = {}
            for o in self.ops[e]:
                if o.idx in need[e]:
                    r = o.epoch % K_ROT
                    c = cnt.get(r, 0) + 1
                    cnt[r] = c
                    o.sig = c
                    sigval[(e, o.idx)] = (r, c)
                    self.maxsig = max(self.maxsig, c)
        dkeys = sorted(self.dcount.keys(), key=str)
        self.n_sems = 5 * K_ROT + len(dkeys)
        with ExitStack() as st:
            for i in range(getattr(self, "pad_sems", 0)):
                st.enter_context(nc.semaphore(f"pad_{i}"))
            esem = {}
            for e in ENGS:
                for ep in range(K_ROT):
                    esem[(e, ep)] = st.enter_context(nc.semaphore(f"s_{e}_{ep}"))
            dsem = {}
            for i, k in enumerate(dkeys):
                dsem[k] = st.enter_context(nc.semaphore(f"d_{i}"))
            block = st.enter_context(nc.Block())

            def run(ename):
                def body(eng):
                    for o in self.ops[ename]:
                        for w in o.waits:
                            if w[0] == "d":
                                eng.wait_ge(dsem[w[1]], w[2])
                            else:
                                ep, c = sigval[(w[1], w[2])]
                                eng.wait_ge(esem[(w[1], ep)], c)
                        if o.fn is None:
                            continue
                        ins = o.fn(eng)
                        if o.dkey is not None:
                            ins.then_inc(dsem[o.dkey], o.dinc)
                        elif o.sig is not None:
                            ins.then_inc(esem[(ename, o.epoch % K_ROT)], 1)
                return body

            block.sync(run("sync"))
            block.scalar(run("scalar"))
            block.vector(run("vector"))
            block.gpsimd(run("gpsimd"))
            block.tensor(run("tensor"))


ARENA_W = 49000


class Ctx:
    def __init__(self, nc, st):
        self.nc = nc
        self.P = Prog(nc)
        self.st = st
        self.arena = st.enter_context(nc.sbuf_tensor("arena", [128, ARENA_W], F32))
        self.top = 0
        self.ps = st.enter_context(nc.psum_tensor("ps", [128, 8, 512], F32))
        self.x_tm = self.alloc([16, D], F32)
        self.xbT = self.alloc([8, NT], BF16)
        self.ident = self.alloc([128], F32)
        self.ones = self.alloc([128], F32)
        self.stat = self.alloc([16, 8], F32)
        self.n_in = 0

    def alloc(self, shape, dt, parts=128):
        n = 1
        for v in shape:
            n *= v
        words = n if dt in (F32, I32) else (n + 1) // 2
        off = self.top
        self.top += words
        assert self.top <= ARENA_W, f"arena overflow {self.top}"
        v = self.arena[0:parts, off:off + words]
        if dt == BF16:
            v = v.bitcast(BF16)
        elif dt == I32:
            v = v.bitcast(I32)
        if len(shape) == 2:
            v = v.rearrange("p (a b) -> p a b", a=shape[0])
        elif len(shape) == 3:
            v = v.rearrange("p (a b c) -> p a b c", a=shape[0], b=shape[1])
        return v

    def mark(self):
        return self.top

    def release(self, m):
        self.P.barrier()
        self.top = m

    def dram_in(self, name, shape, dt=F32):
        return self.nc.dram_tensor(name, list(shape), dt, kind="ExternalInput").ap()

    def dram_out(self, name, shape, dt=F32):
        return self.nc.dram_tensor(name, list(shape), dt, kind="ExternalOutput").ap()

    def op(self, *a, **k):
        return self.P.op(*a, **k)

    def init_consts(self):
        P = self.P
        ident, ones = self.ident, self.ones
        P.op("gpsimd", lambda e: e.memset(ones, 1.0), writes=["ones"])
        P.op("gpsimd", lambda e: e.memset(ident, 1.0), writes=["ident"])
        P.op("gpsimd", lambda e: e.affine_select(
            out=ident, in_=ident, pattern=[[-1, 128]], compare_op=ALU.is_equal,
            fill=0.0, base=0, channel_multiplier=1), reads=["ident"], writes=["ident"])


def load_x(c, x_own):
    xv = x_own.rearrange("(t p) d -> p t d", p=128)
    for q in range(4):
        c.op("sync", lambda e, q=q: e.dma_start(out=c.x_tm[:, 4 * q:4 * q + 4, :], in_=xv[:, 4 * q:4 * q + 4, :]),
             writes=[("x", t) for t in range(4 * q, 4 * q + 4)], dkey=f"xin{q}")


def make_xbT(c, tiles=range(16), router=None):
    ps = c.ps
    for t in tiles:
        b0 = 6
        for kc in range(8):
            c.op("tensor", lambda e, t=t, kc=kc: e.transpose(
                out=ps[:, b0 + kc // 4, (kc % 4) * 128:(kc % 4 + 1) * 128],
                in_=c.x_tm[:, t, kc * 128:(kc + 1) * 128], identity=c.ident),
                reads=[("x", t), "ident"], writes=[("ps", b0 + kc // 4)])
        src = ps[:, 6:8, :].rearrange("p b (k j) -> p (b k) j", j=128)
        c.op("scalar", lambda e, t=t, src=src: e.copy(out=c.xbT[:, :, t * 128:(t + 1) * 128], in_=src),
             reads=[("ps", 6), ("ps", 7)], writes=[("xbT", t // 4)])
        if router is not None:
            wr, logits, xtf = router
            c.op("vector", lambda e, src=src: e.tensor_copy(out=xtf, in_=src),
                 reads=[("ps", 6), ("ps", 7)], writes=["xtf"])
            for kc in range(8):
                c.op("tensor", lambda e, t=t, kc=kc: e.matmul(
                    ps[:, 5, 0:8], lhsT=xtf[:, kc, :], rhs=wr[:, kc, :], start=(kc == 0), stop=(kc == 7)),
                    reads=["xtf", "wr"], writes=[("ps", 5)])
            c.op("vector", lambda e, t=t: e.tensor_copy(out=logits[:, t, :], in_=ps[:, 5, 0:8]),
                 reads=[("ps", 5)], writes=["logits"])


def scale_x(c):
    for t in range(16):
        c.op("gpsimd", lambda e, t=t: e.tensor_scalar(
            out=c.x_tm[:, t, :], in0=c.x_tm[:, t, :], scalar1=float(ALPHA), scalar2=None, op0=ALU.mult),
            reads=[("x", t)], writes=[("x", t)])


def layernorm(c, g_ap, b_ap, router=None, out_dram=None):
    stat = c.stat
    m = c.mark()
    gb = c.alloc([2, D], F32)
    c.gb = gb
    if router is not None:
        router = (router[0], router[1], c.alloc([8, 128], F32))
    c.op("sync", lambda e: e.dma_start(out=gb[:, 0, :], in_=g_ap.partition_broadcast(128)), writes=["gb"], dkey="gb")
    c.op("sync", lambda e: e.dma_start(out=gb[:, 1, :], in_=b_ap.partition_broadcast(128)), writes=["gb"], dkey="gb")
    junk = c.alloc([D], BF16)
    xn = [c.alloc([D], F32) for _ in range(2)]
    col = lambda i: stat[:, :, i]
    for t in range(16):
        xt = c.x_tm[:, t, :]
        c.op("scalar", lambda e, xt=xt, t=t: e.activation(out=junk, in_=xt, func=AF.Identity, accum_out=stat[:, t, 0:1]),
             reads=[("x", t)], writes=[("stA", t)])
        c.op("scalar", lambda e, xt=xt, t=t: e.activation(out=junk, in_=xt, func=AF.Square, accum_out=stat[:, t, 1:2]),
             reads=[("x", t)], writes=[("stB", t)])
    V = "vector"
    c.op(V, lambda e: e.tensor_scalar(out=stat[:, :, 2:4], in0=stat[:, :, 0:2], scalar1=1.0 / D, scalar2=None, op0=ALU.mult),
         reads=[("stA", t) for t in range(16)] + [("stB", t) for t in range(16)], writes=["stat"])
    c.op(V, lambda e: e.tensor_tensor(out=col(4), in0=col(2), in1=col(2), op=ALU.mult), reads=["stat"], writes=["stat"])
    c.op(V, lambda e: e.tensor_tensor(out=col(5), in0=col(3), in1=col(4), op=ALU.subtract), reads=["stat"], writes=["stat"])
    c.op("scalar", lambda e: e.activation(out=col(6), in_=col(5), func=AF.Sqrt, bias=float(LN_EPS), scale=1.0), reads=["stat"], writes=["stat"])
    c.op(V, lambda e: e.reciprocal(out=col(6), in_=col(6)), reads=["stat"], writes=["stat"])
    c.op(V, lambda e: e.scalar_tensor_tensor(out=col(7), in0=col(2), scalar=-1.0, in1=col(6), op0=ALU.mult, op1=ALU.mult),
         reads=["stat"], writes=["stat"])
    ov = out_dram.rearrange("(t p) d -> p t d", p=128) if out_dram is not None else None
    for t in range(16):
        xt = c.x_tm[:, t, :]
        xb = xn[t % 2]
        kx = ("xn", t % 2)
        c.op("scalar", lambda e, xt=xt, t=t, xb=xb: e.activation(out=xb, in_=xt, func=AF.Identity, scale=stat[:, t, 6:7], bias=stat[:, t, 7:8]),
             reads=[("x", t), "stat"], writes=[kx])
        c.op("vector", lambda e, xb=xb: e.tensor_tensor(out=xb, in0=xb, in1=gb[:, 0, :], op=ALU.mult),
             reads=[kx, "gb"], writes=[kx])
        c.op("gpsimd", lambda e, xt=xt, xb=xb: e.tensor_tensor(out=xt, in0=xb, in1=gb[:, 1, :], op=ALU.add),
             reads=[kx, "gb"], writes=[("x", t)])
        if ov is not None:
            c.op("sync", lambda e, t=t: e.dma_start(out=ov[:, t, :], in_=c.x_tm[:, t, :]),
                 reads=[("x", t)], dkey=f"out{t % 4}")
        else:
            make_xbT(c, tiles=[t], router=router)
    c.release(m)


def ffn_phase(c, wg, wu, wd, n_exp, n_f, G, moe=None):
    ps = c.ps
    n_grp = n_f // (G * 128)
    GW = G * 128
    NSTG = 2
    m = c.mark()
    stg_g = [c.alloc([8, GW], F32) for i in range(NSTG)]
    stg_u = [c.alloc([8, GW], F32) for i in range(NSTG)]
    stg_d = [c.alloc([G, D], F32) for i in range(NSTG)]
    wgb = [c.alloc([8, GW], BF16) for i in range(2)]
    wub = [c.alloc([8, GW], BF16) for i in range(2)]
    wdb = [c.alloc([G, D], BF16) for i in range(2)]
    sg = [c.alloc([512], F32) for i in range(2)]
    hb = [c.alloc([G, 512], BF16) for i in range(2)]
    it = 0
    hcnt = 0
    for ex in range(n_exp):
        for g in range(n_grp):
            s_ = it % NSTG
            b_ = it % 2
            c.op("sync", lambda e, ex=ex, g=g, s_=s_: e.dma_start(out=stg_g[s_], in_=wg[ex, g]),
                 writes=[("stg_g", s_)], dkey=f"stg_g{s_}")
            c.op("sync", lambda e, ex=ex, g=g, s_=s_: e.dma_start(out=stg_u[s_], in_=wu[ex, g]),
                 writes=[("stg_u", s_)], dkey=f"stg_u{s_}")
            c.op("sync", lambda e, ex=ex, g=g, s_=s_: e.dma_start(out=stg_d[s_], in_=wd[ex, g]),
                 writes=[("stg_d", s_)], dkey=f"stg_d{s_}")
            c.op("gpsimd", lambda e, s_=s_, b_=b_: e.tensor_copy(out=wgb[b_], in_=stg_g[s_]),
                 reads=[("stg_g", s_)], writes=[("wgb", b_)])
            c.op("gpsimd", lambda e, s_=s_, b_=b_: e.tensor_copy(out=wub[b_], in_=stg_u[s_]),
                 reads=[("stg_u", s_)], writes=[("wub", b_)])
            c.op("gpsimd", lambda e, s_=s_, b_=b_: e.tensor_copy(out=wdb[b_], in_=stg_d[s_]),
                 reads=[("stg_d", s_)], writes=[("wdb", b_)])
            for tt in range(4):
                hs = hcnt % 2
                hcnt += 1
                for fc in range(G):
                    pg = (2 * fc) % 4
                    pu = (2 * fc + 1) % 4
                    for kc in range(8):
                        c.op("tensor", lambda e, kc=kc, fc=fc, b_=b_, tt=tt, pg=pg: e.matmul(
                            ps[:, pg, :], lhsT=wgb[b_][:, kc, fc * 128:(fc + 1) * 128],
                            rhs=c.xbT[:, kc, tt * 512:(tt + 1) * 512], start=(kc == 0), stop=(kc == 7)),
                            reads=[("wgb", b_), ("xbT", tt)], writes=[("ps", pg)])
                    for kc in range(8):
                        c.op("tensor", lambda e, kc=kc, fc=fc, b_=b_, tt=tt, pu=pu: e.matmul(
                            ps[:, pu, :], lhsT=wub[b_][:, kc, fc * 128:(fc + 1) * 128],
                            rhs=c.xbT[:, kc, tt * 512:(tt + 1) * 512], start=(kc == 0), stop=(kc == 7)),
                            reads=[("wub", b_), ("xbT", tt)], writes=[("ps", pu)])
                    si = fc % 2
                    c.op("scalar", lambda e, pg=pg, si=si: e.activation(out=sg[si], in_=ps[:, pg, :], func=AF.Silu),
                         reads=[("ps", pg)], writes=[("sg", si)])
                    c.op("vector", lambda e, pu=pu, si=si, hs=hs, fc=fc: e.tensor_tensor(
                        out=hb[hs][:, fc, :], in0=ps[:, pu, :], in1=sg[si], op=ALU.mult),
                        reads=[("ps", pu), ("sg", si)], writes=[("hb", hs)])
                for ts in range(4):
                    tile = tt * 4 + ts
                    for dh in range(2):
                        py = 4 + (ts * 2 + dh) % 2
                        for fc in range(G):
                            c.op("tensor", lambda e, fc=fc, hs=hs, ts=ts, dh=dh, b_=b_, py=py: e.matmul(
                                ps[:, py, :], lhsT=hb[hs][:, fc, ts * 128:(ts + 1) * 128],
                                rhs=wdb[b_][:, fc, dh * 512:(dh + 1) * 512], start=(fc == 0), stop=(fc == G - 1)),
                                reads=[("hb", hs), ("wdb", b_)], writes=[("ps", py)])
                        xs = c.x_tm[:, tile, dh * 512:(dh + 1) * 512]
                        if moe is None:
                            c.op("vector", lambda e, xs=xs, py=py: e.tensor_tensor(out=xs, in0=ps[:, py, :], in1=xs, op=ALU.add),
                                 reads=[("ps", py), ("x", tile)], writes=[("x", tile)])
                        else:
                            c.op("vector", lambda e, xs=xs, py=py, tile=tile, ex=ex: e.scalar_tensor_tensor(
                                out=xs, in0=ps[:, py, :], scalar=moe[:, tile, ex:ex + 1], in1=xs, op0=ALU.mult, op1=ALU.add),
                                reads=[("ps", py), ("x", tile), "comb"], writes=[("x", tile)])
            it += 1
    c.release(m)


def moe_route(c, logits, comb):
    m = c.mark()
    m1 = c.alloc([16], F32)
    m2 = c.alloc([16], F32)
    t1 = c.alloc([16, 8], F32)
    l2 = c.alloc([16, 8], F32)
    sel = c.alloc([16, 8], F32)
    w1 = c.alloc([16], F32)
    w2 = c.alloc([16], F32)
    V = "vector"
    bc = lambda a: a.unsqueeze(2).to_broadcast([128, 16, 8])
    c.op(V, lambda e: e.tensor_reduce(out=m1, in_=logits, axis=AX.X, op=ALU.max), reads=["logits"], writes=["rt_m1"])
    c.op(V, lambda e: e.tensor_tensor(out=t1, in0=logits, in1=bc(m1), op=ALU.is_equal), reads=["logits", "rt_m1"], writes=["rt_t1"])
    c.op(V, lambda e: e.scalar_tensor_tensor(out=l2, in0=t1, scalar=-1e30, in1=logits, op0=ALU.mult, op1=ALU.add),
         reads=["rt_t1", "logits"], writes=["rt_l2"])
    c.op(V, lambda e: e.tensor_reduce(out=m2, in_=l2, axis=AX.X, op=ALU.max), reads=["rt_l2"], writes=["rt_m2"])
    c.op(V, lambda e: e.tensor_tensor(out=sel, in0=l2, in1=bc(m2), op=ALU.is_equal), reads=["rt_l2", "rt_m2"], writes=["rt_sel"])
    c.op(V, lambda e: e.tensor_tensor(out=w2, in0=m2, in1=m1, op=ALU.subtract), reads=["rt_m1", "rt_m2"], writes=["rt_w2"])
    c.op("scalar", lambda e: e.activation(out=w2, in_=w2, func=AF.Sigmoid), reads=["rt_w2"], writes=["rt_w2"])
    c.op(V, lambda e: e.tensor_scalar(out=w1, in0=w2, scalar1=-1.0, scalar2=1.0, op0=ALU.mult, op1=ALU.add), reads=["rt_w2"], writes=["rt_w1"])
    c.op(V, lambda e: e.tensor_tensor(out=t1, in0=t1, in1=bc(w1), op=ALU.mult), reads=["rt_t1", "rt_w1"], writes=["rt_t1"])
    c.op(V, lambda e: e.tensor_tensor(out=sel, in0=sel, in1=bc(w2), op=ALU.mult), reads=["rt_sel", "rt_w2"], writes=["rt_sel"])
    c.op(V, lambda e: e.tensor_tensor(out=comb, in0=t1, in1=sel, op=ALU.add), reads=["rt_t1", "rt_sel"], writes=["comb"])
    c.release(m)


def lay_gu(w, G):
    K, F = w.shape
    GW = G * 128
    return np.ascontiguousarray(w.reshape(8, 128, F // GW, GW).transpose(2, 1, 0, 3))


def lay_d(w, G):
    F, Dm = w.shape
    return np.ascontiguousarray(w.reshape(F // (G * 128), G, 128, Dm).transpose(0, 2, 1, 3))


POOL_W = (2, 4, 8, 16)


def pool_phase(c, halo_fill, cinfo, pw_d, pscale_d):
    ps = c.ps
    m = c.mark()
    hprev = c.alloc([D], F32)
    hnext = c.alloc([D], F32)
    Bp = c.alloc([4, 128], F32)
    Bm = c.alloc([4, 128], F32)
    Bn = c.alloc([4, 128], F32)
    pos = c.alloc([NT], F32)
    posi = c.alloc([NT], I32)
    cntt = [c.alloc([128], F32) for _ in range(2)]
    rct = [c.alloc([128], F32) for _ in range(2)]
    tmpt = [c.alloc([128], F32) for _ in range(2)]
    wst = c.alloc([4, 2, 256], F32)
    wpb = c.alloc([4, 2, 256], BF16)
    scb = c.alloc([D], F32)
    tB = [c.alloc([128], F32) for _ in range(2)]
    zb = [c.alloc([2, 128], BF16) for _ in range(2)]
    ybuf = [c.alloc([D], F32) for _ in range(3)]
    G_ = "gpsimd"
    V = "vector"
    halo_fill(hprev, hnext)
    c.op("sync", lambda e: e.dma_start(out=wst, in_=pw_d.rearrange("g p c j -> p g c j")), writes=["wst"], dkey="wst")
    c.op("sync", lambda e: e.dma_start(out=scb, in_=pscale_d.partition_broadcast(128)), writes=["scb"], dkey="scb")
    c.op(V, lambda e: e.tensor_copy(out=wpb, in_=wst), reads=["wst"], writes=["wpb"])
    for gi, w in enumerate(POOL_W):
        h = w // 2
        c.op(G_, lambda e, gi=gi: e.memset(Bp[:, gi, :], 1.0), writes=[("Bp", gi)])
        c.op(G_, lambda e, gi=gi, h=h: e.affine_select(out=Bp[:, gi, :], in_=Bp[:, gi, :], pattern=[[-1, 128]],
                                                       compare_op=ALU.is_ge, fill=0.0, base=-(128 - h), channel_multiplier=1),
             reads=[("Bp", gi)], writes=[("Bp", gi)])
        c.op(G_, lambda e, gi=gi: e.memset(Bn[:, gi, :], 1.0), writes=[("Bn", gi)])
        c.op(G_, lambda e, gi=gi, h=h: e.affine_select(out=Bn[:, gi, :], in_=Bn[:, gi, :], pattern=[[1, 128]],
                                                       compare_op=ALU.is_ge, fill=0.0, base=-(129 - h), channel_multiplier=-1),
             reads=[("Bn", gi)], writes=[("Bn", gi)])
        c.op(G_, lambda e, gi=gi: e.memset(Bm[:, gi, :], 1.0), writes=[("Bm", gi)])
        c.op(G_, lambda e, gi=gi, h=h: e.affine_select(out=Bm[:, gi, :], in_=Bm[:, gi, :], pattern=[[-1, 128]],
                                                       compare_op=ALU.is_ge, fill=0.0, base=h, channel_multiplier=1),
             reads=[("Bm", gi)], writes=[("Bm", gi)])
        c.op(G_, lambda e, gi=gi, h=h: e.affine_select(out=Bm[:, gi, :], in_=Bm[:, gi, :], pattern=[[1, 128]],
                                                       compare_op=ALU.is_ge, fill=0.0, base=h - 1, channel_multiplier=-1),
             reads=[("Bm", gi)], writes=[("Bm", gi)])
    c.op(G_, lambda e: e.iota(posi, pattern=[[1, NT]], base=0, channel_multiplier=0), writes=["posi"])
    c.op(V, lambda e: e.tensor_copy(out=pos, in_=posi), reads=["posi"], writes=["pos"])
    c.op(V, lambda e: e.tensor_scalar(out=pos, in0=pos, scalar1=cinfo[:, 0:1], scalar2=None, op0=ALU.add),
         reads=["pos", "cinfo"], writes=["pos"])
    def src_tile(j):
        if j < 0:
            return hprev, "hprev"
        if j > 15:
            return hnext, "hnext"
        return c.x_tm[:, j, :], ("x", j)

    def compute(j):
        yb = ybuf[j % 3]
        ky = ("ybuf", j % 3)
        for gi in range(4):
            k = (j * 4 + gi) % 2
            tok = slice(j * 128, (j + 1) * 128)
            h = POOL_W[gi] // 2
            c.op(V, lambda e, k=k, h=h, tok=tok: e.tensor_scalar(out=cntt[k], in0=pos[:, tok], scalar1=float(h), scalar2=float(SEQ), op0=ALU.add, op1=ALU.min),
                 reads=["pos"], writes=[("cntt", k)])
            c.op(V, lambda e, k=k, h=h, tok=tok: e.tensor_scalar(out=tmpt[k], in0=pos[:, tok], scalar1=float(-h), scalar2=0.0, op0=ALU.add, op1=ALU.max),
                 reads=["pos"], writes=[("tmpt", k)])
            c.op(V, lambda e, k=k: e.tensor_tensor(out=cntt[k], in0=cntt[k], in1=tmpt[k], op=ALU.subtract),
                 reads=[("tmpt", k), ("cntt", k)], writes=[("cntt", k)])
            c.op(V, lambda e, k=k: e.reciprocal(out=rct[k], in_=cntt[k]), reads=[("cntt", k)], writes=[("rct", k)])
            c.op(V, lambda e, k=k: e.scalar_tensor_tensor(out=tB[k], in0=cntt[k], scalar=-1.0, in1=c.ident, op0=ALU.mult, op1=ALU.mult),
                 reads=[("cntt", k), "ident"], writes=[("tB", k)])
            c.op(G_, lambda e, k=k, gi=gi: e.tensor_tensor(out=tB[k], in0=tB[k], in1=Bm[:, gi, :], op=ALU.add),
                 reads=[("tB", k), ("Bm", gi)], writes=[("tB", k)])
            pb = k
            for cc in range(2):
                fs = slice(gi * 256 + cc * 128, gi * 256 + (cc + 1) * 128)
                srcs = [(src_tile(j - 1), Bp[:, gi, :], ("Bp", gi)), (src_tile(j), tB[k], ("tB", k)), (src_tile(j + 1), Bn[:, gi, :], ("Bn", gi))]
                for si, ((sap, skey), bap, bkey) in enumerate(srcs):
                    c.op("tensor", lambda e, sap=sap, fs=fs, bap=bap, pb=pb, cc=cc, si=si: e.matmul(
                        ps[:, pb, cc * 128:(cc + 1) * 128], lhsT=sap[:, fs], rhs=bap, start=(si == 0), stop=(si == 2)),
                        reads=[skey, bkey], writes=[("ps", pb)])
            c.op(V, lambda e, k=k, pb=pb, gi=gi, tok=tok: e.tensor_tensor(
                out=zb[k], in0=ps[:, pb, 0:256].rearrange("p (c t) -> p c t", c=2),
                in1=rct[k].unsqueeze(1).to_broadcast([128, 2, 128]), op=ALU.mult),
                reads=[("ps", pb), ("rct", k)], writes=[("zb", k)])
            py = 2 + (j % 2) * 2 + gi // 2
            for cc in range(2):
                c.op("tensor", lambda e, k=k, cc=cc, gi=gi, py=py: e.matmul(
                    ps[:, py, (gi % 2) * 256:(gi % 2 + 1) * 256], lhsT=zb[k][:, cc, :], rhs=wpb[:, gi, cc, :],
                    start=(cc == 0), stop=(cc == 1)),
                    reads=[("zb", k), "wpb"], writes=[("ps", py)])
        pbase = 2 + (j % 2) * 2
        c.op(V, lambda e, yb=yb, pbase=pbase: e.tensor_tensor(
            out=yb, in0=ps[:, pbase:pbase + 2, :].rearrange("p b j -> p (b j)"), in1=scb, op=ALU.mult),
            reads=[("ps", pbase), ("ps", pbase + 1), "scb"], writes=[ky])

    def update(j):
        yb = ybuf[j % 3]
        ky = ("ybuf", j % 3)
        c.op(V, lambda e, yb=yb, j=j: e.scalar_tensor_tensor(out=c.x_tm[:, j, :], in0=c.x_tm[:, j, :], scalar=float(ALPHA), in1=yb, op0=ALU.mult, op1=ALU.add),
             reads=[ky, ("x", j)], writes=[("x", j)])

    compute(0)
    for j in range(1, 16):
        compute(j)
        update(j - 1)
    update(15)
    c.release(m)


MLA_H = 8
Q_RANK = 384
KV_RANK = 256
ATT_SCALE = (128 + 64) ** -0.5
TWO_PI = 2.0 * math.pi


def mla_phase(c, src, pos_keys, pos_own, wlq_d, wlkv_d, qng_d, kvg_d, wuq_d, wukv_d, wo_d):
    ps = c.ps
    V, G_, A, T = "vector", "gpsimd", "scalar", "tensor"
    m0 = c.mark()
    cos_own = c.alloc([NT], BF16, parts=64)
    ss_own = c.alloc([NT], BF16, parts=64)
    cqnT = c.alloc([3, NT], BF16)
    ckvnT = c.alloc([2, SEQ], BF16)
    krT = c.alloc([SEQ], BF16)
    kmr = c.alloc([16], F32)
    invf = c.alloc([1], F32, parts=64)
    ones_bf = c.alloc([128], BF16)
    qng = c.alloc([3], F32)
    kvg = c.alloc([2], F32)
    m1 = c.mark()
    wlq = c.alloc([8, 384], BF16)
    wlkv = c.alloc([8, 384], BF16)
    xst = c.alloc([8, 256], F32)
    wl_st = xst[:, :, 0:192]
    xob = c.alloc([8, 512], BF16)
    sqr0 = c.alloc([512], BF16, parts=64)
    sq = c.alloc([3, 512], F32)
    rstd = c.alloc([512], F32)
    posi = c.alloc([512], I32, parts=64)
    ang = c.alloc([512], F32, parts=64)
    ang2 = c.alloc([512], F32, parts=64)
    cosb = c.alloc([512], BF16, parts=64)
    ssb = c.alloc([512], BF16, parts=64)
    tmpr = c.alloc([512], F32, parts=64)
    frow = c.alloc([64], F32, parts=1)

    for i in range(64):
        val = float(np.float32(10000.0) ** np.float32(-(2 * (i % 32)) / 64.0))
        c.op(G_, lambda e, i=i, val=val: e.memset(frow[0:1, i:i + 1], val), writes=["frow"])
    c.op(T, lambda e: e.matmul(ps[0:64, 7, 0:1], lhsT=frow[0:1, :], rhs=c.ones[0:1, 0:1], start=True, stop=True),
         reads=["frow", "ones"], writes=[("ps", 7)])
    c.op(V, lambda e: e.tensor_copy(out=invf, in_=ps[0:64, 7, 0:1]), reads=[("ps", 7)], writes=["invf"])
    c.op(G_, lambda e: e.memset(ones_bf, 1.0), writes=["ones_bf"])
    c.op(G_, lambda e: e.memset(krT[64:65, :], 1.0), writes=["krT_ones"])
    c.op("sync", lambda e: e.dma_start(out=qng, in_=qng_d), writes=["qng"], dkey="qng")
    c.op("sync", lambda e: e.dma_start(out=kvg, in_=kvg_d), writes=["kvg"], dkey="kvg")
    for (wd_, wb_, wk_) in ((wlq_d, wlq, "wlq"), (wlkv_d, wlkv, "wlkv")):
        for hf in range(2):
            cs = slice(hf * 192, (hf + 1) * 192)
            c.op("sync", lambda e, wd_=wd_, cs=cs: e.dma_start(out=wl_st, in_=wd_[:, :, cs]), writes=["xst"], dkey="xst")
            c.op(G_, lambda e, wb_=wb_, cs=cs: e.tensor_copy(out=wb_[:, :, cs], in_=wl_st), reads=["xst"], writes=[wk_])

    def rms_norm(banks, nch, rank, gcol, dst, tok, tag, gkey):
        for cc in range(nch):
            c.op(A, lambda e, cc=cc: e.activation(out=sq[:, cc, :], in_=ps[:, banks[cc], :], func=AF.Square),
                 reads=[("ps", banks[cc])], writes=[("sq", cc)])
        for cc in range(nch):
            c.op(T, lambda e, cc=cc: e.matmul(ps[:, 7, :], lhsT=c.ones, rhs=sq[:, cc, :], start=(cc == 0), stop=(cc == nch - 1)),
                 reads=[("sq", cc), "ones"], writes=[("ps", 7)])
        c.op(A, lambda e: e.activation(out=rstd, in_=ps[:, 7, :], func=AF.Sqrt, bias=float(RMS_EPS), scale=1.0 / rank),
             reads=[("ps", 7)], writes=["rstd"])
        c.op(V, lambda e: e.reciprocal(out=rstd, in_=rstd), reads=["rstd"], writes=["rstd"])
        for cc in range(nch):
            c.op(V, lambda e, cc=cc: e.scalar_tensor_tensor(out=dst[:, cc, tok], in0=ps[:, banks[cc], :], scalar=gcol[:, cc:cc + 1],
                                                            in1=rstd, op0=ALU.mult, op1=ALU.mult),
                 reads=[("ps", banks[cc]), "rstd", gkey], writes=[tag])

    def rope_tables(pos_ap, cdst, sdst, ck, sk):
        c.op("sync", lambda e: e.dma_start(out=posi, in_=pos_ap.partition_broadcast(64)), writes=["posi"], dkey="posi")
        c.op(V, lambda e: e.tensor_copy(out=ang, in_=posi), reads=["posi"], writes=["ang"])
        c.op(V, lambda e: e.tensor_scalar(out=ang, in0=ang, scalar1=invf[:, 0:1], scalar2=None, op0=ALU.mult), reads=["ang", "invf"], writes=["ang"])
        c.op(V, lambda e: e.tensor_scalar(out=ang2, in0=ang, scalar1=float(0.5 * math.pi), scalar2=None, op0=ALU.add),
             reads=["ang"], writes=["ang2"])
        for (a_, ak) in ((ang, "ang"), (ang2, "ang2")):
            c.op(V, lambda e, a_=a_: e.tensor_scalar(out=tmpr, in0=a_, scalar1=float(1.0 / TWO_PI), scalar2=None, op0=ALU.mult), reads=[ak], writes=["tmpr"])
            c.op(V, lambda e: e.tensor_copy(out=posi, in_=tmpr), reads=["tmpr"], writes=["posi"])
            c.op(V, lambda e: e.tensor_copy(out=tmpr, in_=posi), reads=["posi"], writes=["tmpr"])
            c.op(V, lambda e, a_=a_: e.scalar_tensor_tensor(out=a_, in0=tmpr, scalar=float(-TWO_PI), in1=a_, op0=ALU.mult, op1=ALU.add),
                 reads=["tmpr", ak], writes=[ak])
            c.op(V, lambda e, a_=a_: e.tensor_scalar(out=tmpr, in0=a_, scalar1=float(math.pi), scalar2=float(-TWO_PI), op0=ALU.is_gt, op1=ALU.mult),
                 reads=[ak], writes=["tmpr"])
            c.op(V, lambda e, a_=a_: e.tensor_tensor(out=a_, in0=a_, in1=tmpr, op=ALU.add), reads=[ak, "tmpr"], writes=[ak])
        c.op(A, lambda e: e.activation(out=cdst, in_=ang2, func=AF.Sin), reads=["ang2"], writes=[ck])
        c.op(A, lambda e: e.activation(out=sdst, in_=ang, func=AF.Sin), reads=["ang"], writes=[sk])
        c.op(G_, lambda e: e.tensor_scalar(out=sdst[0:32, :], in0=sdst[0:32, :], scalar1=-1.0, scalar2=None, op0=ALU.mult), reads=[sk], writes=[sk])

    for i in range(8):
        tok = slice(i * 512, (i + 1) * 512)
        if src[0] == "f32":
            for hf in range(2):
                c.op("sync", lambda e, i=i, hf=hf: e.dma_start(out=xst, in_=src[1][:, :, i * 512 + hf * 256:i * 512 + (hf + 1) * 256]),
                     writes=["xst"], dkey="xst")
                c.op(G_, lambda e, hf=hf: e.tensor_copy(out=xob[:, :, hf * 256:(hf + 1) * 256], in_=xst), reads=["xst"], writes=["xob"])
        else:
            sap = src[1][i // 4]
            c.op("sync", lambda e, sap=sap, i=i: e.dma_start(out=xob, in_=sap[:, :, (i % 4) * 512:(i % 4 + 1) * 512]),
                 reads=[("ex_out", i // 4)], writes=["xob"], dkey="xob")
        rope_tables(pos_keys[tok], cosb, ssb, "cosb", "ssb")
        for cc in range(2):
            for kc in range(8):
                c.op(T, lambda e, cc=cc, kc=kc: e.matmul(ps[:, 3 + cc, :], lhsT=wlkv[:, kc, cc * 128:(cc + 1) * 128], rhs=xob[:, kc, :],
                                                         start=(kc == 0), stop=(kc == 7)),
                     reads=["wlkv", "xob"], writes=[("ps", 3 + cc)])
        for r in range(2):
            for kc in range(8):
                c.op(T, lambda e, r=r, kc=kc: e.matmul(ps[0:64, 5 + r, :], lhsT=wlkv[:, kc, 256 + r * 64:256 + (r + 1) * 64], rhs=xob[:, kc, :],
                                                       start=(kc == 0), stop=(kc == 7)),
                     reads=["wlkv", "xob"], writes=[("ps", 5 + r)])
        rms_norm([3, 4], 2, KV_RANK, kvg, ckvnT, tok, "ckvnT", "kvg")
        t1, t2 = ang, ang2
        c.op(V, lambda e: e.tensor_tensor(out=t1, in0=ps[0:64, 5, :], in1=cosb, op=ALU.mult), reads=[("ps", 5), "cosb"], writes=["ang"])
        c.op(V, lambda e: e.tensor_tensor(out=t2, in0=ps[0:64, 6, :], in1=ssb, op=ALU.mult), reads=[("ps", 6), "ssb"], writes=["ang2"])
        c.op(G_, lambda e: e.tensor_tensor(out=t1, in0=t1, in1=t2, op=ALU.add), reads=["ang", "ang2"], writes=["ang"])
        c.op(G_, lambda e, tok=tok: e.tensor_copy(out=krT[0:64, tok], in_=t1), reads=["ang"], writes=["krT"])
        c.op(A, lambda e: e.activation(out=sqr0, in_=t1, func=AF.Square), reads=["ang"], writes=["sqr0"])
        c.op(T, lambda e: e.matmul(ps[0:65, 7, :], lhsT=ones_bf[0:64, 0:65], rhs=sqr0, start=True, stop=True),
             reads=["sqr0", "ones_bf"], writes=[("ps", 7)])
        c.op(V, lambda e, i=i: e.tensor_reduce(out=kmr[64:65, i:i + 1], in_=ps[64:65, 7, :], axis=AX.X, op=ALU.max),
             reads=[("ps", 7)], writes=["kmr"])
    c.op(V, lambda e: e.tensor_reduce(out=kmr[64:65, 8:9], in_=kmr[64:65, 0:8], axis=AX.X, op=ALU.max), reads=["kmr"], writes=["kmr2"])
    for qt in range(4):
        tok = slice(qt * 512, (qt + 1) * 512)
        rope_tables(pos_own[tok], cos_own[:, tok], ss_own[:, tok], ("cos_own", qt), ("ss_own", qt))
        for cc in range(3):
            for kc in range(8):
                c.op(T, lambda e, cc=cc, kc=kc, tok=tok: e.matmul(ps[:, cc, :], lhsT=wlq[:, kc, cc * 128:(cc + 1) * 128], rhs=c.xbT[:, kc, tok],
                                                                  start=(kc == 0), stop=(kc == 7)),
                     reads=["wlq", ("xbT", qt)], writes=[("ps", cc)])
        rms_norm([0, 1, 2], 3, Q_RANK, qng, cqnT, tok, "cqnT", "qng")
    c.release(m1)

    wuq_st = c.alloc([3, 256], F32)
    wuq_b = c.alloc([3, 256], BF16)
    wukv_st = c.alloc([2, 256], F32)
    wukv_b = c.alloc([2, 256], BF16)
    wo_st = c.alloc([D], F32)
    wo_b = c.alloc([D], BF16)
    PT = [c.alloc([512], BF16) for _ in range(2)]
    onf = [c.alloc([128], F32) for _ in range(2)]
    rcp = c.alloc([4], F32)
    sqk = c.alloc([512], BF16)
    sqr = c.alloc([512], BF16, parts=64)
    qrf = c.alloc([512], F32, parts=64)
    q2 = c.alloc([512], F32, parts=64)
    kmx = c.alloc([16], F32)
    brow = c.alloc([512], F32)
    al = c.xbT.rearrange("p a b -> p (a b)")
    knT = al[:, 0:4096]
    Vaug = al[:, 4096:4096 + 32 * 129].rearrange("p (a b) -> p a b", a=32)
    o0 = 4096 + 32 * 129
    qnT = al[:, o0:o0 + 2048]
    qrT = al[:, o0 + 2048:o0 + 4096]
    oT = al[:, o0 + 4096:o0 + 6144]
    c.op(G_, lambda e: e.memset(Vaug[:, :, 128:129], 1.0), writes=["Vones"])

    for h in range(MLA_H):
        c.op("sync", lambda e, h=h: e.dma_start(out=wuq_st, in_=wuq_d[h]), writes=["wuq_st"], dkey="wuq_st")
        c.op("sync", lambda e, h=h: e.dma_start(out=wukv_st, in_=wukv_d[h]), writes=["wukv_st"], dkey="wukv_st")
        c.op("sync", lambda e, h=h: e.dma_start(out=wo_st, in_=wo_d[h]), writes=["wo_st"], dkey="wo_st")
        c.op(G_, lambda e: e.tensor_copy(out=wuq_b, in_=wuq_st), reads=["wuq_st"], writes=["wuq_b"])
        c.op(G_, lambda e: e.tensor_copy(out=wukv_b, in_=wukv_st), reads=["wukv_st"], writes=["wukv_b"])
        c.op(G_, lambda e: e.tensor_copy(out=wo_b, in_=wo_st), reads=["wo_st"], writes=["wo_b"])
        for i in range(8):
            tok = slice(i * 512, (i + 1) * 512)
            for kc in range(2):
                c.op(T, lambda e, kc=kc, tok=tok: e.matmul(ps[:, 6, :], lhsT=wukv_b[:, kc, 0:128], rhs=ckvnT[:, kc, tok], start=(kc == 0), stop=(kc == 1)),
                     reads=["wukv_b", "ckvnT"], writes=[("ps", 6)])
            c.op(A, lambda e, tok=tok: e.copy(out=knT[:, tok], in_=ps[:, 6, :]), reads=[("ps", 6)], writes=["knT"])
            c.op(A, lambda e: e.activation(out=sqk, in_=ps[:, 6, :], func=AF.Square), reads=[("ps", 6)], writes=["sqk"])
            c.op(T, lambda e: e.matmul(ps[0:65, 7, :], lhsT=ones_bf[:, 0:65], rhs=sqk, start=True, stop=True),
                 reads=["sqk", "ones_bf"], writes=[("ps", 7)])
            c.op(V, lambda e, i=i: e.tensor_reduce(out=kmx[64:65, i:i + 1], in_=ps[64:65, 7, :], axis=AX.X, op=ALU.max),
                 reads=[("ps", 7)], writes=["kmx"])
            for j4 in range(4):
                kch = i * 4 + j4
                ks = slice(kch * 128, (kch + 1) * 128)
                for kc in range(2):
                    c.op(T, lambda e, kc=kc, ks=ks, j4=j4: e.matmul(ps[:, 5, j4 * 128:(j4 + 1) * 128], lhsT=ckvnT[:, kc, ks], rhs=wukv_b[:, kc, 128:256],
                                                                    start=(kc == 0), stop=(kc == 1)),
                         reads=["wukv_b", "ckvnT"], writes=[("ps", 5)])
            c.op(V, lambda e, i=i: e.tensor_copy(out=Vaug[:, i * 4:(i + 1) * 4, 0:128], in_=ps[:, 5, :].rearrange("p (a b) -> p a b", a=4)),
                 reads=[("ps", 5)], writes=["Vaug"])
        c.op(V, lambda e: e.tensor_reduce(out=kmx[64:65, 9:10], in_=kmx[64:65, 0:8], axis=AX.X, op=ALU.max), reads=["kmx"], writes=["kmx1"])
        c.op(V, lambda e: e.tensor_tensor(out=kmx[64:65, 8:9], in0=kmx[64:65, 9:10], in1=kmr[64:65, 8:9], op=ALU.add), reads=["kmx1", "kmr2"], writes=["kmx2"])
        for qt in range(4):
            tok = slice(qt * 512, (qt + 1) * 512)
            for kc in range(3):
                c.op(T, lambda e, kc=kc, tok=tok: e.matmul(ps[:, 6, :], lhsT=wuq_b[:, kc, 0:128], rhs=cqnT[:, kc, tok], start=(kc == 0), stop=(kc == 2)),
                     reads=["wuq_b", "cqnT"], writes=[("ps", 6)])
            c.op(A, lambda e, tok=tok: e.copy(out=qnT[:, tok], in_=ps[:, 6, :]), reads=[("ps", 6)], writes=["qnT"])
            c.op(A, lambda e: e.activation(out=sqk, in_=ps[:, 6, :], func=AF.Square), reads=[("ps", 6)], writes=["sqk"])
            for r in range(2):
                for kc in range(3):
                    c.op(T, lambda e, kc=kc, tok=tok, r=r: e.matmul(ps[0:64, r, :], lhsT=wuq_b[:, kc, 128 + r * 64:192 + r * 64], rhs=cqnT[:, kc, tok],
                                                                    start=(kc == 0), stop=(kc == 2)),
                         reads=["wuq_b", "cqnT"], writes=[("ps", r)])
            c.op(V, lambda e, tok=tok: e.tensor_tensor(out=qrf, in0=ps[0:64, 0, :], in1=cos_own[:, tok], op=ALU.mult),
                 reads=[("ps", 0), ("cos_own", qt)], writes=["qrf"])
            c.op(V, lambda e, tok=tok: e.tensor_tensor(out=q2, in0=ps[0:64, 1, :], in1=ss_own[:, tok], op=ALU.mult),
                 reads=[("ps", 1), ("ss_own", qt)], writes=["q2"])
            c.op(G_, lambda e: e.tensor_tensor(out=qrf, in0=qrf, in1=q2, op=ALU.add), reads=["qrf", "q2"], writes=["qrf"])
            c.op(G_, lambda e, tok=tok: e.tensor_copy(out=qrT[0:64, tok], in_=qrf), reads=["qrf"], writes=["qrT"])
            c.op(A, lambda e: e.activation(out=sqr, in_=qrf, func=AF.Square), reads=["qrf"], writes=["sqr"])
            c.op(T, lambda e: e.matmul(ps[0:65, 7, :], lhsT=ones_bf[:, 0:65], rhs=sqk, start=True, stop=False),
                 reads=["sqk", "ones_bf"], writes=[("ps", 7)])
            c.op(T, lambda e: e.matmul(ps[0:65, 7, :], lhsT=ones_bf[0:64, 0:65], rhs=sqr, start=False, stop=True),
                 reads=["sqr", "ones_bf"], writes=[("ps", 7)])
            c.op(A, lambda e: e.activation(out=brow[64:65, :], in_=ps[64:65, 7, :], func=AF.Sqrt, scale=kmx[64:65, 8:9]),
                 reads=[("ps", 7), "kmx2"], writes=["brow"])
            c.op(V, lambda e, tok=tok: e.tensor_scalar(out=qrT[64:65, tok], in0=brow[64:65, :], scalar1=-1.0, scalar2=None, op0=ALU.mult),
                 reads=["brow"], writes=["qrT"])
        for qb in range(4):
            qs_ = slice(qb * 512, (qb + 1) * 512)
            for kc in range(32):
                sb_ = kc % 2
                ks = slice(kc * 128, (kc + 1) * 128)
                c.op(T, lambda e, ks=ks, qs_=qs_, sb_=sb_: e.matmul(ps[:, sb_, :], lhsT=knT[:, ks], rhs=qnT[:, qs_], start=True, stop=False),
                     reads=["knT", "qnT"], writes=[("ps", sb_)])
                c.op(T, lambda e, ks=ks, qs_=qs_, sb_=sb_: e.matmul(ps[:, sb_, :], lhsT=krT[0:65, ks], rhs=qrT[0:65, qs_], start=False, stop=True),
                     reads=["krT", "krT_ones", "qrT"], writes=[("ps", sb_)])
                c.op(A, lambda e, sb_=sb_: e.activation(out=PT[sb_], in_=ps[:, sb_, :], func=AF.Exp, scale=float(ATT_SCALE)),
                     reads=[("ps", sb_)], writes=[("PT", sb_)])
                for q4 in range(4):
                    c.op(T, lambda e, q4=q4, sb_=sb_, kc=kc: e.matmul(ps[:, 2 + q4, 0:129], lhsT=PT[sb_][:, q4 * 128:(q4 + 1) * 128], rhs=Vaug[:, kc, :],
                                                                      start=(kc == 0), stop=(kc == 31)),
                         reads=[("PT", sb_), "Vaug", "Vones"], writes=[("ps", 2 + q4)])
            for q4 in range(4):
                k2 = q4 % 2
                c.op(V, lambda e, q4=q4: e.reciprocal(out=rcp[:, q4:q4 + 1], in_=ps[:, 2 + q4, 128:129]), reads=[("ps", 2 + q4)], writes=[("rcp", q4)])
                c.op(V, lambda e, q4=q4, k2=k2: e.tensor_scalar(out=onf[k2], in0=ps[:, 2 + q4, 0:128], scalar1=rcp[:, q4:q4 + 1], scalar2=None, op0=ALU.mult),
                     reads=[("ps", 2 + q4), ("rcp", q4)], writes=[("onf", k2)])
                c.op(T, lambda e, q4=q4, k2=k2: e.transpose(out=ps[:, 6, q4 * 128:(q4 + 1) * 128], in_=onf[k2], identity=c.ident),
                     reads=[("onf", k2), "ident"], writes=[("ps", 6)])
            c.op(A, lambda e, qs_=qs_: e.copy(out=oT[:, qs_], in_=ps[:, 6, :]), reads=[("ps", 6)], writes=["oT"])
        for tile in range(16):
            for dh in range(2):
                c.op(T, lambda e, tile=tile, dh=dh: e.matmul(ps[:, 7, :], lhsT=oT[:, tile * 128:(tile + 1) * 128], rhs=wo_b[:, dh * 512:(dh + 1) * 512],
                                                             start=True, stop=True),
                     reads=["oT", "wo_b"], writes=[("ps", 7)])
                xs = c.x_tm[:, tile, dh * 512:(dh + 1) * 512]
                c.op(V, lambda e, xs=xs: e.tensor_tensor(out=xs, in0=ps[:, 7, :], in1=xs, op=ALU.add),
                     reads=[("ps", 7), ("x", tile)], writes=[("x", tile)])
    c.release(m0)


def lay_mla(w_dqkv, q_norm, w_uq, kv_norm, w_ukv, w_o):
    kc = lambda w: np.ascontiguousarray(w.reshape(-1, 128, w.shape[1]).transpose(1, 0, 2))
    wlq = kc(w_dqkv[:, :384])
    kr = w_dqkv[:, 640:704]
    kr_sw = np.concatenate([kr[:, 32:], kr[:, :32]], axis=1)
    wlkv = kc(np.concatenate([w_dqkv[:, 384:640], kr, kr_sw], axis=1))
    qng = np.ascontiguousarray(q_norm.reshape(3, 128).T)
    kvg = np.ascontiguousarray(kv_norm.reshape(2, 128).T)
    wuq = []
    for h in range(8):
        blk = w_uq[:, h * 192:(h + 1) * 192]
        r = blk[:, 128:]
        wuq.append(kc(np.concatenate([blk[:, :128], r, r[:, 32:], r[:, :32]], axis=1)))
    wukv = [kc(w_ukv[:, h * 256:(h + 1) * 256]) for h in range(8)]
    wo = np.ascontiguousarray(w_o.reshape(8, 128, 1024))
    return dict(wlq=wlq, wlkv=wlkv, qng=qng, kvg=kvg, wuq=np.stack(wuq), wukv=np.stack(wukv), wo=wo)


ML_H = 4
LN_KS = math.log(128 ** -0.5)
BIG = 1.0e30


def mlstm_phase(c, ex, cinfo, W):
    ps = c.ps
    V, G_, A, T = "vector", "gpsimd", "scalar", "tensor"
    m0 = c.mark()
    wgate = c.alloc([8, 16], BF16)
    bg = c.alloc([4], F32, parts=4)
    convw = c.alloc([8, 5], F32)
    id4 = c.ident[0:4, 0:4]
    rst = c.alloc([512], F32, parts=4)
    rstn = c.alloc([512], F32, parts=4)
    maskP = [c.alloc([128], F32) for _ in range(2)]
    colq = c.alloc([16, 2, 20], F32)
    cwB = c.alloc([2, 32, 4], F32)
    Cfin = c.alloc([2, 4, 257], F32)
    mfin = c.alloc([2], F32, parts=4)
    ones4 = c.alloc([128], F32, parts=4)
    st4 = c.alloc([8, 16], F32)
    c.op("sync", lambda e: e.dma_start(out=st4, in_=W["wgate"]), writes=["st4"], dkey="st4")
    c.op(G_, lambda e: e.tensor_copy(out=wgate, in_=st4), reads=["st4"], writes=["wgate"])
    c.op("sync", lambda e: e.dma_start(out=bg, in_=W["bgate"]), writes=["bg"], dkey="bg")
    c.op("sync", lambda e: e.dma_start(out=convw, in_=W["conv"]), writes=["convw"], dkey="convw")
    c.op(G_, lambda e: e.memset(ones4, 1.0), writes=["ones4"])
    c.op(G_, lambda e: e.memset(rst, 1.0), writes=["rst"])
    c.op(G_, lambda e: e.memset(rst.rearrange("p (a b) -> p a b", b=64)[:, :, 0:1], 0.0), reads=["rst"], writes=["rst"])
    c.op(G_, lambda e: e.memset(rstn, 0.0), writes=["rstn"])
    c.op(G_, lambda e: e.memset(rstn.rearrange("p (a b) -> p a b", b=64)[:, :, 0:1], -BIG), reads=["rstn"], writes=["rstn"])
    for d_ in range(2):
        mk = maskP[d_]
        c.op(G_, lambda e, mk=mk: e.memset(mk, BIG), writes=[("maskP", d_)])
        for hb in range(2):
            blk = mk[64 * hb:64 * hb + 64, 64 * hb:64 * hb + 64]
            c.op(G_, lambda e, blk=blk: e.memset(blk, 0.0), reads=[("maskP", d_)], writes=[("maskP", d_)])
            if d_ == 0:
                c.op(G_, lambda e, blk=blk: e.affine_select(out=blk, in_=blk, pattern=[[1, 64]], compare_op=ALU.is_ge, fill=BIG, base=0, channel_multiplier=-1),
                     reads=[("maskP", d_)], writes=[("maskP", d_)])
            else:
                c.op(G_, lambda e, blk=blk: e.affine_select(out=blk, in_=blk, pattern=[[-1, 64]], compare_op=ALU.is_ge, fill=BIG, base=0, channel_multiplier=1),
                     reads=[("maskP", d_)], writes=[("maskP", d_)])

    gm = c.mark()
    R = {n: c.alloc([512], F32, parts=4) for n in ("li", "xf", "t0", "t1", "b", "cc", "pm", "pm2", "aw")}
    mblk = c.alloc([16], F32, parts=4)
    uL = c.alloc([8], F32, parts=4)
    cwr = c.alloc([8], F32, parts=4)
    cwx = c.alloc([8, 4], F32, parts=4)
    awall = c.alloc([16, 4], F32)

    def v3(a):
        return a.rearrange("p (a b) -> p a b", b=64)

    def gate_rows(xb, xkey, d_, m_in, own, blk_i):
        for gsel, bank in ((d_, 6), (2 + d_, 7)):
            for kc in range(8):
                c.op(T, lambda e, kc=kc, gsel=gsel, bank=bank: e.matmul(ps[0:4, bank, :], lhsT=wgate[:, kc, 4 * gsel:4 * gsel + 4], rhs=xb[:, kc, :],
                                                                          start=(kc == 0), stop=(kc == 7)),
                     reads=["wgate", xkey], writes=[("ps", bank)])
        c.op(V, lambda e: e.tensor_scalar(out=R["li"], in0=ps[0:4, 6, :], scalar1=bg[:, d_:d_ + 1], scalar2=None, op0=ALU.add),
             reads=[("ps", 6), "bg"], writes=["r_li"])
        c.op(V, lambda e: e.tensor_scalar(out=R["xf"], in0=ps[0:4, 7, :], scalar1=bg[:, 2 + d_:3 + d_], scalar2=None, op0=ALU.add),
             reads=[("ps", 7), "bg"], writes=["r_xf"])
        c.op(V, lambda e: e.scalar_tensor_tensor(out=R["t0"], in0=R["xf"], scalar=-1.0, in1=R["xf"], op0=ALU.mult, op1=ALU.max), reads=["r_xf"], writes=["r_t0"])
        c.op(A, lambda e: e.activation(out=R["t0"], in_=R["t0"], func=AF.Exp, scale=-1.0), reads=["r_t0"], writes=["r_t0"])
        c.op(A, lambda e: e.activation(out=R["t0"], in_=R["t0"], func=AF.Ln, bias=1.0, scale=1.0), reads=["r_t0"], writes=["r_t0"])
        c.op(V, lambda e: e.tensor_scalar(out=R["t1"], in0=R["xf"], scalar1=0.0, scalar2=None, op0=ALU.min), reads=["r_xf"], writes=["r_t1"])
        c.op(V, lambda e: e.tensor_tensor(out=R["t1"], in0=R["t1"], in1=R["t0"], op=ALU.subtract), reads=["r_t1", "r_t0"], writes=["r_t1"])
        c.op(V, lambda e: e.tensor_tensor_scan(out=R["b"], data0=rst, data1=R["t1"], initial=0.0, op0=ALU.mult, op1=ALU.add),
             reads=["rst", "r_t1"], writes=["r_b"])
        if d_ == 1:
            c.op(V, lambda e: e.tensor_tensor(out=v3(R["t0"]), in0=v3(R["b"])[:, :, 63:64].to_broadcast([4, 8, 64]), in1=v3(R["b"]), op=ALU.subtract),
                 reads=["r_b"], writes=["r_t0"])
            c.op(V, lambda e: e.tensor_tensor(out=R["b"], in0=R["t0"], in1=R["t1"], op=ALU.add), reads=["r_t0", "r_t1", "r_b"], writes=["r_b"])
        c.op(V, lambda e: e.tensor_tensor(out=R["cc"], in0=R["li"], in1=R["b"], op=ALU.subtract), reads=["r_li", "r_b"], writes=["r_cc"])
        if d_ == 0:
            c.op(V, lambda e: e.tensor_tensor_scan(out=R["pm"], data0=rstn, data1=R["cc"], initial=-BIG, op0=ALU.add, op1=ALU.max),
                 reads=["rstn", "r_cc"], writes=["r_pm"])
        else:
            seq = [("cc", "pm"), ("pm", "pm2"), ("pm2", "pm"), ("pm", "pm2"), ("pm2", "pm"), ("pm", "pm2")]
            for k, (sn, dn) in zip((1, 2, 4, 8, 16, 32), seq):
                src, dst = R[sn], R[dn]
                c.op(V, lambda e, src=src, dst=dst, k=k: e.tensor_tensor(out=v3(dst)[:, :, 0:64 - k], in0=v3(src)[:, :, 0:64 - k], in1=v3(src)[:, :, k:64], op=ALU.max),
                     reads=["r_" + sn], writes=["r_" + dn])
                c.op(V, lambda e, src=src, dst=dst, k=k: e.tensor_copy(out=v3(dst)[:, :, 64 - k:64], in_=v3(src)[:, :, 64 - k:64]),
                     reads=["r_" + sn, "r_" + dn], writes=["r_" + dn])
            c.op(V, lambda e: e.tensor_copy(out=R["pm"], in_=R["pm2"]), reads=["r_pm2"], writes=["r_pm"])
        last = 63 if d_ == 0 else 0
        bL = v3(R["b"])[:, :, last]
        pmL = v3(R["pm"])[:, :, last]
        order = list(range(8)) if d_ == 0 else list(range(7, -1, -1))
        c.op(V, lambda e: e.tensor_copy(out=mblk[:, order[0]:order[0] + 1], in_=m_in), reads=["mcar", "r_b", "r_pm"], writes=["mblk"])
        for i, ch in enumerate(order):
            nxt = order[i + 1] if i < 7 else 8
            c.op(V, lambda e, ch=ch, nxt=nxt: e.scalar_tensor_tensor(out=mblk[:, nxt:nxt + 1], in0=mblk[:, ch:ch + 1], scalar=pmL[:, ch:ch + 1],
                                                                     in1=bL[:, ch:ch + 1], op0=ALU.max, op1=ALU.add),
                 reads=["mblk", "r_b", "r_pm"], writes=["mblk"])
        c.op(V, lambda e: e.tensor_tensor(out=uL, in0=mblk[:, 0:8], in1=pmL, op=ALU.max), reads=["mblk", "r_pm"], writes=["uL"])
        c.op(V, lambda e: e.tensor_tensor(out=cwr, in0=mblk[:, 0:8], in1=uL, op=ALU.subtract), reads=["mblk", "uL"], writes=["cwr"])
        c.op(A, lambda e: e.activation(out=cwr, in_=cwr, func=AF.Exp), reads=["cwr"], writes=["cwr"])
        c.op(V, lambda e: e.tensor_scalar(out=R["cc"], in0=R["cc"], scalar1=float(LN_KS), scalar2=None, op0=ALU.add), reads=["r_cc", "r_pm"], writes=["r_cc"])
        c.op(V, lambda e: e.tensor_tensor(out=v3(R["aw"]), in0=v3(R["cc"]), in1=uL.unsqueeze(2).to_broadcast([4, 8, 64]), op=ALU.subtract),
             reads=["r_cc", "uL"], writes=["r_aw"])
        c.op(A, lambda e: e.activation(out=R["aw"], in_=R["aw"], func=AF.Exp), reads=["r_aw"], writes=["r_aw"])
        c.op(V, lambda e: e.tensor_tensor(out=cwx, in0=cwr.unsqueeze(2).to_broadcast([4, 8, 4]), in1=id4.unsqueeze(1).to_broadcast([4, 8, 4]), op=ALU.mult),
             reads=["cwr", "ident"], writes=["cwx"])
        c.op(T, lambda e: e.matmul(ps[:, 6, 0:32], lhsT=ones4, rhs=cwx.rearrange("p a b -> p (a b)"), start=True, stop=True),
             reads=["ones4", "cwx"], writes=[("ps", 6)])
        c.op(V, lambda e: e.tensor_copy(out=cwB[:, d_, blk_i * 8:(blk_i + 1) * 8, :], in_=ps[:, 6, 0:32].rearrange("p (a b) -> p a b", b=4)),
             reads=[("ps", 6)], writes=[("cwB", d_)])
        if own:
            c.op(V, lambda e: e.tensor_tensor(out=v3(R["pm2"]), in0=v3(R["pm"]), in1=mblk[:, 0:8].unsqueeze(2).to_broadcast([4, 8, 64]), op=ALU.max),
                 reads=["r_pm", "mblk"], writes=["r_pm2"])
            c.op(V, lambda e: e.tensor_tensor(out=v3(R["t0"]), in0=mblk[:, 0:8].unsqueeze(2).to_broadcast([4, 8, 64]), in1=v3(R["pm2"]), op=ALU.subtract),
                 reads=["r_pm2", "mblk"], writes=["r_t0"])
            c.op(A, lambda e: e.activation(out=R["t0"], in_=R["t0"], func=AF.Exp), reads=["r_t0"], writes=["r_t0"])
            c.op(V, lambda e: e.tensor_tensor(out=R["t1"], in0=R["b"], in1=R["pm2"], op=ALU.add), reads=["r_b", "r_pm2"], writes=["r_t1"])
            c.op(A, lambda e: e.activation(out=R["t1"], in_=R["t1"], func=AF.Exp, scale=-1.0), reads=["r_t1"], writes=["r_t1"])
            qs = (("cc", "r_cc"), ("t0", "r_t0"), ("t1", "r_t1"), ("aw", "r_aw"), ("pm2", "r_pm2"))
        else:
            qs = (("aw", "r_aw"),)
        for t4 in range(4):
            tile = blk_i * 4 + t4
            ts = slice(t4 * 128, (t4 + 1) * 128)
            for qi, (rn, rk) in enumerate(qs):
                c.op(T, lambda e, rn=rn, ts=ts, qi=qi: e.matmul(ps[:, 5, qi * 4:(qi + 1) * 4], lhsT=R[rn][:, ts], rhs=id4, start=True, stop=True),
                     reads=[rk, "ident"], writes=[("ps", 5)])
            if own:
                c.op(V, lambda e, tile=tile: e.tensor_copy(out=colq[:, tile, d_, :], in_=ps[:, 5, 0:20]), reads=[("ps", 5)], writes=["colq"])
            else:
                c.op(V, lambda e, tile=tile: e.tensor_copy(out=awall[:, tile, :], in_=ps[:, 5, 0:4]), reads=[("ps", 5)], writes=["awall"])

    def conv_silu(pre, prekey, ci, n, dst_f, dkey):
        c.op(V, lambda e: e.tensor_scalar(out=dst_f, in0=pre[:, 0:n], scalar1=convw[:, ci, 0:1], scalar2=None, op0=ALU.mult),
             reads=[prekey, "convw"], writes=[dkey])
        for j in range(1, 5):
            c.op(V, lambda e, j=j: e.scalar_tensor_tensor(out=dst_f, in0=pre[:, j:n + j], scalar=convw[:, ci, j:j + 1], in1=dst_f, op0=ALU.mult, op1=ALU.add),
                 reads=[prekey, "convw", dkey], writes=[dkey])
        c.op(A, lambda e: e.activation(out=dst_f, in_=dst_f, func=AF.Silu), reads=[dkey], writes=[dkey])

    p1 = c.mark()
    xst_p1 = c.alloc([8, 171], F32)
    xob_p1 = c.alloc([8, NT + 4], BF16)
    wk_b_p1 = c.alloc([8, 128], BF16)
    wv_b_p1 = c.alloc([8, 256], BF16)
    pre_p1 = c.alloc([516], F32)
    kf_p1 = c.alloc([512], F32)
    ktm_p1 = c.alloc([4, 128], BF16)
    vau_p1 = c.alloc([4, 258], BF16)
    Cst_p1 = c.alloc([257], F32)
    zcol_p1 = c.alloc([1], F32, parts=4)
    c.op(G_, lambda e: e.memset(zcol_p1, 0.0), writes=["zcol"])
    c.op(G_, lambda e: e.memset(vau_p1[:, :, 256:257], 1.0), writes=["vau1"])
    wstg_p1 = xst_p1[:, :, 0:128]
    for d_ in range(2):
        if d_ == 0:
            c.op(G_, lambda e: e.memset(xob_p1[:, :, 0:2], 0.0), writes=["xob"])
            c.op("sync", lambda e: e.dma_start(out=xob_p1[:, :, 2:NT + 2], in_=ex[0]), reads=[("ex_out", 0)], writes=["xob"], dkey="xob")
            c.op("sync", lambda e: e.dma_start(out=xob_p1[:, :, NT + 2:NT + 4], in_=ex[1][:, :, 0:2]), reads=[("ex_out", 1)], writes=["xob"], dkey="xob")
        else:
            c.op(G_, lambda e: e.memset(xob_p1[:, :, NT + 2:NT + 4], 0.0), writes=["xob"])
            c.op("sync", lambda e: e.dma_start(out=xob_p1[:, :, 2:NT + 2], in_=ex[1]), reads=[("ex_out", 1)], writes=["xob"], dkey="xob")
            c.op("sync", lambda e: e.dma_start(out=xob_p1[:, :, 0:2], in_=ex[0][:, :, NT - 2:NT]), reads=[("ex_out", 0)], writes=["xob"], dkey="xob")
        c.op(V, lambda e, d_=d_: e.tensor_copy(out=mfin[:, d_:d_ + 1], in_=zcol_p1), reads=["zcol"], writes=["mcar"])
        blocks = list(range(4)) if d_ == 0 else list(range(3, -1, -1))
        for bi in blocks:
            gate_rows(xob_p1[:, :, 2 + bi * 512:2 + (bi + 1) * 512], "xob", d_, mfin[:, d_:d_ + 1], False, bi)
            c.op(V, lambda e, d_=d_: e.tensor_copy(out=mfin[:, d_:d_ + 1], in_=mblk[:, 8:9]), reads=["mblk"], writes=["mcar"])
        for h in range(ML_H):
            c.op("sync", lambda e, h=h: e.dma_start(out=wstg_p1, in_=W["wk"][h]), writes=["xst"], dkey="xst")
            c.op(G_, lambda e: e.tensor_copy(out=wk_b_p1, in_=wstg_p1), reads=["xst"], writes=["wk_b"])
            for hf in range(2):
                c.op("sync", lambda e, h=h, hf=hf: e.dma_start(out=wstg_p1, in_=W["wv"][h, :, :, hf * 128:(hf + 1) * 128]), writes=["xst"], dkey="xst")
                c.op(G_, lambda e, hf=hf: e.tensor_copy(out=wv_b_p1[:, :, hf * 128:(hf + 1) * 128], in_=wstg_p1), reads=["xst"], writes=["wv_b"])
            c.op(G_, lambda e: e.memset(Cst_p1, 0.0), writes=["Cst"])
            for bi in blocks:
                x0 = bi * 512
                for (bank, c0, n) in ((0, 0, 512), (1, 512, 4)):
                    for kc in range(8):
                        c.op(T, lambda e, kc=kc, bank=bank, c0=c0, n=n, x0=x0: e.matmul(ps[:, bank, 0:n], lhsT=wk_b_p1[:, kc, :], rhs=xob_p1[:, kc, x0 + c0:x0 + c0 + n],
                                                                                      start=(kc == 0), stop=(kc == 7)),
                             reads=["wk_b", "xob"], writes=[("ps", bank)])
                    c.op(A, lambda e, bank=bank, c0=c0, n=n: e.copy(out=pre_p1[:, c0:c0 + n], in_=ps[:, bank, 0:n]), reads=[("ps", bank)], writes=["pre"])
                conv_silu(pre_p1, "pre", 4 + h, 512, kf_p1, "kf")
                for t4 in range(4):
                    c.op(T, lambda e, t4=t4: e.transpose(out=ps[:, 2, t4 * 128:(t4 + 1) * 128], in_=kf_p1[:, t4 * 128:(t4 + 1) * 128], identity=c.ident),
                         reads=["kf", "ident"], writes=[("ps", 2)])
                c.op(V, lambda e, h=h, bi=bi: e.tensor_tensor(out=ktm_p1, in0=ps[:, 2, :].rearrange("p (a b) -> p a b", b=128),
                                                              in1=awall[:, bi * 4:(bi + 1) * 4, h:h + 1].to_broadcast([128, 4, 128]), op=ALU.mult),
                     reads=[("ps", 2), "awall"], writes=["ktm"])
                for t4 in range(4):
                    for kc in range(8):
                        c.op(T, lambda e, kc=kc, t4=t4, x0=x0: e.matmul(ps[:, 3, 0:256], lhsT=xob_p1[:, kc, 2 + x0 + t4 * 128:2 + x0 + (t4 + 1) * 128], rhs=wv_b_p1[:, kc, :],
                                                                        start=(kc == 0), stop=(kc == 7)),
                             reads=["wv_b", "xob"], writes=[("ps", 3)])
                    c.op(A, lambda e, t4=t4: e.copy(out=vau_p1[:, t4, 0:256], in_=ps[:, 3, 0:256]), reads=[("ps", 3)], writes=["vau"])
                chunks = list(range(8)) if d_ == 0 else list(range(7, -1, -1))
                for ch in chunks:
                    t4, hb = ch // 2, ch % 2
                    rs = slice(64 * hb, 64 * hb + 64)
                    c.op(T, lambda e, t4=t4, rs=rs: e.matmul(ps[:, 4, 0:257], lhsT=ktm_p1[rs, t4, :], rhs=vau_p1[rs, t4, 0:257], start=True, stop=True),
                         reads=["ktm", "vau", "vau1"], writes=[("ps", 4)])
                    c.op(V, lambda e, h=h, ch=ch, bi=bi, d_=d_: e.scalar_tensor_tensor(out=Cst_p1, in0=Cst_p1, scalar=cwB[:, d_, bi * 8 + ch, h:h + 1],
                                                                                      in1=ps[:, 4, 0:257], op0=ALU.mult, op1=ALU.add),
                         reads=["Cst", ("cwB", d_), ("ps", 4)], writes=["Cst"])
            c.op(V, lambda e, d_=d_, h=h: e.tensor_scalar(out=Cfin[:, d_, h, :], in0=Cst_p1, scalar1=cinfo[:, 1 + d_:2 + d_], scalar2=None, op0=ALU.mult),
                 reads=["Cst", "cinfo"], writes=[("Cfin", d_)])
        c.op(V, lambda e, d_=d_: e.tensor_scalar(out=mfin[:, d_:d_ + 1], in0=mfin[:, d_:d_ + 1], scalar1=cinfo[0:4, 1 + d_:2 + d_], scalar2=None, op0=ALU.mult),
             reads=["mcar", "cinfo"], writes=["mcar"])
    c.release(p1)

    mcar2 = c.alloc([2], F32, parts=4)
    for d_ in range(2):
        c.op(V, lambda e, d_=d_: e.tensor_copy(out=mcar2[:, d_:d_ + 1], in_=mfin[:, d_:d_ + 1]), reads=["mcar"], writes=["mcar"])
        blocks = list(range(4)) if d_ == 0 else list(range(3, -1, -1))
        for bi in blocks:
            gate_rows(c.xbT[:, :, bi * 512:(bi + 1) * 512], ("xbT", bi), d_, mcar2[:, d_:d_ + 1], True, bi)
            c.op(V, lambda e, d_=d_: e.tensor_copy(out=mcar2[:, d_:d_ + 1], in_=mblk[:, 8:9]), reads=["mblk"], writes=["mcar"])
    c.release(gm)

    wst = c.alloc([8, 128], F32)
    wq_b = c.alloc([8, 128], BF16)
    wk_b = c.alloc([8, 128], BF16)
    wv_b = c.alloc([8, 256], BF16)
    wout_b = c.alloc([2, D], BF16)
    xh_b = c.alloc([8, 4], BF16)
    pre = c.alloc([516], F32)
    kf = c.alloc([512], F32)
    qT = c.alloc([NT], BF16)
    kT = c.alloc([NT], BF16)
    ktm = c.alloc([16, 128], BF16)
    kaw = c.alloc([128], BF16)
    vau = c.alloc([16, 258], BF16)
    hacc = c.alloc([16, 256], F32)
    dg = [c.alloc([128], F32) for _ in range(2)]
    Ub = [c.alloc([128], F32) for _ in range(2)]
    DwT = [c.alloc([128], F32) for _ in range(2)]
    SDT = [c.alloc([128], BF16) for _ in range(2)]
    hB = [c.alloc([257], F32) for _ in range(2)]
    hN = c.alloc([257], F32)
    Cf = c.alloc([257], F32)
    Cb = [c.alloc([258], BF16) for _ in range(2)]
    dcol = c.alloc([4], F32)
    ng = c.alloc([256], F32)
    ysb = c.alloc([256], F32)
    og = c.alloc([256], F32)
    yT = c.alloc([2, 128], BF16)
    c.op("sync", lambda e: e.dma_start(out=xh_b[:, :, 0:2], in_=ex[0][:, :, NT - 2:NT]), reads=[("ex_out", 0)], writes=["xh_b"], dkey="xh_b")
    c.op("sync", lambda e: e.dma_start(out=xh_b[:, :, 2:4], in_=ex[1][:, :, 0:2]), reads=[("ex_out", 1)], writes=["xh_b"], dkey="xh_b")
    c.op(G_, lambda e: e.tensor_scalar(out=xh_b[:, :, 0:2], in0=xh_b[:, :, 0:2], scalar1=cinfo[:, 1:2], scalar2=None, op0=ALU.mult), reads=["xh_b", "cinfo"], writes=["xh_b"])
    c.op(G_, lambda e: e.tensor_scalar(out=xh_b[:, :, 2:4], in0=xh_b[:, :, 2:4], scalar1=cinfo[:, 2:3], scalar2=None, op0=ALU.mult), reads=["xh_b", "cinfo"], writes=["xh_b"])
    c.op(G_, lambda e: e.memset(vau[:, :, 256:257], 1.0), writes=["vau1"])

    def load_w(src, dst, n, key):
        for hf in range(n // 128):
            c.op("sync", lambda e, hf=hf: e.dma_start(out=wst, in_=src[:, :, hf * 128:(hf + 1) * 128]), writes=["wst"], dkey="wst")
            c.op(G_, lambda e, hf=hf: e.tensor_copy(out=dst[:, :, hf * 128:(hf + 1) * 128], in_=wst), reads=["wst"], writes=[key])

    def proj_fm(wb, wkey, ci, dst_bf, dkey, tm):
        for qt in range(4):
            lo = qt * 512 - 2
            for kc in range(8):
                c.op(T, lambda e, kc=kc, qt=qt: e.matmul(ps[:, 0, :], lhsT=wb[:, kc, :], rhs=c.xbT[:, kc, qt * 512:(qt + 1) * 512], start=(kc == 0), stop=(kc == 7)),
                     reads=[wkey, ("xbT", qt)], writes=[("ps", 0)])
            c.op(A, lambda e: e.copy(out=pre[:, 2:514], in_=ps[:, 0, :]), reads=[("ps", 0)], writes=["pre"])
            for side, (pc, tok0) in enumerate(((0, lo), (514, lo + 514))):
                if tok0 < 0 or tok0 >= NT:
                    rhs = xh_b[:, :, 0:2] if tok0 < 0 else xh_b[:, :, 2:4]
                    rk = "xh_b"
                else:
                    rhs = c.xbT[:, :, tok0:tok0 + 2]
                    rk = ("xbT", tok0 // 512)
                for kc in range(8):
                    c.op(T, lambda e, kc=kc, rhs=rhs, side=side: e.matmul(ps[:, 1, side * 2:side * 2 + 2], lhsT=wb[:, kc, :], rhs=rhs[:, kc, :], start=(kc == 0), stop=(kc == 7)),
                         reads=[wkey, rk], writes=[("ps", 1)])
            c.op(A, lambda e: e.copy(out=pre[:, 0:2], in_=ps[:, 1, 0:2]), reads=[("ps", 1)], writes=["pre"])
            c.op(A, lambda e: e.copy(out=pre[:, 514:516], in_=ps[:, 1, 2:4]), reads=[("ps", 1)], writes=["pre"])
            conv_silu(pre, "pre", ci, 512, kf, "kf")
            c.op(G_, lambda e, qt=qt: e.tensor_copy(out=dst_bf[:, qt * 512:(qt + 1) * 512], in_=kf), reads=["kf"], writes=[dkey])
            if tm:
                for t4 in range(4):
                    c.op(T, lambda e, t4=t4: e.transpose(out=ps[:, 2, t4 * 128:(t4 + 1) * 128], in_=kf[:, t4 * 128:(t4 + 1) * 128], identity=c.ident),
                         reads=["kf", "ident"], writes=[("ps", 2)])
                c.op(A, lambda e, qt=qt: e.copy(out=ktm[:, qt * 4:(qt + 1) * 4, :], in_=ps[:, 2, :].rearrange("p (a b) -> p a b", b=128)),
                     reads=[("ps", 2)], writes=["ktm"])

    for h in range(ML_H):
        load_w(W["wq"][h], wq_b, 128, "wq")
        load_w(W["wk"][h], wk_b, 128, "wk")
        load_w(W["wv"][h], wv_b, 256, "wv")
        for cc_ in range(2):
            for q8 in range(8):
                c.op("sync", lambda e, h=h, cc_=cc_, q8=q8: e.dma_start(out=wst[:, 0, :], in_=W["wout"][h, :, cc_, q8 * 128:(q8 + 1) * 128]), writes=["wst"], dkey="wst")
                c.op(G_, lambda e, cc_=cc_, q8=q8: e.tensor_copy(out=wout_b[:, cc_, q8 * 128:(q8 + 1) * 128], in_=wst[:, 0, :]), reads=["wst"], writes=["wout"])
        c.op("sync", lambda e, h=h: e.dma_start(out=ng, in_=W["normg"][h * 256:(h + 1) * 256].partition_broadcast(128)), writes=["ng"], dkey="ng")
        proj_fm(wq_b, "wq", h, qT, "qT", False)
        proj_fm(wk_b, "wk", 4 + h, kT, "kT", True)
        for t4 in range(16):
            ts = slice(t4 * 128, (t4 + 1) * 128)
            for kc in range(8):
                c.op(T, lambda e, kc=kc, ts=ts: e.matmul(ps[:, 3, 0:256], lhsT=c.xbT[:, kc, ts], rhs=wv_b[:, kc, :], start=(kc == 0), stop=(kc == 7)),
                     reads=["wv", ("xbT", t4 // 4)], writes=[("ps", 3)])
            c.op(A, lambda e, t4=t4: e.copy(out=vau[:, t4, 0:256], in_=ps[:, 3, 0:256]), reads=[("ps", 3)], writes=["vau"])
        load_w(W["wog"][h], wv_b, 256, "wv")
        for d_ in range(2):
            c.op(V, lambda e, d_=d_, h=h: e.tensor_copy(out=Cf, in_=Cfin[:, d_, h, :]), reads=[("Cfin", d_)], writes=["Cf"])
            c.op(A, lambda e: e.copy(out=Cb[0][:, 0:257], in_=Cf), reads=["Cf"], writes=[("Cb", 0)])
            tiles = list(range(16)) if d_ == 0 else list(range(15, -1, -1))
            cbi = 0
            for ti, tile in enumerate(tiles):
                ts = slice(tile * 128, (tile + 1) * 128)
                k2 = ti % 2
                ccol = lambda qi, tile=tile, d_=d_, h=h: colq[:, tile, d_, qi * 4 + h:qi * 4 + h + 1]
                c.op(V, lambda e, k2=k2, ccol=ccol: e.tensor_scalar(out=dg[k2], in0=c.ident, scalar1=ccol(4), scalar2=None, op0=ALU.mult),
                     reads=["ident", "colq"], writes=[("dg", k2)])
                c.op(T, lambda e, k2=k2: e.matmul(ps[:, 4 + k2, 0:128], lhsT=c.ones, rhs=dg[k2], start=True, stop=True),
                     reads=["ones", ("dg", k2)], writes=[("ps", 4 + k2)])
                c.op(V, lambda e, k2=k2, d_=d_: e.tensor_tensor(out=Ub[k2], in0=ps[:, 4 + k2, 0:128], in1=maskP[d_], op=ALU.add),
                     reads=[("ps", 4 + k2), ("maskP", d_)], writes=[("Ub", k2)])
                c.op(T, lambda e, ts=ts, k2=k2: e.matmul(ps[:, k2, 0:128], lhsT=kT[:, ts], rhs=qT[:, ts], start=True, stop=True),
                     reads=["kT", "qT"], writes=[("ps", k2)])
                c.op(A, lambda e, k2=k2, ccol=ccol: e.activation(out=DwT[k2], in_=Ub[k2], func=AF.Exp, scale=-1.0, bias=ccol(0)),
                     reads=[("Ub", k2), "colq"], writes=[("DwT", k2)])
                c.op(V, lambda e, k2=k2: e.tensor_tensor(out=SDT[k2], in0=ps[:, k2, 0:128], in1=DwT[k2], op=ALU.mult),
                     reads=[("ps", k2), ("DwT", k2)], writes=[("SDT", k2)])
                c.op(T, lambda e, k2=k2, tile=tile: e.matmul(ps[:, 2 + k2, 0:257], lhsT=SDT[k2], rhs=vau[:, tile, 0:257], start=True, stop=True),
                     reads=[("SDT", k2), "vau", "vau1"], writes=[("ps", 2 + k2)])
                c.op(A, lambda e, k2=k2: e.copy(out=hB[k2], in_=ps[:, 2 + k2, 0:257]), reads=[("ps", 2 + k2)], writes=[("hB", k2)])
                c.op(V, lambda e, tile=tile, ccol=ccol: e.tensor_scalar(out=kaw, in0=ktm[:, tile, :], scalar1=ccol(3), scalar2=None, op0=ALU.mult),
                     reads=["ktm", "colq"], writes=["kaw"])
                for hb in ((0, 1) if d_ == 0 else (1, 0)):
                    rs = slice(64 * hb, 64 * hb + 64)
                    ch = tile * 2 + hb
                    cur = Cb[cbi % 2]
                    c.op(T, lambda e, ts=ts, cur=cur: e.matmul(ps[:, 6, 0:257], lhsT=qT[:, ts], rhs=cur[:, 0:257], start=True, stop=True),
                         reads=["qT", ("Cb", cbi % 2)], writes=[("ps", 6)])
                    c.op(V, lambda e, rs=rs, k2=k2, ccol=ccol: e.scalar_tensor_tensor(out=hN[rs, :], in0=ps[rs, 6, 0:257], scalar=ccol(1)[rs, :], in1=hB[k2][rs, :],
                                                                                     op0=ALU.mult, op1=ALU.add),
                         reads=[("ps", 6), "colq", ("hB", k2)], writes=["hN"])
                    c.op(T, lambda e, rs=rs, tile=tile: e.matmul(ps[:, 7, 0:257], lhsT=kaw[rs, :], rhs=vau[rs, tile, 0:257], start=True, stop=True),
                         reads=["kaw", "vau", "vau1"], writes=[("ps", 7)])
                    c.op(V, lambda e, ch=ch, d_=d_, h=h: e.scalar_tensor_tensor(out=Cf, in0=Cf, scalar=cwB[:, d_, ch, h:h + 1], in1=ps[:, 7, 0:257], op0=ALU.mult, op1=ALU.add),
                         reads=["Cf", ("cwB", d_), ("ps", 7)], writes=["Cf"])
                    cbi += 1
                    nxt = Cb[cbi % 2]
                    c.op(A, lambda e, nxt=nxt: e.copy(out=nxt[:, 0:257], in_=Cf), reads=["Cf"], writes=[("Cb", cbi % 2)])
                c.op(V, lambda e: e.scalar_tensor_tensor(out=dcol[:, 0:1], in0=hN[:, 256:257], scalar=-1.0, in1=hN[:, 256:257], op0=ALU.mult, op1=ALU.max), reads=["hN"], writes=["dcol"])
                c.op(V, lambda e, ccol=ccol: e.tensor_tensor(out=dcol[:, 0:1], in0=dcol[:, 0:1], in1=ccol(2), op=ALU.max), reads=["dcol", "colq"], writes=["dcol"])
                c.op(V, lambda e: e.reciprocal(out=dcol[:, 1:2], in_=dcol[:, 0:1]), reads=["dcol"], writes=["dcol"])
                if d_ == 0:
                    c.op(V, lambda e, tile=tile: e.tensor_scalar(out=hacc[:, tile, :], in0=hN[:, 0:256], scalar1=dcol[:, 1:2], scalar2=None, op0=ALU.mult),
                         reads=["hN", "dcol"], writes=[("hacc", tile)])
                else:
                    c.op(V, lambda e, tile=tile: e.scalar_tensor_tensor(out=hacc[:, tile, :], in0=hN[:, 0:256], scalar=dcol[:, 1:2], in1=hacc[:, tile, :], op0=ALU.mult, op1=ALU.add),
                         reads=["hN", "dcol", ("hacc", tile)], writes=[("hacc", tile)])
        for tile in range(16):
            ha = hacc[:, tile, :]
            hk = ("hacc", tile)
            ts = slice(tile * 128, (tile + 1) * 128)
            for kc in range(8):
                c.op(T, lambda e, kc=kc, ts=ts: e.matmul(ps[:, 4, 0:256], lhsT=c.xbT[:, kc, ts], rhs=wv_b[:, kc, :], start=(kc == 0), stop=(kc == 7)),
                     reads=["wv", ("xbT", tile // 4)], writes=[("ps", 4)])
            c.op(A, lambda e: e.activation(out=og, in_=ps[:, 4, 0:256], func=AF.Sigmoid), reads=[("ps", 4)], writes=["og"])
            c.op(A, lambda e, ha=ha: e.activation(out=ysb, in_=ha, func=AF.Identity, accum_out=dcol[:, 0:1]), reads=[hk, "dcol"], writes=["ysb", "dcol"])
            c.op(A, lambda e, ha=ha: e.activation(out=ysb, in_=ha, func=AF.Square, accum_out=dcol[:, 1:2]), reads=[hk, "dcol"], writes=["ysb", "dcol"])
            c.op(V, lambda e: e.tensor_scalar(out=dcol[:, 0:2], in0=dcol[:, 0:2], scalar1=1.0 / 256, scalar2=None, op0=ALU.mult), reads=["dcol"], writes=["dcol"])
            c.op(V, lambda e: e.tensor_tensor(out=dcol[:, 2:3], in0=dcol[:, 0:1], in1=dcol[:, 0:1], op=ALU.mult), reads=["dcol"], writes=["dcol"])
            c.op(V, lambda e: e.tensor_tensor(out=dcol[:, 2:3], in0=dcol[:, 1:2], in1=dcol[:, 2:3], op=ALU.subtract), reads=["dcol"], writes=["dcol"])
            c.op(A, lambda e: e.activation(out=dcol[:, 2:3], in_=dcol[:, 2:3], func=AF.Sqrt, bias=float(LN_EPS), scale=1.0), reads=["dcol"], writes=["dcol"])
            c.op(V, lambda e: e.reciprocal(out=dcol[:, 2:3], in_=dcol[:, 2:3]), reads=["dcol"], writes=["dcol"])
            c.op(V, lambda e: e.scalar_tensor_tensor(out=dcol[:, 3:4], in0=dcol[:, 0:1], scalar=-1.0, in1=dcol[:, 2:3], op0=ALU.mult, op1=ALU.mult), reads=["dcol"], writes=["dcol"])
            c.op(A, lambda e, ha=ha: e.activation(out=ysb, in_=ha, func=AF.Identity, scale=dcol[:, 2:3], bias=dcol[:, 3:4]), reads=[hk, "dcol"], writes=["ysb"])
            c.op(V, lambda e: e.tensor_tensor(out=ysb, in0=ysb, in1=ng, op=ALU.mult), reads=["ysb", "ng"], writes=["ysb"])
            c.op(V, lambda e: e.tensor_tensor(out=ysb, in0=ysb, in1=og, op=ALU.mult), reads=["ysb", "og"], writes=["ysb"])
            for cc_ in range(2):
                c.op(T, lambda e, cc_=cc_: e.transpose(out=ps[:, 0, cc_ * 128:(cc_ + 1) * 128], in_=ysb[:, cc_ * 128:(cc_ + 1) * 128], identity=c.ident),
                     reads=["ysb", "ident"], writes=[("ps", 0)])
            c.op(A, lambda e: e.copy(out=yT, in_=ps[:, 0, 0:256].rearrange("p (a b) -> p a b", b=128)), reads=[("ps", 0)], writes=["yT"])
            for dh in range(2):
                for cc_ in range(2):
                    c.op(T, lambda e, cc_=cc_, dh=dh: e.matmul(ps[:, 1, :], lhsT=yT[:, cc_, :], rhs=wout_b[:, cc_, dh * 512:(dh + 1) * 512], start=(cc_ == 0), stop=(cc_ == 1)),
                         reads=["yT", "wout"], writes=[("ps", 1)])
                xs = c.x_tm[:, tile, dh * 512:(dh + 1) * 512]
                c.op(V, lambda e, xs=xs: e.tensor_tensor(out=xs, in0=ps[:, 1, :], in1=xs, op=ALU.add), reads=[("ps", 1), ("x", tile)], writes=[("x", tile)])
    c.release(m0)


def lay_mlstm(w_in, conv, b_gate, norm_g, w_out):
    kc = lambda w: np.ascontiguousarray(w.reshape(-1, 128, w.shape[1]).transpose(1, 0, 2))
    wq = np.stack([kc(w_in[:, h * 128:(h + 1) * 128]) for h in range(4)])
    wk = np.stack([kc(w_in[:, 512 + h * 128:512 + (h + 1) * 128]) for h in range(4)])
    wv = np.stack([kc(w_in[:, 1024 + h * 256:1024 + (h + 1) * 256]) for h in range(4)])
    wog = np.stack([kc(w_in[:, 2048 + h * 256:2048 + (h + 1) * 256]) for h in range(4)])
    wgate = kc(w_in[:, 3072:3088])
    bgate = np.ascontiguousarray(b_gate.reshape(4, 4).T)
    convl = np.ascontiguousarray(conv.T.reshape(8, 128, 5).transpose(1, 0, 2))
    wout = np.ascontiguousarray(w_out.reshape(4, 2, 128, 1024).transpose(0, 2, 1, 3))
    return dict(wq=wq, wk=wk, wv=wv, wog=wog, wgate=wgate, bgate=bgate, conv=convl, normg=np.ascontiguousarray(norm_g), wout=wout)


G_DENSE = 2
G_MOE = 2


def _fm(a):
    return np.ascontiguousarray(a.T.reshape(8, 128, a.shape[0]).transpose(1, 0, 2))


def layer_weights(i, inp):
    w = {}
    mix = i % 3
    if mix == 0:
        j = i // 3
        L = lay_mla(inp["mla_w_dqkv"][j], inp["mla_q_norm"][j], inp["mla_w_uq"][j], inp["mla_kv_norm"][j], inp["mla_w_ukv"][j], inp["mla_w_o"][j])
        w.update({"mla_" + k: v for k, v in L.items()})
    elif mix == 1:
        j = i // 3
        w["pool_w"] = np.ascontiguousarray(inp["pool_w"][j].reshape(4, 2, 128, 256).transpose(0, 2, 1, 3))
        w["pool_scale"] = np.ascontiguousarray(inp["pool_scale"][j])
    else:
        j = i // 3
        L = lay_mlstm(inp["mlstm_w_in"][j], inp["mlstm_conv"][j], inp["mlstm_b_gate"][j], inp["mlstm_norm"][j], inp["mlstm_w_out"][j])
        w.update({"ml_" + k: v for k, v in L.items()})
    cidx = i // 2
    if i % 2 == 0:
        w["wg"] = lay_gu(inp["ffn_w_gate"][cidx], G_DENSE)[None]
        w["wu"] = lay_gu(inp["ffn_w_up"][cidx], G_DENSE)[None]
        w["wd"] = lay_d(inp["ffn_w_down"][cidx], G_DENSE)[None]
    else:
        w["wg"] = np.stack([lay_gu(inp["moe_w_gate"][cidx, e], G_MOE) for e in range(NE)])
        w["wu"] = np.stack([lay_gu(inp["moe_w_up"][cidx, e], G_MOE) for e in range(NE)])
        w["wd"] = np.stack([lay_d(inp["moe_w_down"][cidx, e], G_MOE) for e in range(NE)])
        w["wr"] = np.ascontiguousarray(inp["moe_router"][cidx].reshape(8, 128, NE).transpose(1, 0, 2))
    w["ln_g"] = np.ascontiguousarray(inp["ln_g"][i])
    w["ln_b"] = np.ascontiguousarray(inp["ln_b"][i])
    return w


PAIRS = [[0, 1], [2, 3], [4, 5], [6, 7]]


def exchange_x(c, exin, exout, cinfo):
    m = c.mark()
    tmp = [c.alloc([8, 512], BF16) for _ in range(2)]
    k = 0
    for h in range(2):
        fl = c_flag(cinfo, h)
        v_in = exin[h].ap().rearrange("(k p) t -> p k t", p=128)
        for q in range(4):
            tb = tmp[k % 2]
            key = ("extmp", k % 2)
            c.op("gpsimd", lambda e, tb=tb, q=q, fl=fl: e.tensor_scalar(out=tb, in0=c.xbT[:, :, q * 512:(q + 1) * 512], scalar1=fl, scalar2=None, op0=ALU.mult),
                 reads=[("xbT", q), "cinfo"], writes=[key])
            c.op("sync", lambda e, tb=tb, q=q, v_in=v_in: e.dma_start(out=v_in[:, :, q * 512:(q + 1) * 512], in_=tb),
                 reads=[key], writes=[("ex_in", h)], dkey=f"exin{k % 2}")
            k += 1
        c.op("gpsimd", lambda e, h=h: e.collective_compute("AllReduce", ALU.add, replica_groups=PAIRS,
                                                           ins=[exin[h].ap().opt()], outs=[exout[h].ap().opt()]),
             reads=[("ex_in", h)], writes=[("ex_out", h)], dkey=f"cc{h}", dinc=1)
    c.release(m)


def c_flag(cinfo, h):
    return cinfo[:, 2:3] if h == 0 else cinfo[:, 1:2]


def exchange_pool_halo(c, pxin, pxout, cinfo):
    m = c.mark()
    ta = c.alloc([D], F32)
    tb = c.alloc([D], F32)
    pi = pxin.ap()
    for h in range(2):
        fl = c_flag(cinfo, h)
        c.op("gpsimd", lambda e, fl=fl: e.tensor_scalar(out=ta[0:32, :], in0=c.x_tm[0:32, 0, :], scalar1=fl[0:32, :], scalar2=None, op0=ALU.mult),
             reads=[("x", 0), "cinfo"], writes=["pxa"])
        c.op("gpsimd", lambda e, fl=fl: e.tensor_scalar(out=tb[96:128, :], in0=c.x_tm[96:128, 15, :], scalar1=fl[96:128, :], scalar2=None, op0=ALU.mult),
             reads=[("x", 15), "cinfo"], writes=["pxb"])
        c.op("sync", lambda e, h=h: e.dma_start(out=pi[h * 16:h * 16 + 8, :], in_=ta[0:8, :]), reads=["pxa"], writes=["px_in"], dkey="pxin")
        c.op("sync", lambda e, h=h: e.dma_start(out=pi[h * 16 + 8:h * 16 + 16, :], in_=tb[120:128, :]), reads=["pxb"], writes=["px_in"], dkey="pxin")
    c.op("gpsimd", lambda e: e.collective_compute("AllReduce", ALU.add, replica_groups=PAIRS, ins=[pxin.ap().opt()], outs=[pxout.ap().opt()]),
         reads=["px_in"], writes=["px_out"], dkey="ccp", dinc=1)
    c.release(m)


DEBUG_DUMP = False
FORCE_NONLAST = False
STOP_STAGE = None
N_LAYERS = DEPTH


def build_fused(wshapes):
    nc = bass.Bass("TRN2", target_bir_lowering=False)
    with ExitStack() as st:
        c = Ctx(nc, st)
        c.init_consts()
        Wl = [{k[3:]: c.dram_in(k, shp) for k, shp in wshapes.items() if k.startswith(f"L{i}_")} for i in range(DEPTH)]
        x_own = c.dram_in("x_own", [NT, D])
        xT_seq = c.dram_in("xT_seq", [128, 8, SEQ])
        pos_seq = c.dram_in("pos_seq", [SEQ], I32)
        pos_own = c.dram_in("pos_own", [NT], I32)
        ci_d = c.dram_in("cinfo", [128, 4])
        out = c.dram_out("out", [NT, D])
        exin = [nc.dram_tensor(f"exin{h}", [D, NT], BF16) for h in range(2)]
        exout = [nc.dram_tensor(f"exout{h}", [D, NT], BF16) for h in range(2)]
        pxin = nc.dram_tensor("pxin", [32, D], F32)
        pxout = nc.dram_tensor("pxout", [32, D], F32)
        exv = [t.ap().rearrange("(k p) t -> p k t", p=128) for t in exout]
        cinfo = c.alloc([4], F32)
        c.op("sync", lambda e: e.dma_start(out=cinfo, in_=ci_d), writes=["cinfo"], dkey="cinfo")
        load_x(c, x_own)
        for i in range(N_LAYERS):
            W = Wl[i]
            mk = c.mark()
            moe = (i % 2 == 1)
            router = None
            if moe:
                wr = c.alloc([8, NE], F32)
                logits = c.alloc([16, NE], F32)
                comb = c.alloc([16, NE], F32)
                c.op("sync", lambda e, wr=wr, W=W: e.dma_start(out=wr, in_=W["wr"]), writes=["wr"], dkey="wr")
                router = (wr, logits)
            mix = i % 3
            if mix == 0:
                if i == 0:
                    make_xbT(c)
                    src = ("f32", xT_seq)
                else:
                    src = ("bf16", exv)
                scale_x(c)
                mla_phase(c, src, pos_seq, pos_own, W["mla_wlq"], W["mla_wlkv"], W["mla_qng"], W["mla_kvg"], W["mla_wuq"], W["mla_wukv"], W["mla_wo"])
            elif mix == 1:
                exchange_pool_halo(c, pxin, pxout, cinfo)
                po = pxout.ap()

                def halo_fill(hprev, hnext):
                    c.op("gpsimd", lambda e: e.memset(hprev, 0.0), writes=["hprev"])
                    c.op("gpsimd", lambda e: e.memset(hnext, 0.0), writes=["hnext"])
                    c.op("sync", lambda e: e.dma_start(out=hprev[120:128, :], in_=po[8:16, :]), reads=["px_out"], writes=["hprev"], dkey="hprev")
                    c.op("sync", lambda e: e.dma_start(out=hnext[0:8, :], in_=po[16:24, :]), reads=["px_out"], writes=["hnext"], dkey="hnext")
                    c.op("gpsimd", lambda e: e.tensor_scalar(out=hprev[96:128, :], in0=hprev[96:128, :], scalar1=cinfo[96:128, 1:2], scalar2=None, op0=ALU.mult),
                         reads=["hprev", "cinfo"], writes=["hprev"])
                    c.op("gpsimd", lambda e: e.tensor_scalar(out=hnext[0:32, :], in0=hnext[0:32, :], scalar1=cinfo[0:32, 2:3], scalar2=None, op0=ALU.mult),
                         reads=["hnext", "cinfo"], writes=["hnext"])

                pool_phase(c, halo_fill, cinfo, W["pool_w"], W["pool_scale"])
            else:
                scale_x(c)
                mlstm_phase(c, exv, cinfo, {k[3:]: v for k, v in W.items() if k.startswith("ml_")})
            if STOP_STAGE == (i, "mixer"):
                ov = out.rearrange("(t p) d -> p t d", p=128)
                for t in range(16):
                    c.op("sync", lambda e, t=t, ov=ov: e.dma_start(out=ov[:, t, :], in_=c.x_tm[:, t, :]), reads=[("x", t)], dkey=f"dbg{t % 4}")
                break
            layernorm(c, W["ln_g"][0], W["ln_b"][0], router=router)
            if STOP_STAGE == (i, "ln1"):
                ov = out.rearrange("(t p) d -> p t d", p=128)
                for t in range(16):
                    c.op("sync", lambda e, t=t, ov=ov: e.dma_start(out=ov[:, t, :], in_=c.x_tm[:, t, :]), reads=[("x", t)], dkey=f"dbg{t % 4}")
                break
            if moe:
                moe_route(c, logits, comb)
                scale_x(c)
                ffn_phase(c, W["wg"], W["wu"], W["wd"], NE, D_FFE, G_MOE, moe=comb)
            else:
                scale_x(c)
                ffn_phase(c, W["wg"], W["wu"], W["wd"], 1, D_FF, G_DENSE)
            last = (i == N_LAYERS - 1)
            if FORCE_NONLAST and last:
                layernorm(c, W["ln_g"][1], W["ln_b"][1], out_dram=None)
                ov = out.rearrange("(t p) d -> p t d", p=128)
                for t in range(16):
                    c.op("sync", lambda e, t=t, ov=ov: e.dma_start(out=ov[:, t, :], in_=c.x_tm[:, t, :]), reads=[("x", t)], dkey=f"dbg{t % 4}")
                c.release(mk)
                continue
            layernorm(c, W["ln_g"][1], W["ln_b"][1], out_dram=out if last else None)
            if DEBUG_DUMP and not last:
                dbg = c.dram_out(f"dbg{i}", [NT, D]).rearrange("(t p) d -> p t d", p=128)
                for t in range(16):
                    c.op("sync", lambda e, t=t, dbg=dbg: e.dma_start(out=dbg[:, t, :], in_=c.x_tm[:, t, :]), reads=[("x", t)], dkey=f"dbg{t % 4}")
            if not last and (i + 1) % 3 != 1:
                exchange_x(c, exin, exout, cinfo)
            c.release(mk)
        c.P.emit()
    return nc


def kernel(**inputs):
    inp = {k: np.asarray(v) for k, v in inputs.items()}
    x = np.ascontiguousarray(inp["x"], dtype=np.float32)
    positions = np.asarray(inp["positions"]).astype(np.int32)
    weights = {}
    for i in range(N_LAYERS):
        if STOP_STAGE is not None and STOP_STAGE[0] == i and i % 2 == 1:
            inp = dict(inp)
            for kk in ("moe_w_gate", "moe_w_up", "moe_w_down"):
                inp[kk] = inp[kk][:, :, :, :512] if kk != "moe_w_down" else inp[kk][:, :, :512, :]
        for k, v in layer_weights(i, inp).items():
            weights[f"L{i}_{k}"] = v
    nc = build_fused({k: list(v.shape) for k, v in weights.items()})
    xT = [_fm(x[b]) for b in range(4)]
    in_maps = []
    for core in range(8):
        bi, hf = core // 2, core % 2
        ci = np.zeros((128, 4), np.float32)
        ci[:, 0] = hf * NT
        ci[:, 1] = float(hf == 1)
        ci[:, 2] = float(hf == 0)
        m = dict(weights)
        m["x_own"] = np.ascontiguousarray(x[bi, hf * NT:(hf + 1) * NT])
        m["xT_seq"] = xT[bi]
        m["pos_seq"] = np.ascontiguousarray(positions[bi])
        m["pos_own"] = np.ascontiguousarray(positions[bi, hf * NT:(hf + 1) * NT])
        m["cinfo"] = ci
        in_maps.append(m)
    res = run_bass_kernel_spmd(nc, in_maps, core_ids=list(range(8)))
    outs = [np.asarray(r["out"]) for r in res.results]
    return np.stack([np.concatenate([outs[2 * b], outs[2 * b + 1]], axis=0) for b in range(4)]).astype(np.float32)
```

```python
import math
from contextlib import ExitStack

import numpy as np
import concourse.bass as bass
import concourse.mybir as mybir
from concourse.bass_utils import run_bass_kernel_spmd

F32 = mybir.dt.float32
BF16 = mybir.dt.bfloat16
I32 = mybir.dt.int32
AF = mybir.ActivationFunctionType
ALU = mybir.AluOpType
AX = mybir.AxisListType

D = 1024
NT = 2048
SEQ = 4096
DEPTH = 4
ALPHA = (2.0 * DEPTH) ** 0.25
LN_EPS = 1e-5
RMS_EPS = 1e-6
D_FF = 2816
D_FFE = 3584
NE = 8

ENGS = ("sync", "scalar", "vector", "gpsimd", "tensor")


class Op:
    __slots__ = ("eng", "fn", "waits", "dkey", "dval", "sig", "idx", "epoch", "dinc")

    def __init__(self, eng, fn):
        self.eng = eng
        self.fn = fn
        self.waits = []
        self.dkey = None
        self.dval = 0
        self.sig = None
        self.idx = None
        self.epoch = 0
        self.dinc = 16


class Prog:
    def __init__(self, nc):
        self.nc = nc
        self.ops = {e: [] for e in ENGS}
        self.last_write = {}
        self.readers = {}
        self.dcount = {}
        self.epoch = 0
        self.waited_e = {}
        self.waited_d = {}
        self.n_epochs = 1
        self.epoch_dkeys = set()

    def op(self, eng, fn, reads=(), writes=(), dkey=None, dinc=16):
        o = Op(eng, fn)
        o.dinc = dinc
        o.epoch = self.epoch
        o.idx = len(self.ops[eng])
        psr = [k for k in reads if isinstance(k, tuple) and k[0] == "ps"]
        if psr:
            writes = list(writes) + [k for k in psr if k not in writes]
        deps = []
        for k in reads:
            w = self.last_write.get(k)
            if w is not None:
                deps.append(w)
        for k in writes:
            w = self.last_write.get(k)
            if w is not None:
                deps.append(w)
            deps.extend(self.readers.get(k, ()))
        for d in deps:
            self._add_wait(o, d)
        if dkey is not None:
            key = dkey
            self.epoch_dkeys.add(key)
            self.dcount[key] = self.dcount.get(key, 0) + dinc
            o.dkey = key
            o.dval = self.dcount[key]
            tok = ("d", key, o.dval)
        else:
            tok = ("e", eng, o.idx)
        for k in writes:
            self.last_write[k] = tok
            self.readers[k] = []
        for k in reads:
            self.readers.setdefault(k, []).append(tok)
        self.ops[eng].append(o)
        return o

    def _add_wait(self, o, tok):
        eng = o.eng
        if tok[0] == "d":
            _, key, val = tok
            pk = (eng, key)
            if self.waited_d.get(pk, 0) >= val:
                return
            self.waited_d[pk] = val
            o.waits.append(tok)
        else:
            _, peng, pidx = tok
            if peng == eng and eng in ("tensor", "sync"):
                return
            pk = (eng, peng)
            if self.waited_e.get(pk, -1) >= pidx:
                return
            self.waited_e[pk] = pidx
            o.waits.append(tok)

    def barrier(self):
        lasts = {}
        for e in ENGS:
            li = -1
            for o in reversed(self.ops[e]):
                if o.fn is not None and o.dkey is None:
                    li = o.idx
                    break
            lasts[e] = li
        dk = [(k, self.dcount[k]) for k in sorted(self.epoch_dkeys, key=str)]
        self.epoch_dkeys = set()
        for e in ENGS:
            o = Op(e, None)
            o.epoch = self.epoch
            o.idx = len(self.ops[e])
            for pe in ENGS:
                if pe != e and lasts[pe] >= 0:
                    self._add_wait(o, ("e", pe, lasts[pe]))
            for k, v in dk:
                self._add_wait(o, ("d", k, v))
            self.ops[e].append(o)
        self.last_write = {}
        self.readers = {}
        self.epoch += 1
        self.n_epochs = self.epoch + 1
        self.waited_e = {}
        self.waited_d = {}

    def emit(self):
        nc = self.nc
        self.barrier()
        need = {e: set() for e in ENGS}
        for e in ENGS:
            for o in self.ops[e]:
                for w in o.waits:
                    if w[0] == "e":
                        need[w[1]].add(w[2])
        K_ROT = 4
        sigval = {}
        self.maxsig = 0
        for e in ENGS:
            cnt = {}
            for o in self.ops[e]:
                if o.idx in need[e]:
                    r = o.epoch % K_ROT
                    c = cnt.get(r, 0) + 1
                    cnt[r] = c
                    o.sig = c
                    sigval[(e, o.idx)] = (r, c)
                    self.maxsig = max(self.maxsig, c)
        dkeys = sorted(self.dcount.keys(), key=str)
        self.n_sems = 5 * K_ROT + len(dkeys)
        with ExitStack() as st:
            for i in range(getattr(self, "pad_sems", 0)):
                st.enter_context(nc.semaphore(f"pad_{i}"))
            esem = {}
            for e in ENGS:
                for ep in range(K_ROT):
                    esem[(e, ep)] = st.enter_context(nc.semaphore(f"s_{e}_{ep}"))
            dsem = {}
            for i, k in enumerate(dkeys):
                dsem[k] = st.enter_context(nc.semaphore(f"d_{i}"))
            block = st.enter_context(nc.Block())

            def run(ename):
                def body(eng):
                    for o in self.ops[ename]:
                        for w in o.waits:
                            if w[0] == "d":
                                eng.wait_ge(dsem[w[1]], w[2])
                            else:
                                ep, c = sigval[(w[1], w[2])]
                                eng.wait_ge(esem[(w[1], ep)], c)
                        if o.fn is None:
                            continue
                        ins = o.fn(eng)
                        if o.dkey is not None:
                            ins.then_inc(dsem[o.dkey], o.dinc)
                        elif o.sig is not None:
                            ins.then_inc(esem[(ename, o.epoch % K_ROT)], 1)
                return body

            block.sync(run("sync"))
            block.scalar(run("scalar"))
            block.vector(run("vector"))
            block.gpsimd(run("gpsimd"))
            block.tensor(run("tensor"))


ARENA_W = 49000


class Ctx:
    def __init__(self, nc, st):
        self.nc = nc
        self.P = Prog(nc)
        self.st = st
        self.arena = st.enter_context(nc.sbuf_tensor("arena", [128, ARENA_W], F32))
        self.top = 0
        self.ps = st.enter_context(nc.psum_tensor("ps", [128, 8, 512], F32))
        self.x_tm = self.alloc([16, D], F32)
        self.xbT = self.alloc([8, NT], BF16)
        self.ident = self.alloc([128], F32)
        self.ones = self.alloc([128], F32)
        self.stat = self.alloc([16, 8], F32)
        self.n_in = 0

    def alloc(self, shape, dt, parts=128):
        n = 1
        for v in shape:
            n *= v
        words = n if dt in (F32, I32) else (n + 1) // 2
        off = self.top
        self.top += words
        assert self.top <= ARENA_W, f"arena overflow {self.top}"
        v = self.arena[0:parts, off:off + words]
        if dt == BF16:
            v = v.bitcast(BF16)
        elif dt == I32:
            v = v.bitcast(I32)
        if len(shape) == 2:
            v = v.rearrange("p (a b) -> p a b", a=shape[0])
        elif len(shape) == 3:
            v = v.rearrange("p (a b c) -> p a b c", a=shape[0], b=shape[1])
        return v

    def mark(self):
        return self.top

    def release(self, m):
        self.P.barrier()
        self.top = m

    def dram_in(self, name, shape, dt=F32):
        return self.nc.dram_tensor(name, list(shape), dt, kind="ExternalInput").ap()

    def dram_out(self, name, shape, dt=F32):
        return self.nc.dram_tensor(name, list(shape), dt, kind="ExternalOutput").ap()

    def op(self, *a, **k):
        return self.P.op(*a, **k)

    def init_consts(self):
        P = self.P
        ident, ones = self.ident, self.ones
        P.op("gpsimd", lambda e: e.memset(ones, 1.0), writes=["ones"])
        P.op("gpsimd", lambda e: e.memset(ident, 1.0), writes=["ident"])
        P.op("gpsimd", lambda e: e.affine_select(
            out=ident, in_=ident, pattern=[[-1, 128]], compare_op=ALU.is_equal,
            fill=0.0, base=0, channel_multiplier=1), reads=["ident"], writes=["ident"])


def load_x(c, x_own):
    xv = x_own.rearrange("(t p) d -> p t d", p=128)
    for q in range(4):
        c.op("sync", lambda e, q=q: e.dma_start(out=c.x_tm[:, 4 * q:4 * q + 4, :], in_=xv[:, 4 * q:4 * q + 4, :]),
             writes=[("x", t) for t in range(4 * q, 4 * q + 4)], dkey=f"xin{q}")


def make_xbT(c, tiles=range(16), router=None):
    ps = c.ps
    for t in tiles:
        b0 = 6
        for kc in range(8):
            c.op("tensor", lambda e, t=t, kc=kc: e.transpose(
                out=ps[:, b0 + kc // 4, (kc % 4) * 128:(kc % 4 + 1) * 128],
                in_=c.x_tm[:, t, kc * 128:(kc + 1) * 128], identity=c.ident),
                reads=[("x", t), "ident"], writes=[("ps", b0 + kc // 4)])
        src = ps[:, 6:8, :].rearrange("p b (k j) -> p (b k) j", j=128)
        c.op("scalar", lambda e, t=t, src=src: e.copy(out=c.xbT[:, :, t * 128:(t + 1) * 128], in_=src),
             reads=[("ps", 6), ("ps", 7)], writes=[("xbT", t // 4)])
        if router is not None:
            wr, logits, xtf = router
            c.op("vector", lambda e, src=src: e.tensor_copy(out=xtf, in_=src),
                 reads=[("ps", 6), ("ps", 7)], writes=["xtf"])
            for kc in range(8):
                c.op("tensor", lambda e, t=t, kc=kc: e.matmul(
                    ps[:, 5, 0:8], lhsT=xtf[:, kc, :], rhs=wr[:, kc, :], start=(kc == 0), stop=(kc == 7)),
                    reads=["xtf", "wr"], writes=[("ps", 5)])
            c.op("vector", lambda e, t=t: e.tensor_copy(out=logits[:, t, :], in_=ps[:, 5, 0:8]),
                 reads=[("ps", 5)], writes=["logits"])


def scale_x(c):
    for t in range(16):
        c.op("gpsimd", lambda e, t=t: e.tensor_scalar(
            out=c.x_tm[:, t, :], in0=c.x_tm[:, t, :], scalar1=float(ALPHA), scalar2=None, op0=ALU.mult),
            reads=[("x", t)], writes=[("x", t)])


def layernorm(c, g_ap, b_ap, router=None, out_dram=None):
    stat = c.stat
    m = c.mark()
    gb = c.alloc([2, D], F32)
    c.gb = gb
    if router is not None:
        router = (router[0], router[1], c.alloc([8, 128], F32))
    c.op("sync", lambda e: e.dma_start(out=gb[:, 0, :], in_=g_ap.partition_broadcast(128)), writes=["gb"], dkey="gb")
    c.op("sync", lambda e: e.dma_start(out=gb[:, 1, :], in_=b_ap.partition_broadcast(128)), writes=["gb"], dkey="gb")
    junk = c.alloc([D], BF16)
    xn = [c.alloc([D], F32) for _ in range(2)]
    col = lambda i: stat[:, :, i]
    for t in range(16):
        xt = c.x_tm[:, t, :]
        c.op("scalar", lambda e, xt=xt, t=t: e.activation(out=junk, in_=xt, func=AF.Identity, accum_out=stat[:, t, 0:1]),
             reads=[("x", t)], writes=[("stA", t)])
        c.op("scalar", lambda e, xt=xt, t=t: e.activation(out=junk, in_=xt, func=AF.Square, accum_out=stat[:, t, 1:2]),
             reads=[("x", t)], writes=[("stB", t)])
    V = "vector"
    c.op(V, lambda e: e.tensor_scalar(out=stat[:, :, 2:4], in0=stat[:, :, 0:2], scalar1=1.0 / D, scalar2=None, op0=ALU.mult),
         reads=[("stA", t) for t in range(16)] + [("stB", t) for t in range(16)], writes=["stat"])
    c.op(V, lambda e: e.tensor_tensor(out=col(4), in0=col(2), in1=col(2), op=ALU.mult), reads=["stat"], writes=["stat"])
    c.op(V, lambda e: e.tensor_tensor(out=col(5), in0=col(3), in1=col(4), op=ALU.subtract), reads=["stat"], writes=["stat"])
    c.op("scalar", lambda e: e.activation(out=col(6), in_=col(5), func=AF.Sqrt, bias=float(LN_EPS), scale=1.0), reads=["stat"], writes=["stat"])
    c.op(V, lambda e: e.reciprocal(out=col(6), in_=col(6)), reads=["stat"], writes=["stat"])
    c.op(V, lambda e: e.scalar_tensor_tensor(out=col(7), in0=col(2), scalar=-1.0, in1=col(6), op0=ALU.mult, op1=ALU.mult),
         reads=["stat"], writes=["stat"])
    ov = out_dram.rearrange("(t p) d -> p t d", p=128) if out_dram is not None else None
    for t in range(16):
        xt = c.x_tm[:, t, :]
        xb = xn[t % 2]
        kx = ("xn", t % 2)
        c.op("scalar", lambda e, xt=xt, t=t, xb=xb: e.activation(out=xb, in_=xt, func=AF.Identity, scale=stat[:, t, 6:7], bias=stat[:, t, 7:8]),
             reads=[("x", t), "stat"], writes=[kx])
        c.op("vector", lambda e, xb=xb: e.tensor_tensor(out=xb, in0=xb, in1=gb[:, 0, :], op=ALU.mult),
             reads=[kx, "gb"], writes=[kx])
        c.op("gpsimd", lambda e, xt=xt, xb=xb: e.tensor_tensor(out=xt, in0=xb, in1=gb[:, 1, :], op=ALU.add),
             reads=[kx, "gb"], writes=[("x", t)])
        if ov is not None:
            c.op("sync", lambda e, t=t: e.dma_start(out=ov[:, t, :], in_=c.x_tm[:, t, :]),
                 reads=[("x", t)], dkey=f"out{t % 4}")
        else:
            make_xbT(c, tiles=[t], router=router)
    c.release(m)


def ffn_phase(c, wg, wu, wd, n_exp, n_f, G, moe=None):
    ps = c.ps
    n_grp = n_f // (G * 128)
    GW = G * 128
    NSTG = 2
    m = c.mark()
    stg_g = [c.alloc([8, GW], F32) for i in range(NSTG)]
    stg_u = [c.alloc([8, GW], F32) for i in range(NSTG)]
    stg_d = [c.alloc([G, D], F32) for i in range(NSTG)]
    wgb = [c.alloc([8, GW], BF16) for i in range(2)]
    wub = [c.alloc([8, GW], BF16) for i in range(2)]
    wdb = [c.alloc([G, D], BF16) for i in range(2)]
    sg = [c.alloc([512], F32) for i in range(2)]
    hb = [c.alloc([G, 512], BF16) for i in range(2)]
    it = 0
    hcnt = 0
    pending = None

    def emit_gateup(b_, tt, hs, fc):
        pg = (2 * fc) % 4
        pu = (2 * fc + 1) % 4
        for kc in range(8):
            c.op("tensor", lambda e, kc=kc: e.matmul(
                ps[:, pg, :], lhsT=wgb[b_][:, kc, fc * 128:(fc + 1) * 128],
                rhs=c.xbT[:, kc, tt * 512:(tt + 1) * 512], start=(kc == 0), stop=(kc == 7)),
                reads=[("wgb", b_), ("xbT", tt)], writes=[("ps", pg)])
        for kc in range(8):
            c.op("tensor", lambda e, kc=kc: e.matmul(
                ps[:, pu, :], lhsT=wub[b_][:, kc, fc * 128:(fc + 1) * 128],
                rhs=c.xbT[:, kc, tt * 512:(tt + 1) * 512], start=(kc == 0), stop=(kc == 7)),
                reads=[("wub", b_), ("xbT", tt)], writes=[("ps", pu)])
        si = fc % 2
        c.op("scalar", lambda e: e.activation(out=sg[si], in_=ps[:, pg, :], func=AF.Silu),
             reads=[("ps", pg)], writes=[("sg", si)])
        c.op("vector", lambda e: e.tensor_tensor(out=hb[hs][:, fc, :], in0=ps[:, pu, :], in1=sg[si], op=ALU.mult),
             reads=[("ps", pu), ("sg", si)], writes=[("hb", hs)])

    def emit_down(ex, b_, tt, hs):
        for ts in range(4):
            tile = tt * 4 + ts
            for dh in range(2):
                py = 4 + (ts * 2 + dh) % 2
                for fc in range(G):
                    c.op("tensor", lambda e, fc=fc, ts=ts, dh=dh, py=py: e.matmul(
                        ps[:, py, :], lhsT=hb[hs][:, fc, ts * 128:(ts + 1) * 128],
                        rhs=wdb[b_][:, fc, dh * 512:(dh + 1) * 512], start=(fc == 0), stop=(fc == G - 1)),
                        reads=[("hb", hs), ("wdb", b_)], writes=[("ps", py)])
                xs = c.x_tm[:, tile, dh * 512:(dh + 1) * 512]
                if moe is None:
                    c.op("vector", lambda e, xs=xs, py=py: e.tensor_tensor(out=xs, in0=ps[:, py, :], in1=xs, op=ALU.add),
                         reads=[("ps", py), ("x", tile)], writes=[("x", tile)])
                else:
                    c.op("vector", lambda e, xs=xs, py=py, tile=tile: e.scalar_tensor_tensor(
                        out=xs, in0=ps[:, py, :], scalar=moe[:, tile, ex:ex + 1], in1=xs, op0=ALU.mult, op1=ALU.add),
                        reads=[("ps", py), ("x", tile), "comb"], writes=[("x", tile)])

    for ex in range(n_exp):
        for g in range(n_grp):
            s_ = it % NSTG
            b_ = it % 2
            c.op("sync", lambda e, ex=ex, g=g, s_=s_: e.dma_start(out=stg_g[s_], in_=wg[ex, g]),
                 writes=[("stg_g", s_)], dkey=f"stg_g{s_}")
            c.op("sync", lambda e, ex=ex, g=g, s_=s_: e.dma_start(out=stg_u[s_], in_=wu[ex, g]),
                 writes=[("stg_u", s_)], dkey=f"stg_u{s_}")
            c.op("sync", lambda e, ex=ex, g=g, s_=s_: e.dma_start(out=stg_d[s_], in_=wd[ex, g]),
                 writes=[("stg_d", s_)], dkey=f"stg_d{s_}")
            c.op("gpsimd", lambda e, s_=s_, b_=b_: e.tensor_copy(out=wgb[b_], in_=stg_g[s_]),
                 reads=[("stg_g", s_)], writes=[("wgb", b_)])
            c.op("gpsimd", lambda e, s_=s_, b_=b_: e.tensor_copy(out=wub[b_], in_=stg_u[s_]),
                 reads=[("stg_u", s_)], writes=[("wub", b_)])
            c.op("gpsimd", lambda e, s_=s_, b_=b_: e.tensor_copy(out=wdb[b_], in_=stg_d[s_]),
                 reads=[("stg_d", s_)], writes=[("wdb", b_)])
            for tt in range(4):
                hs = hcnt % 2
                hcnt += 1
                for fc in range(G):
                    emit_gateup(b_, tt, hs, fc)
                    if fc == 0 and pending is not None:
                        emit_down(*pending)
                        pending = None
                pending = (ex, b_, tt, hs)
            it += 1
    if pending is not None:
        emit_down(*pending)
    c.release(m)


def moe_route(c, logits, comb):
    m = c.mark()
    m1 = c.alloc([16], F32)
    m2 = c.alloc([16], F32)
    t1 = c.alloc([16, 8], F32)
    l2 = c.alloc([16, 8], F32)
    sel = c.alloc([16, 8], F32)
    w1 = c.alloc([16], F32)
    w2 = c.alloc([16], F32)
    V = "vector"
    bc = lambda a: a.unsqueeze(2).to_broadcast([128, 16, 8])
    c.op(V, lambda e: e.tensor_reduce(out=m1, in_=logits, axis=AX.X, op=ALU.max), reads=["logits"], writes=["rt_m1"])
    c.op(V, lambda e: e.tensor_tensor(out=t1, in0=logits, in1=bc(m1), op=ALU.is_equal), reads=["logits", "rt_m1"], writes=["rt_t1"])
    c.op(V, lambda e: e.scalar_tensor_tensor(out=l2, in0=t1, scalar=-1e30, in1=logits, op0=ALU.mult, op1=ALU.add),
         reads=["rt_t1", "logits"], writes=["rt_l2"])
    c.op(V, lambda e: e.tensor_reduce(out=m2, in_=l2, axis=AX.X, op=ALU.max), reads=["rt_l2"], writes=["rt_m2"])
    c.op(V, lambda e: e.tensor_tensor(out=sel, in0=l2, in1=bc(m2), op=ALU.is_equal), reads=["rt_l2", "rt_m2"], writes=["rt_sel"])
    c.op(V, lambda e: e.tensor_tensor(out=w2, in0=m2, in1=m1, op=ALU.subtract), reads=["rt_m1", "rt_m2"], writes=["rt_w2"])
    c.op("scalar", lambda e: e.activation(out=w2, in_=w2, func=AF.Sigmoid), reads=["rt_w2"], writes=["rt_w2"])
    c.op(V, lambda e: e.tensor_scalar(out=w1, in0=w2, scalar1=-1.0, scalar2=1.0, op0=ALU.mult, op1=ALU.add), reads=["rt_w2"], writes=["rt_w1"])
    c.op(V, lambda e: e.tensor_tensor(out=t1, in0=t1, in1=bc(w1), op=ALU.mult), reads=["rt_t1", "rt_w1"], writes=["rt_t1"])
    c.op(V, lambda e: e.tensor_tensor(out=sel, in0=sel, in1=bc(w2), op=ALU.mult), reads=["rt_sel", "rt_w2"], writes=["rt_sel"])
    c.op(V, lambda e: e.tensor_tensor(out=comb, in0=t1, in1=sel, op=ALU.add), reads=["rt_t1", "rt_sel"], writes=["comb"])
    c.release(m)


def lay_gu(w, G):
    K, F = w.shape
    GW = G * 128
    return np.ascontiguousarray(w.reshape(8, 128, F // GW, GW).transpose(2, 1, 0, 3))


def lay_d(w, G):
    F, Dm = w.shape
    return np.ascontiguousarray(w.reshape(F // (G * 128), G, 128, Dm).transpose(0, 2, 1, 3))


POOL_W = (2, 4, 8, 16)


def pool_phase(c, halo_fill, cinfo, pw_d, pscale_d):
    ps = c.ps
    m = c.mark()
    hprev = c.alloc([D], F32)
    hnext = c.alloc([D], F32)
    Bp = c.alloc([4, 128], F32)
    Bm = c.alloc([4, 128], F32)
    Bn = c.alloc([4, 128], F32)
    pos = c.alloc([NT], F32)
    posi = c.alloc([NT], I32)
    cntt = [c.alloc([128], F32) for _ in range(2)]
    rct = [c.alloc([128], F32) for _ in range(2)]
    tmpt = [c.alloc([128], F32) for _ in range(2)]
    wst = c.alloc([4, 2, 256], F32)
    wpb = c.alloc([4, 2, 256], BF16)
    scb = c.alloc([D], F32)
    tB = [c.alloc([128], F32) for _ in range(2)]
    zb = [c.alloc([2, 128], BF16) for _ in range(2)]
    ybuf = [c.alloc([D], F32) for _ in range(3)]
    G_ = "gpsimd"
    V = "vector"
    halo_fill(hprev, hnext)
    c.op("sync", lambda e: e.dma_start(out=wst, in_=pw_d.rearrange("g p c j -> p g c j")), writes=["wst"], dkey="wst")
    c.op("sync", lambda e: e.dma_start(out=scb, in_=pscale_d.partition_broadcast(128)), writes=["scb"], dkey="scb")
    c.op(V, lambda e: e.tensor_copy(out=wpb, in_=wst), reads=["wst"], writes=["wpb"])
    for gi, w in enumerate(POOL_W):
        h = w // 2
        c.op(G_, lambda e, gi=gi: e.memset(Bp[:, gi, :], 1.0), writes=[("Bp", gi)])
        c.op(G_, lambda e, gi=gi, h=h: e.affine_select(out=Bp[:, gi, :], in_=Bp[:, gi, :], pattern=[[-1, 128]],
                                                       compare_op=ALU.is_ge, fill=0.0, base=-(128 - h), channel_multiplier=1),
             reads=[("Bp", gi)], writes=[("Bp", gi)])
        c.op(G_, lambda e, gi=gi: e.memset(Bn[:, gi, :], 1.0), writes=[("Bn", gi)])
        c.op(G_, lambda e, gi=gi, h=h: e.affine_select(out=Bn[:, gi, :], in_=Bn[:, gi, :], pattern=[[1, 128]],
                                                       compare_op=ALU.is_ge, fill=0.0, base=-(129 - h), channel_multiplier=-1),
             reads=[("Bn", gi)], writes=[("Bn", gi)])
        c.op(G_, lambda e, gi=gi: e.memset(Bm[:, gi, :], 1.0), writes=[("Bm", gi)])
        c.op(G_, lambda e, gi=gi, h=h: e.affine_select(out=Bm[:, gi, :], in_=Bm[:, gi, :], pattern=[[-1, 128]],
                                                       compare_op=ALU.is_ge, fill=0.0, base=h, channel_multiplier=1),
             reads=[("Bm", gi)], writes=[("Bm", gi)])
        c.op(G_, lambda e, gi=gi, h=h: e.affine_select(out=Bm[:, gi, :], in_=Bm[:, gi, :], pattern=[[1, 128]],
                                                       compare_op=ALU.is_ge, fill=0.0, base=h - 1, channel_multiplier=-1),
             reads=[("Bm", gi)], writes=[("Bm", gi)])
    c.op(G_, lambda e: e.iota(posi, pattern=[[1, NT]], base=0, channel_multiplier=0), writes=["posi"])
    c.op(V, lambda e: e.tensor_copy(out=pos, in_=posi), reads=["posi"], writes=["pos"])
    c.op(V, lambda e: e.tensor_scalar(out=pos, in0=pos, scalar1=cinfo[:, 0:1], scalar2=None, op0=ALU.add),
         reads=["pos", "cinfo"], writes=["pos"])
    def src_tile(j):
        if j < 0:
            return hprev, "hprev"
        if j > 15:
            return hnext, "hnext"
        return c.x_tm[:, j, :], ("x", j)

    def compute(j):
        yb = ybuf[j % 3]
        ky = ("ybuf", j % 3)
        for gi in range(4):
            k = (j * 4 + gi) % 2
            tok = slice(j * 128, (j + 1) * 128)
            h = POOL_W[gi] // 2
            c.op(V, lambda e, k=k, h=h, tok=tok: e.tensor_scalar(out=cntt[k], in0=pos[:, tok], scalar1=float(h), scalar2=float(SEQ), op0=ALU.add, op1=ALU.min),
                 reads=["pos"], writes=[("cntt", k)])
            c.op(V, lambda e, k=k, h=h, tok=tok: e.tensor_scalar(out=tmpt[k], in0=pos[:, tok], scalar1=float(-h), scalar2=0.0, op0=ALU.add, op1=ALU.max),
                 reads=["pos"], writes=[("tmpt", k)])
            c.op(V, lambda e, k=k: e.tensor_tensor(out=cntt[k], in0=cntt[k], in1=tmpt[k], op=ALU.subtract),
                 reads=[("tmpt", k), ("cntt", k)], writes=[("cntt", k)])
            c.op(V, lambda e, k=k: e.reciprocal(out=rct[k], in_=cntt[k]), reads=[("cntt", k)], writes=[("rct", k)])
            c.op(V, lambda e, k=k: e.scalar_tensor_tensor(out=tB[k], in0=cntt[k], scalar=-1.0, in1=c.ident, op0=ALU.mult, op1=ALU.mult),
                 reads=[("cntt", k), "ident"], writes=[("tB", k)])
            c.op(G_, lambda e, k=k, gi=gi: e.tensor_tensor(out=tB[k], in0=tB[k], in1=Bm[:, gi, :], op=ALU.add),
                 reads=[("tB", k), ("Bm", gi)], writes=[("tB", k)])
            pb = k
            for cc in range(2):
                fs = slice(gi * 256 + cc * 128, gi * 256 + (cc + 1) * 128)
                srcs = [(src_tile(j - 1), Bp[:, gi, :], ("Bp", gi)), (src_tile(j), tB[k], ("tB", k)), (src_tile(j + 1), Bn[:, gi, :], ("Bn", gi))]
                for si, ((sap, skey), bap, bkey) in enumerate(srcs):
                    c.op("tensor", lambda e, sap=sap, fs=fs, bap=bap, pb=pb, cc=cc, si=si: e.matmul(
                        ps[:, pb, cc * 128:(cc + 1) * 128], lhsT=sap[:, fs], rhs=bap, start=(si == 0), stop=(si == 2)),
                        reads=[skey, bkey], writes=[("ps", pb)])
            c.op(V, lambda e, k=k, pb=pb, gi=gi, tok=tok: e.tensor_tensor(
                out=zb[k], in0=ps[:, pb, 0:256].rearrange("p (c t) -> p c t", c=2),
                in1=rct[k].unsqueeze(1).to_broadcast([128, 2, 128]), op=ALU.mult),
                reads=[("ps", pb), ("rct", k)], writes=[("zb", k)])
            py = 2 + (j % 2) * 2 + gi // 2
            for cc in range(2):
                c.op("tensor", lambda e, k=k, cc=cc, gi=gi, py=py: e.matmul(
                    ps[:, py, (gi % 2) * 256:(gi % 2 + 1) * 256], lhsT=zb[k][:, cc, :], rhs=wpb[:, gi, cc, :],
                    start=(cc == 0), stop=(cc == 1)),
                    reads=[("zb", k), "wpb"], writes=[("ps", py)])
        pbase = 2 + (j % 2) * 2
        c.op(V, lambda e, yb=yb, pbase=pbase: e.tensor_tensor(
            out=yb, in0=ps[:, pbase:pbase + 2, :].rearrange("p b j -> p (b j)"), in1=scb, op=ALU.mult),
            reads=[("ps", pbase), ("ps", pbase + 1), "scb"], writes=[ky])

    def update(j):
        yb = ybuf[j % 3]
        ky = ("ybuf", j % 3)
        c.op(V, lambda e, yb=yb, j=j: e.scalar_tensor_tensor(out=c.x_tm[:, j, :], in0=c.x_tm[:, j, :], scalar=float(ALPHA), in1=yb, op0=ALU.mult, op1=ALU.add),
             reads=[ky, ("x", j)], writes=[("x", j)])

    compute(0)
    for j in range(1, 16):
        compute(j)
        update(j - 1)
    update(15)
    c.release(m)


MLA_H = 8
Q_RANK = 384
KV_RANK = 256
ATT_SCALE = (128 + 64) ** -0.5
TWO_PI = 2.0 * math.pi


def mla_phase(c, src, pos_keys, pos_own, wlq_d, wlkv_d, qng_d, kvg_d, wuq_d, wukv_d, wo_d):
    ps = c.ps
    V, G_, A, T = "vector", "gpsimd", "scalar", "tensor"
    m0 = c.mark()
    cos_own = c.alloc([NT], BF16, parts=64)
    ss_own = c.alloc([NT], BF16, parts=64)
    cqnT = c.alloc([3, NT], BF16)
    ckvnT = c.alloc([2, SEQ], BF16)
    krT = c.alloc([SEQ], BF16)
    kmr = c.alloc([16], F32)
    invf = c.alloc([1], F32, parts=64)
    ones_bf = c.alloc([128], BF16)
    qng = c.alloc([3], F32)
    kvg = c.alloc([2], F32)
    m1 = c.mark()
    wlq = c.alloc([8, 384], BF16)
    wlkv = c.alloc([8, 384], BF16)
    xst = c.alloc([8, 256], F32)
    wl_st = xst[:, :, 0:192]
    xob = c.alloc([8, 512], BF16)
    sqr0 = c.alloc([512], BF16, parts=64)
    sq = c.alloc([3, 512], F32)
    rstd = c.alloc([512], F32)
    posi = c.alloc([512], I32, parts=64)
    ang = c.alloc([512], F32, parts=64)
    ang2 = c.alloc([512], F32, parts=64)
    cosb = c.alloc([512], BF16, parts=64)
    ssb = c.alloc([512], BF16, parts=64)
    tmpr = c.alloc([512], F32, parts=64)
    frow = c.alloc([64], F32, parts=1)

    for i in range(64):
        val = float(np.float32(10000.0) ** np.float32(-(2 * (i % 32)) / 64.0))
        c.op(G_, lambda e, i=i, val=val: e.memset(frow[0:1, i:i + 1], val), writes=["frow"])
    c.op(T, lambda e: e.matmul(ps[0:64, 7, 0:1], lhsT=frow[0:1, :], rhs=c.ones[0:1, 0:1], start=True, stop=True),
         reads=["frow", "ones"], writes=[("ps", 7)])
    c.op(V, lambda e: e.tensor_copy(out=invf, in_=ps[0:64, 7, 0:1]), reads=[("ps", 7)], writes=["invf"])
    c.op(G_, lambda e: e.memset(ones_bf, 1.0), writes=["ones_bf"])
    c.op(G_, lambda e: e.memset(krT[64:65, :], 1.0), writes=["krT_ones"])
    c.op("sync", lambda e: e.dma_start(out=qng, in_=qng_d), writes=["qng"], dkey="qng")
    c.op("sync", lambda e: e.dma_start(out=kvg, in_=kvg_d), writes=["kvg"], dkey="kvg")
    for (wd_, wb_, wk_) in ((wlq_d, wlq, "wlq"), (wlkv_d, wlkv, "wlkv")):
        for hf in range(2):
            cs = slice(hf * 192, (hf + 1) * 192)
            c.op("sync", lambda e, wd_=wd_, cs=cs: e.dma_start(out=wl_st, in_=wd_[:, :, cs]), writes=["xst"], dkey="xst")
            c.op(G_, lambda e, wb_=wb_, cs=cs: e.tensor_copy(out=wb_[:, :, cs], in_=wl_st), reads=["xst"], writes=[wk_])

    def rms_norm(banks, nch, rank, gcol, dst, tok, tag, gkey):
        for cc in range(nch):
            c.op(A, lambda e, cc=cc: e.activation(out=sq[:, cc, :], in_=ps[:, banks[cc], :], func=AF.Square),
                 reads=[("ps", banks[cc])], writes=[("sq", cc)])
        for cc in range(nch):
            c.op(T, lambda e, cc=cc: e.matmul(ps[:, 7, :], lhsT=c.ones, rhs=sq[:, cc, :], start=(cc == 0), stop=(cc == nch - 1)),
                 reads=[("sq", cc), "ones"], writes=[("ps", 7)])
        c.op(A, lambda e: e.activation(out=rstd, in_=ps[:, 7, :], func=AF.Sqrt, bias=float(RMS_EPS), scale=1.0 / rank),
             reads=[("ps", 7)], writes=["rstd"])
        c.op(V, lambda e: e.reciprocal(out=rstd, in_=rstd), reads=["rstd"], writes=["rstd"])
        for cc in range(nch):
            c.op(V, lambda e, cc=cc: e.scalar_tensor_tensor(out=dst[:, cc, tok], in0=ps[:, banks[cc], :], scalar=gcol[:, cc:cc + 1],
                                                            in1=rstd, op0=ALU.mult, op1=ALU.mult),
                 reads=[("ps", banks[cc]), "rstd", gkey], writes=[tag])

    def rope_tables(pos_ap, cdst, sdst, ck, sk):
        c.op("sync", lambda e: e.dma_start(out=posi, in_=pos_ap.partition_broadcast(64)), writes=["posi"], dkey="posi")
        c.op(V, lambda e: e.tensor_copy(out=ang, in_=posi), reads=["posi"], writes=["ang"])
        c.op(V, lambda e: e.tensor_scalar(out=ang, in0=ang, scalar1=invf[:, 0:1], scalar2=None, op0=ALU.mult), reads=["ang", "invf"], writes=["ang"])
        c.op(V, lambda e: e.tensor_scalar(out=ang2, in0=ang, scalar1=float(0.5 * math.pi), scalar2=None, op0=ALU.add),
             reads=["ang"], writes=["ang2"])
        for (a_, ak) in ((ang, "ang"), (ang2, "ang2")):
            c.op(V, lambda e, a_=a_: e.tensor_scalar(out=tmpr, in0=a_, scalar1=float(1.0 / TWO_PI), scalar2=None, op0=ALU.mult), reads=[ak], writes=["tmpr"])
            c.op(V, lambda e: e.tensor_copy(out=posi, in_=tmpr), reads=["tmpr"], writes=["posi"])
            c.op(V, lambda e: e.tensor_copy(out=tmpr, in_=posi), reads=["posi"], writes=["tmpr"])
            c.op(V, lambda e, a_=a_: e.scalar_tensor_tensor(out=a_, in0=tmpr, scalar=float(-TWO_PI), in1=a_, op0=ALU.mult, op1=ALU.add),
                 reads=["tmpr", ak], writes=[ak])
            c.op(V, lambda e, a_=a_: e.tensor_scalar(out=tmpr, in0=a_, scalar1=float(math.pi), scalar2=float(-TWO_PI), op0=ALU.is_gt, op1=ALU.mult),
                 reads=[ak], writes=["tmpr"])
            c.op(V, lambda e, a_=a_: e.tensor_tensor(out=a_, in0=a_, in1=tmpr, op=ALU.add), reads=[ak, "tmpr"], writes=[ak])
        c.op(A, lambda e: e.activation(out=cdst, in_=ang2, func=AF.Sin), reads=["ang2"], writes=[ck])
        c.op(A, lambda e: e.activation(out=sdst, in_=ang, func=AF.Sin), reads=["ang"], writes=[sk])
        c.op(G_, lambda e: e.tensor_scalar(out=sdst[0:32, :], in0=sdst[0:32, :], scalar1=-1.0, scalar2=None, op0=ALU.mult), reads=[sk], writes=[sk])

    for i in range(8):
        tok = slice(i * 512, (i + 1) * 512)
        if src[0] == "f32":
            for hf in range(2):
                c.op("sync", lambda e, i=i, hf=hf: e.dma_start(out=xst, in_=src[1][:, :, i * 512 + hf * 256:i * 512 + (hf + 1) * 256]),
                     writes=["xst"], dkey="xst")
                c.op(G_, lambda e, hf=hf: e.tensor_copy(out=xob[:, :, hf * 256:(hf + 1) * 256], in_=xst), reads=["xst"], writes=["xob"])
        else:
            sap = src[1][i // 4]
            c.op("sync", lambda e, sap=sap, i=i: e.dma_start(out=xob, in_=sap[:, :, (i % 4) * 512:(i % 4 + 1) * 512]),
                 reads=[("ex_out", i // 4)], writes=["xob"], dkey="xob")
        rope_tables(pos_keys[tok], cosb, ssb, "cosb", "ssb")
        for cc in range(2):
            for kc in range(8):
                c.op(T, lambda e, cc=cc, kc=kc: e.matmul(ps[:, 3 + cc, :], lhsT=wlkv[:, kc, cc * 128:(cc + 1) * 128], rhs=xob[:, kc, :],
                                                         start=(kc == 0), stop=(kc == 7)),
                     reads=["wlkv", "xob"], writes=[("ps", 3 + cc)])
        for r in range(2):
            for kc in range(8):
                c.op(T, lambda e, r=r, kc=kc: e.matmul(ps[0:64, 5 + r, :], lhsT=wlkv[:, kc, 256 + r * 64:256 + (r + 1) * 64], rhs=xob[:, kc, :],
                                                       start=(kc == 0), stop=(kc == 7)),
                     reads=["wlkv", "xob"], writes=[("ps", 5 + r)])
        rms_norm([3, 4], 2, KV_RANK, kvg, ckvnT, tok, "ckvnT", "kvg")
        t1, t2 = ang, ang2
        c.op(V, lambda e: e.tensor_tensor(out=t1, in0=ps[0:64, 5, :], in1=cosb, op=ALU.mult), reads=[("ps", 5), "cosb"], writes=["ang"])
        c.op(V, lambda e: e.tensor_tensor(out=t2, in0=ps[0:64, 6, :], in1=ssb, op=ALU.mult), reads=[("ps", 6), "ssb"], writes=["ang2"])
        c.op(G_, lambda e: e.tensor_tensor(out=t1, in0=t1, in1=t2, op=ALU.add), reads=["ang", "ang2"], writes=["ang"])
        c.op(G_, lambda e, tok=tok: e.tensor_copy(out=krT[0:64, tok], in_=t1), reads=["ang"], writes=["krT"])
        c.op(A, lambda e: e.activation(out=sqr0, in_=t1, func=AF.Square), reads=["ang"], writes=["sqr0"])
        c.op(T, lambda e: e.matmul(ps[0:65, 7, :], lhsT=ones_bf[0:64, 0:65], rhs=sqr0, start=True, stop=True),
             reads=["sqr0", "ones_bf"], writes=[("ps", 7)])
        c.op(V, lambda e, i=i: e.tensor_reduce(out=kmr[64:65, i:i + 1], in_=ps[64:65, 7, :], axis=AX.X, op=ALU.max),
             reads=[("ps", 7)], writes=["kmr"])
    c.op(V, lambda e: e.tensor_reduce(out=kmr[64:65, 8:9], in_=kmr[64:65, 0:8], axis=AX.X, op=ALU.max), reads=["kmr"], writes=["kmr2"])
    for qt in range(4):
        tok = slice(qt * 512, (qt + 1) * 512)
        rope_tables(pos_own[tok], cos_own[:, tok], ss_own[:, tok], ("cos_own", qt), ("ss_own", qt))
        for cc in range(3):
            for kc in range(8):
                c.op(T, lambda e, cc=cc, kc=kc, tok=tok: e.matmul(ps[:, cc, :], lhsT=wlq[:, kc, cc * 128:(cc + 1) * 128], rhs=c.xbT[:, kc, tok],
                                                                  start=(kc == 0), stop=(kc == 7)),
                     reads=["wlq", ("xbT", qt)], writes=[("ps", cc)])
        rms_norm([0, 1, 2], 3, Q_RANK, qng, cqnT, tok, "cqnT", "qng")
    c.release(m1)

    wuq_st = c.alloc([3, 256], F32)
    wuq_b = c.alloc([3, 256], BF16)
    wukv_st = c.alloc([2, 256], F32)
    wukv_b = c.alloc([2, 256], BF16)
    wo_st = c.alloc([D], F32)
    wo_b = c.alloc([D], BF16)
    PT = [c.alloc([512], BF16) for _ in range(2)]
    onf = [c.alloc([128], F32) for _ in range(2)]
    rcp = c.alloc([4], F32)
    sqk = c.alloc([512], BF16)
    sqr = c.alloc([512], BF16, parts=64)
    qrf = c.alloc([512], F32, parts=64)
    q2 = c.alloc([512], F32, parts=64)
    kmx = c.alloc([16], F32)
    brow = c.alloc([512], F32)
    al = c.xbT.rearrange("p a b -> p (a b)")
    knT = al[:, 0:4096]
    Vaug = al[:, 4096:4096 + 32 * 129].rearrange("p (a b) -> p a b", a=32)
    o0 = 4096 + 32 * 129
    qnT = al[:, o0:o0 + 2048]
    qrT = al[:, o0 + 2048:o0 + 4096]
    oT = al[:, o0 + 4096:o0 + 6144]
    c.op(G_, lambda e: e.memset(Vaug[:, :, 128:129], 1.0), writes=["Vones"])

    for h in range(MLA_H):
        c.op("sync", lambda e, h=h: e.dma_start(out=wuq_st, in_=wuq_d[h]), writes=["wuq_st"], dkey="wuq_st")
        c.op("sync", lambda e, h=h: e.dma_start(out=wukv_st, in_=wukv_d[h]), writes=["wukv_st"], dkey="wukv_st")
        c.op("sync", lambda e, h=h: e.dma_start(out=wo_st, in_=wo_d[h]), writes=["wo_st"], dkey="wo_st")
        c.op(G_, lambda e: e.tensor_copy(out=wuq_b, in_=wuq_st), reads=["wuq_st"], writes=["wuq_b"])
        c.op(G_, lambda e: e.tensor_copy(out=wukv_b, in_=wukv_st), reads=["wukv_st"], writes=["wukv_b"])
        c.op(G_, lambda e: e.tensor_copy(out=wo_b, in_=wo_st), reads=["wo_st"], writes=["wo_b"])
        for i in range(8):
            tok = slice(i * 512, (i + 1) * 512)
            for kc in range(2):
                c.op(T, lambda e, kc=kc, tok=tok: e.matmul(ps[:, 6, :], lhsT=wukv_b[:, kc, 0:128], rhs=ckvnT[:, kc, tok], start=(kc == 0), stop=(kc == 1)),
                     reads=["wukv_b", "ckvnT"], writes=[("ps", 6)])
            c.op(A, lambda e, tok=tok: e.copy(out=knT[:, tok], in_=ps[:, 6, :]), reads=[("ps", 6)], writes=["knT"])
            c.op(A, lambda e: e.activation(out=sqk, in_=ps[:, 6, :], func=AF.Square), reads=[("ps", 6)], writes=["sqk"])
            c.op(T, lambda e: e.matmul(ps[0:65, 7, :], lhsT=ones_bf[:, 0:65], rhs=sqk, start=True, stop=True),
                 reads=["sqk", "ones_bf"], writes=[("ps", 7)])
            c.op(V, lambda e, i=i: e.tensor_reduce(out=kmx[64:65, i:i + 1], in_=ps[64:65, 7, :], axis=AX.X, op=ALU.max),
                 reads=[("ps", 7)], writes=["kmx"])
            for j4 in range(4):
                kch = i * 4 + j4
                ks = slice(kch * 128, (kch + 1) * 128)
                for kc in range(2):
                    c.op(T, lambda e, kc=kc, ks=ks, j4=j4: e.matmul(ps[:, 5, j4 * 128:(j4 + 1) * 128], lhsT=ckvnT[:, kc, ks], rhs=wukv_b[:, kc, 128:256],
                                                                    start=(kc == 0), stop=(kc == 1)),
                         reads=["wukv_b", "ckvnT"], writes=[("ps", 5)])
            c.op(V, lambda e, i=i: e.tensor_copy(out=Vaug[:, i * 4:(i + 1) * 4, 0:128], in_=ps[:, 5, :].rearrange("p (a b) -> p a b", a=4)),
                 reads=[("ps", 5)], writes=["Vaug"])
        c.op(V, lambda e: e.tensor_reduce(out=kmx[64:65, 9:10], in_=kmx[64:65, 0:8], axis=AX.X, op=ALU.max), reads=["kmx"], writes=["kmx1"])
        c.op(V, lambda e: e.tensor_tensor(out=kmx[64:65, 8:9], in0=kmx[64:65, 9:10], in1=kmr[64:65, 8:9], op=ALU.add), reads=["kmx1", "kmr2"], writes=["kmx2"])
        for qt in range(4):
            tok = slice(qt * 512, (qt + 1) * 512)
            for kc in range(3):
                c.op(T, lambda e, kc=kc, tok=tok: e.matmul(ps[:, 6, :], lhsT=wuq_b[:, kc, 0:128], rhs=cqnT[:, kc, tok], start=(kc == 0), stop=(kc == 2)),
                     reads=["wuq_b", "cqnT"], writes=[("ps", 6)])
            c.op(A, lambda e, tok=tok: e.copy(out=qnT[:, tok], in_=ps[:, 6, :]), reads=[("ps", 6)], writes=["qnT"])
            c.op(A, lambda e: e.activation(out=sqk, in_=ps[:, 6, :], func=AF.Square), reads=[("ps", 6)], writes=["sqk"])
            for r in range(2):
                for kc in range(3):
                    c.op(T, lambda e, kc=kc, tok=tok, r=r: e.matmul(ps[0:64, r, :], lhsT=wuq_b[:, kc, 128 + r * 64:192 + r * 64], rhs=cqnT[:, kc, tok],
                                                                    start=(kc == 0), stop=(kc == 2)),
                         reads=["wuq_b", "cqnT"], writes=[("ps", r)])
            c.op(V, lambda e, tok=tok: e.tensor_tensor(out=qrf, in0=ps[0:64, 0, :], in1=cos_own[:, tok], op=ALU.mult),
                 reads=[("ps", 0), ("cos_own", qt)], writes=["qrf"])
            c.op(V, lambda e, tok=tok: e.tensor_tensor(out=q2, in0=ps[0:64, 1, :], in1=ss_own[:, tok], op=ALU.mult),
                 reads=[("ps", 1), ("ss_own", qt)], writes=["q2"])
            c.op(G_, lambda e: e.tensor_tensor(out=qrf, in0=qrf, in1=q2, op=ALU.add), reads=["qrf", "q2"], writes=["qrf"])
            c.op(G_, lambda e, tok=tok: e.tensor_copy(out=qrT[0:64, tok], in_=qrf), reads=["qrf"], writes=["qrT"])
            c.op(A, lambda e: e.activation(out=sqr, in_=qrf, func=AF.Square), reads=["qrf"], writes=["sqr"])
            c.op(T, lambda e: e.matmul(ps[0:65, 7, :], lhsT=ones_bf[:, 0:65], rhs=sqk, start=True, stop=False),
                 reads=["sqk", "ones_bf"], writes=[("ps", 7)])
            c.op(T, lambda e: e.matmul(ps[0:65, 7, :], lhsT=ones_bf[0:64, 0:65], rhs=sqr, start=False, stop=True),
                 reads=["sqr", "ones_bf"], writes=[("ps", 7)])
            c.op(A, lambda e: e.activation(out=brow[64:65, :], in_=ps[64:65, 7, :], func=AF.Sqrt, scale=kmx[64:65, 8:9]),
                 reads=[("ps", 7), "kmx2"], writes=["brow"])
            c.op(V, lambda e, tok=tok: e.tensor_scalar(out=qrT[64:65, tok], in0=brow[64:65, :], scalar1=-1.0, scalar2=None, op0=ALU.mult),
                 reads=["brow"], writes=["qrT"])
        for qb in range(4):
            qs_ = slice(qb * 512, (qb + 1) * 512)
            def emit_scores(kc, qs_=qs_):
                sb_ = kc % 2
                ks = slice(kc * 128, (kc + 1) * 128)
                c.op(T, lambda e: e.matmul(ps[:, sb_, :], lhsT=knT[:, ks], rhs=qnT[:, qs_], start=True, stop=False),
                     reads=["knT", "qnT"], writes=[("ps", sb_)])
                c.op(T, lambda e: e.matmul(ps[:, sb_, :], lhsT=krT[0:65, ks], rhs=qrT[0:65, qs_], start=False, stop=True),
                     reads=["krT", "krT_ones", "qrT"], writes=[("ps", sb_)])

            emit_scores(0)
            for kc in range(32):
                sb_ = kc % 2
                if kc + 1 < 32:
                    emit_scores(kc + 1)
                c.op(A, lambda e, sb_=sb_: e.activation(out=PT[sb_], in_=ps[:, sb_, :], func=AF.Exp, scale=float(ATT_SCALE)),
                     reads=[("ps", sb_)], writes=[("PT", sb_)])
                for q4 in range(4):
                    c.op(T, lambda e, q4=q4, sb_=sb_, kc=kc: e.matmul(ps[:, 2 + q4, 0:129], lhsT=PT[sb_][:, q4 * 128:(q4 + 1) * 128], rhs=Vaug[:, kc, :],
                                                                      start=(kc == 0), stop=(kc == 31)),
                         reads=[("PT", sb_), "Vaug", "Vones"], writes=[("ps", 2 + q4)])
            for q4 in range(4):
                k2 = q4 % 2
                c.op(V, lambda e, q4=q4: e.reciprocal(out=rcp[:, q4:q4 + 1], in_=ps[:, 2 + q4, 128:129]), reads=[("ps", 2 + q4)], writes=[("rcp", q4)])
                c.op(V, lambda e, q4=q4, k2=k2: e.tensor_scalar(out=onf[k2], in0=ps[:, 2 + q4, 0:128], scalar1=rcp[:, q4:q4 + 1], scalar2=None, op0=ALU.mult),
                     reads=[("ps", 2 + q4), ("rcp", q4)], writes=[("onf", k2)])
                c.op(T, lambda e, q4=q4, k2=k2: e.transpose(out=ps[:, 6, q4 * 128:(q4 + 1) * 128], in_=onf[k2], identity=c.ident),
                     reads=[("onf", k2), "ident"], writes=[("ps", 6)])
            c.op(A, lambda e, qs_=qs_: e.copy(out=oT[:, qs_], in_=ps[:, 6, :]), reads=[("ps", 6)], writes=["oT"])
        for tile in range(16):
            for dh in range(2):
                c.op(T, lambda e, tile=tile, dh=dh: e.matmul(ps[:, 7, :], lhsT=oT[:, tile * 128:(tile + 1) * 128], rhs=wo_b[:, dh * 512:(dh + 1) * 512],
                                                             start=True, stop=True),
                     reads=["oT", "wo_b"], writes=[("ps", 7)])
                xs = c.x_tm[:, tile, dh * 512:(dh + 1) * 512]
                c.op(V, lambda e, xs=xs: e.tensor_tensor(out=xs, in0=ps[:, 7, :], in1=xs, op=ALU.add),
                     reads=[("ps", 7), ("x", tile)], writes=[("x", tile)])
    c.release(m0)


def lay_mla(w_dqkv, q_norm, w_uq, kv_norm, w_ukv, w_o):
    kc = lambda w: np.ascontiguousarray(w.reshape(-1, 128, w.shape[1]).transpose(1, 0, 2))
    wlq = kc(w_dqkv[:, :384])
    kr = w_dqkv[:, 640:704]
    kr_sw = np.concatenate([kr[:, 32:], kr[:, :32]], axis=1)
    wlkv = kc(np.concatenate([w_dqkv[:, 384:640], kr, kr_sw], axis=1))
    qng = np.ascontiguousarray(q_norm.reshape(3, 128).T)
    kvg = np.ascontiguousarray(kv_norm.reshape(2, 128).T)
    wuq = []
    for h in range(8):
        blk = w_uq[:, h * 192:(h + 1) * 192]
        r = blk[:, 128:]
        wuq.append(kc(np.concatenate([blk[:, :128], r, r[:, 32:], r[:, :32]], axis=1)))
    wukv = [kc(w_ukv[:, h * 256:(h + 1) * 256]) for h in range(8)]
    wo = np.ascontiguousarray(w_o.reshape(8, 128, 1024))
    return dict(wlq=wlq, wlkv=wlkv, qng=qng, kvg=kvg, wuq=np.stack(wuq), wukv=np.stack(wukv), wo=wo)


ML_H = 4
LN_KS = math.log(128 ** -0.5)
BIG = 1.0e30


def mlstm_phase(c, ex, cinfo, W):
    ps = c.ps
    V, G_, A, T = "vector", "gpsimd", "scalar", "tensor"
    m0 = c.mark()
    wgate = c.alloc([8, 16], BF16)
    bg = c.alloc([4], F32, parts=4)
    convw = c.alloc([8, 5], F32)
    id4 = c.ident[0:4, 0:4]
    rst = c.alloc([512], F32, parts=4)
    rstn = c.alloc([512], F32, parts=4)
    maskP = [c.alloc([128], F32) for _ in range(2)]
    colq = c.alloc([16, 2, 20], F32)
    cwB = c.alloc([2, 32, 4], F32)
    Cfin = c.alloc([2, 4, 257], F32)
    mfin = c.alloc([2], F32, parts=4)
    ones4 = c.alloc([128], F32, parts=4)
    st4 = c.alloc([8, 16], F32)
    c.op("sync", lambda e: e.dma_start(out=st4, in_=W["wgate"]), writes=["st4"], dkey="st4")
    c.op(G_, lambda e: e.tensor_copy(out=wgate, in_=st4), reads=["st4"], writes=["wgate"])
    c.op("sync", lambda e: e.dma_start(out=bg, in_=W["bgate"]), writes=["bg"], dkey="bg")
    c.op("sync", lambda e: e.dma_start(out=convw, in_=W["conv"]), writes=["convw"], dkey="convw")
    c.op(G_, lambda e: e.memset(ones4, 1.0), writes=["ones4"])
    c.op(G_, lambda e: e.memset(rst, 1.0), writes=["rst"])
    c.op(G_, lambda e: e.memset(rst.rearrange("p (a b) -> p a b", b=64)[:, :, 0:1], 0.0), reads=["rst"], writes=["rst"])
    c.op(G_, lambda e: e.memset(rstn, 0.0), writes=["rstn"])
    c.op(G_, lambda e: e.memset(rstn.rearrange("p (a b) -> p a b", b=64)[:, :, 0:1], -BIG), reads=["rstn"], writes=["rstn"])
    for d_ in range(2):
        mk = maskP[d_]
        c.op(G_, lambda e, mk=mk: e.memset(mk, BIG), writes=[("maskP", d_)])
        for hb in range(2):
            blk = mk[64 * hb:64 * hb + 64, 64 * hb:64 * hb + 64]
            c.op(G_, lambda e, blk=blk: e.memset(blk, 0.0), reads=[("maskP", d_)], writes=[("maskP", d_)])
            if d_ == 0:
                c.op(G_, lambda e, blk=blk: e.affine_select(out=blk, in_=blk, pattern=[[1, 64]], compare_op=ALU.is_ge, fill=BIG, base=0, channel_multiplier=-1),
                     reads=[("maskP", d_)], writes=[("maskP", d_)])
            else:
                c.op(G_, lambda e, blk=blk: e.affine_select(out=blk, in_=blk, pattern=[[-1, 64]], compare_op=ALU.is_ge, fill=BIG, base=0, channel_multiplier=1),
                     reads=[("maskP", d_)], writes=[("maskP", d_)])

    gm = c.mark()
    R = {n: c.alloc([512], F32, parts=4) for n in ("li", "xf", "t0", "t1", "b", "cc", "pm", "pm2", "aw")}
    mblk = c.alloc([16], F32, parts=4)
    uL = c.alloc([8], F32, parts=4)
    cwr = c.alloc([8], F32, parts=4)
    cwx = c.alloc([8, 4], F32, parts=4)
    awall = c.alloc([16, 4], F32)

    def v3(a):
        return a.rearrange("p (a b) -> p a b", b=64)

    def gate_rows(xb, xkey, d_, m_in, own, blk_i):
        for gsel, bank in ((d_, 6), (2 + d_, 7)):
            for kc in range(8):
                c.op(T, lambda e, kc=kc, gsel=gsel, bank=bank: e.matmul(ps[0:4, bank, :], lhsT=wgate[:, kc, 4 * gsel:4 * gsel + 4], rhs=xb[:, kc, :],
                                                                          start=(kc == 0), stop=(kc == 7)),
                     reads=["wgate", xkey], writes=[("ps", bank)])
        c.op(V, lambda e: e.tensor_scalar(out=R["li"], in0=ps[0:4, 6, :], scalar1=bg[:, d_:d_ + 1], scalar2=None, op0=ALU.add),
             reads=[("ps", 6), "bg"], writes=["r_li"])
        c.op(V, lambda e: e.tensor_scalar(out=R["xf"], in0=ps[0:4, 7, :], scalar1=bg[:, 2 + d_:3 + d_], scalar2=None, op0=ALU.add),
             reads=[("ps", 7), "bg"], writes=["r_xf"])
        c.op(V, lambda e: e.scalar_tensor_tensor(out=R["t0"], in0=R["xf"], scalar=-1.0, in1=R["xf"], op0=ALU.mult, op1=ALU.max), reads=["r_xf"], writes=["r_t0"])
        c.op(A, lambda e: e.activation(out=R["t0"], in_=R["t0"], func=AF.Exp, scale=-1.0), reads=["r_t0"], writes=["r_t0"])
        c.op(A, lambda e: e.activation(out=R["t0"], in_=R["t0"], func=AF.Ln, bias=1.0, scale=1.0), reads=["r_t0"], writes=["r_t0"])
        c.op(V, lambda e: e.tensor_scalar(out=R["t1"], in0=R["xf"], scalar1=0.0, scalar2=None, op0=ALU.min), reads=["r_xf"], writes=["r_t1"])
        c.op(V, lambda e: e.tensor_tensor(out=R["t1"], in0=R["t1"], in1=R["t0"], op=ALU.subtract), reads=["r_t1", "r_t0"], writes=["r_t1"])
        c.op(V, lambda e: e.tensor_tensor_scan(out=R["b"], data0=rst, data1=R["t1"], initial=0.0, op0=ALU.mult, op1=ALU.add),
             reads=["rst", "r_t1"], writes=["r_b"])
        if d_ == 1:
            c.op(V, lambda e: e.tensor_tensor(out=v3(R["t0"]), in0=v3(R["b"])[:, :, 63:64].to_broadcast([4, 8, 64]), in1=v3(R["b"]), op=ALU.subtract),
                 reads=["r_b"], writes=["r_t0"])
            c.op(V, lambda e: e.tensor_tensor(out=R["b"], in0=R["t0"], in1=R["t1"], op=ALU.add), reads=["r_t0", "r_t1", "r_b"], writes=["r_b"])
        c.op(V, lambda e: e.tensor_tensor(out=R["cc"], in0=R["li"], in1=R["b"], op=ALU.subtract), reads=["r_li", "r_b"], writes=["r_cc"])
        if d_ == 0:
            c.op(V, lambda e: e.tensor_tensor_scan(out=R["pm"], data0=rstn, data1=R["cc"], initial=-BIG, op0=ALU.add, op1=ALU.max),
                 reads=["rstn", "r_cc"], writes=["r_pm"])
        else:
            seq = [("cc", "pm"), ("pm", "pm2"), ("pm2", "pm"), ("pm", "pm2"), ("pm2", "pm"), ("pm", "pm2")]
            for k, (sn, dn) in zip((1, 2, 4, 8, 16, 32), seq):
                src, dst = R[sn], R[dn]
                c.op(V, lambda e, src=src, dst=dst, k=k: e.tensor_tensor(out=v3(dst)[:, :, 0:64 - k], in0=v3(src)[:, :, 0:64 - k], in1=v3(src)[:, :, k:64], op=ALU.max),
                     reads=["r_" + sn], writes=["r_" + dn])
                c.op(V, lambda e, src=src, dst=dst, k=k: e.tensor_copy(out=v3(dst)[:, :, 64 - k:64], in_=v3(src)[:, :, 64 - k:64]),
                     reads=["r_" + sn, "r_" + dn], writes=["r_" + dn])
            c.op(V, lambda e: e.tensor_copy(out=R["pm"], in_=R["pm2"]), reads=["r_pm2"], writes=["r_pm"])
        last = 63 if d_ == 0 else 0
        bL = v3(R["b"])[:, :, last]
        pmL = v3(R["pm"])[:, :, last]
        order = list(range(8)) if d_ == 0 else list(range(7, -1, -1))
        c.op(V, lambda e: e.tensor_copy(out=mblk[:, order[0]:order[0] + 1], in_=m_in), reads=["mcar", "r_b", "r_pm"], writes=["mblk"])
        for i, ch in enumerate(order):
            nxt = order[i + 1] if i < 7 else 8
            c.op(V, lambda e, ch=ch, nxt=nxt: e.scalar_tensor_tensor(out=mblk[:, nxt:nxt + 1], in0=mblk[:, ch:ch + 1], scalar=pmL[:, ch:ch + 1],
                                                                     in1=bL[:, ch:ch + 1], op0=ALU.max, op1=ALU.add),
                 reads=["mblk", "r_b", "r_pm"], writes=["mblk"])
        c.op(V, lambda e: e.tensor_tensor(out=uL, in0=mblk[:, 0:8], in1=pmL, op=ALU.max), reads=["mblk", "r_pm"], writes=["uL"])
        c.op(V, lambda e: e.tensor_tensor(out=cwr, in0=mblk[:, 0:8], in1=uL, op=ALU.subtract), reads=["mblk", "uL"], writes=["cwr"])
        c.op(A, lambda e: e.activation(out=cwr, in_=cwr, func=AF.Exp), reads=["cwr"], writes=["cwr"])
        c.op(V, lambda e: e.tensor_scalar(out=R["cc"], in0=R["cc"], scalar1=float(LN_KS), scalar2=None, op0=ALU.add), reads=["r_cc", "r_pm"], writes=["r_cc"])
        c.op(V, lambda e: e.tensor_tensor(out=v3(R["aw"]), in0=v3(R["cc"]), in1=uL.unsqueeze(2).to_broadcast([4, 8, 64]), op=ALU.subtract),
             reads=["r_cc", "uL"], writes=["r_aw"])
        c.op(A, lambda e: e.activation(out=R["aw"], in_=R["aw"], func=AF.Exp), reads=["r_aw"], writes=["r_aw"])
        c.op(V, lambda e: e.tensor_tensor(out=cwx, in0=cwr.unsqueeze(2).to_broadcast([4, 8, 4]), in1=id4.unsqueeze(1).to_broadcast([4, 8, 4]), op=ALU.mult),
             reads=["cwr", "ident"], writes=["cwx"])
        c.op(T, lambda e: e.matmul(ps[:, 6, 0:32], lhsT=ones4, rhs=cwx.rearrange("p a b -> p (a b)"), start=True, stop=True),
             reads=["ones4", "cwx"], writes=[("ps", 6)])
        c.op(V, lambda e: e.tensor_copy(out=cwB[:, d_, blk_i * 8:(blk_i + 1) * 8, :], in_=ps[:, 6, 0:32].rearrange("p (a b) -> p a b", b=4)),
             reads=[("ps", 6)], writes=[("cwB", d_)])
        if own:
            c.op(V, lambda e: e.tensor_tensor(out=v3(R["pm2"]), in0=v3(R["pm"]), in1=mblk[:, 0:8].unsqueeze(2).to_broadcast([4, 8, 64]), op=ALU.max),
                 reads=["r_pm", "mblk"], writes=["r_pm2"])
            c.op(V, lambda e: e.tensor_tensor(out=v3(R["t0"]), in0=mblk[:, 0:8].unsqueeze(2).to_broadcast([4, 8, 64]), in1=v3(R["pm2"]), op=ALU.subtract),
                 reads=["r_pm2", "mblk"], writes=["r_t0"])
            c.op(A, lambda e: e.activation(out=R["t0"], in_=R["t0"], func=AF.Exp), reads=["r_t0"], writes=["r_t0"])
            c.op(V, lambda e: e.tensor_tensor(out=R["t1"], in0=R["b"], in1=R["pm2"], op=ALU.add), reads=["r_b", "r_pm2"], writes=["r_t1"])
            c.op(A, lambda e: e.activation(out=R["t1"], in_=R["t1"], func=AF.Exp, scale=-1.0), reads=["r_t1"], writes=["r_t1"])
            qs = (("cc", "r_cc"), ("t0", "r_t0"), ("t1", "r_t1"), ("aw", "r_aw"), ("pm2", "r_pm2"))
        else:
            qs = (("aw", "r_aw"),)
        for t4 in range(4):
            tile = blk_i * 4 + t4
            ts = slice(t4 * 128, (t4 + 1) * 128)
            for qi, (rn, rk) in enumerate(qs):
                c.op(T, lambda e, rn=rn, ts=ts, qi=qi: e.matmul(ps[:, 5, qi * 4:(qi + 1) * 4], lhsT=R[rn][:, ts], rhs=id4, start=True, stop=True),
                     reads=[rk, "ident"], writes=[("ps", 5)])
            if own:
                c.op(V, lambda e, tile=tile: e.tensor_copy(out=colq[:, tile, d_, :], in_=ps[:, 5, 0:20]), reads=[("ps", 5)], writes=["colq"])
            else:
                c.op(V, lambda e, tile=tile: e.tensor_copy(out=awall[:, tile, :], in_=ps[:, 5, 0:4]), reads=[("ps", 5)], writes=["awall"])

    def conv_silu(pre, prekey, ci, n, dst_f, dkey):
        c.op(V, lambda e: e.tensor_scalar(out=dst_f, in0=pre[:, 0:n], scalar1=convw[:, ci, 0:1], scalar2=None, op0=ALU.mult),
             reads=[prekey, "convw"], writes=[dkey])
        for j in range(1, 5):
            c.op(V, lambda e, j=j: e.scalar_tensor_tensor(out=dst_f, in0=pre[:, j:n + j], scalar=convw[:, ci, j:j + 1], in1=dst_f, op0=ALU.mult, op1=ALU.add),
                 reads=[prekey, "convw", dkey], writes=[dkey])
        c.op(A, lambda e: e.activation(out=dst_f, in_=dst_f, func=AF.Silu), reads=[dkey], writes=[dkey])

    p1 = c.mark()
    xst_p1 = c.alloc([8, 171], F32)
    xob_p1 = c.alloc([8, NT + 4], BF16)
    wk_b_p1 = c.alloc([8, 128], BF16)
    wv_b_p1 = c.alloc([8, 256], BF16)
    pre_p1 = c.alloc([516], F32)
    kf_p1 = c.alloc([512], F32)
    ktm_p1 = c.alloc([4, 128], BF16)
    vau_p1 = c.alloc([4, 258], BF16)
    Cst_p1 = c.alloc([257], F32)
    zcol_p1 = c.alloc([1], F32, parts=4)
    c.op(G_, lambda e: e.memset(zcol_p1, 0.0), writes=["zcol"])
    c.op(G_, lambda e: e.memset(vau_p1[:, :, 256:257], 1.0), writes=["vau1"])
    wstg_p1 = xst_p1[:, :, 0:128]
    for d_ in range(2):
        if d_ == 0:
            c.op(G_, lambda e: e.memset(xob_p1[:, :, 0:2], 0.0), writes=["xob"])
            c.op("sync", lambda e: e.dma_start(out=xob_p1[:, :, 2:NT + 2], in_=ex[0]), reads=[("ex_out", 0)], writes=["xob"], dkey="xob")
            c.op("sync", lambda e: e.dma_start(out=xob_p1[:, :, NT + 2:NT + 4], in_=ex[1][:, :, 0:2]), reads=[("ex_out", 1)], writes=["xob"], dkey="xob")
        else:
            c.op(G_, lambda e: e.memset(xob_p1[:, :, NT + 2:NT + 4], 0.0), writes=["xob"])
            c.op("sync", lambda e: e.dma_start(out=xob_p1[:, :, 2:NT + 2], in_=ex[1]), reads=[("ex_out", 1)], writes=["xob"], dkey="xob")
            c.op("sync", lambda e: e.dma_start(out=xob_p1[:, :, 0:2], in_=ex[0][:, :, NT - 2:NT]), reads=[("ex_out", 0)], writes=["xob"], dkey="xob")
        c.op(V, lambda e, d_=d_: e.tensor_copy(out=mfin[:, d_:d_ + 1], in_=zcol_p1), reads=["zcol"], writes=["mcar"])
        blocks = list(range(4)) if d_ == 0 else list(range(3, -1, -1))
        for bi in blocks:
            gate_rows(xob_p1[:, :, 2 + bi * 512:2 + (bi + 1) * 512], "xob", d_, mfin[:, d_:d_ + 1], False, bi)
            c.op(V, lambda e, d_=d_: e.tensor_copy(out=mfin[:, d_:d_ + 1], in_=mblk[:, 8:9]), reads=["mblk"], writes=["mcar"])
        for h in range(ML_H):
            c.op("sync", lambda e, h=h: e.dma_start(out=wstg_p1, in_=W["wk"][h]), writes=["xst"], dkey="xst")
            c.op(G_, lambda e: e.tensor_copy(out=wk_b_p1, in_=wstg_p1), reads=["xst"], writes=["wk_b"])
            for hf in range(2):
                c.op("sync", lambda e, h=h, hf=hf: e.dma_start(out=wstg_p1, in_=W["wv"][h, :, :, hf * 128:(hf + 1) * 128]), writes=["xst"], dkey="xst")
                c.op(G_, lambda e, hf=hf: e.tensor_copy(out=wv_b_p1[:, :, hf * 128:(hf + 1) * 128], in_=wstg_p1), reads=["xst"], writes=["wv_b"])
            c.op(G_, lambda e: e.memset(Cst_p1, 0.0), writes=["Cst"])
            for bi in blocks:
                x0 = bi * 512
                for (bank, c0, n) in ((0, 0, 512), (1, 512, 4)):
                    for kc in range(8):
                        c.op(T, lambda e, kc=kc, bank=bank, c0=c0, n=n, x0=x0: e.matmul(ps[:, bank, 0:n], lhsT=wk_b_p1[:, kc, :], rhs=xob_p1[:, kc, x0 + c0:x0 + c0 + n],
                                                                                      start=(kc == 0), stop=(kc == 7)),
                             reads=["wk_b", "xob"], writes=[("ps", bank)])
                    c.op(A, lambda e, bank=bank, c0=c0, n=n: e.copy(out=pre_p1[:, c0:c0 + n], in_=ps[:, bank, 0:n]), reads=[("ps", bank)], writes=["pre"])
                conv_silu(pre_p1, "pre", 4 + h, 512, kf_p1, "kf")
                for t4 in range(4):
                    c.op(T, lambda e, t4=t4: e.transpose(out=ps[:, 2, t4 * 128:(t4 + 1) * 128], in_=kf_p1[:, t4 * 128:(t4 + 1) * 128], identity=c.ident),
                         reads=["kf", "ident"], writes=[("ps", 2)])
                c.op(V, lambda e, h=h, bi=bi: e.tensor_tensor(out=ktm_p1, in0=ps[:, 2, :].rearrange("p (a b) -> p a b", b=128),
                                                              in1=awall[:, bi * 4:(bi + 1) * 4, h:h + 1].to_broadcast([128, 4, 128]), op=ALU.mult),
                     reads=[("ps", 2), "awall"], writes=["ktm"])
                for t4 in range(4):
                    for kc in range(8):
                        c.op(T, lambda e, kc=kc, t4=t4, x0=x0: e.matmul(ps[:, 3, 0:256], lhsT=xob_p1[:, kc, 2 + x0 + t4 * 128:2 + x0 + (t4 + 1) * 128], rhs=wv_b_p1[:, kc, :],
                                                                        start=(kc == 0), stop=(kc == 7)),
                             reads=["wv_b", "xob"], writes=[("ps", 3)])
                    c.op(A, lambda e, t4=t4: e.copy(out=vau_p1[:, t4, 0:256], in_=ps[:, 3, 0:256]), reads=[("ps", 3)], writes=["vau"])
                chunks = list(range(8)) if d_ == 0 else list(range(7, -1, -1))
                for ch in chunks:
                    t4, hb = ch // 2, ch % 2
                    rs = slice(64 * hb, 64 * hb + 64)
                    c.op(T, lambda e, t4=t4, rs=rs: e.matmul(ps[:, 4, 0:257], lhsT=ktm_p1[rs, t4, :], rhs=vau_p1[rs, t4, 0:257], start=True, stop=True),
                         reads=["ktm", "vau", "vau1"], writes=[("ps", 4)])
                    c.op(V, lambda e, h=h, ch=ch, bi=bi, d_=d_: e.scalar_tensor_tensor(out=Cst_p1, in0=Cst_p1, scalar=cwB[:, d_, bi * 8 + ch, h:h + 1],
                                                                                      in1=ps[:, 4, 0:257], op0=ALU.mult, op1=ALU.add),
                         reads=["Cst", ("cwB", d_), ("ps", 4)], writes=["Cst"])
            c.op(V, lambda e, d_=d_, h=h: e.tensor_scalar(out=Cfin[:, d_, h, :], in0=Cst_p1, scalar1=cinfo[:, 1 + d_:2 + d_], scalar2=None, op0=ALU.mult),
                 reads=["Cst", "cinfo"], writes=[("Cfin", d_)])
        c.op(V, lambda e, d_=d_: e.tensor_scalar(out=mfin[:, d_:d_ + 1], in0=mfin[:, d_:d_ + 1], scalar1=cinfo[0:4, 1 + d_:2 + d_], scalar2=None, op0=ALU.mult),
             reads=["mcar", "cinfo"], writes=["mcar"])
    c.release(p1)

    mcar2 = c.alloc([2], F32, parts=4)
    for d_ in range(2):
        c.op(V, lambda e, d_=d_: e.tensor_copy(out=mcar2[:, d_:d_ + 1], in_=mfin[:, d_:d_ + 1]), reads=["mcar"], writes=["mcar"])
        blocks = list(range(4)) if d_ == 0 else list(range(3, -1, -1))
        for bi in blocks:
            gate_rows(c.xbT[:, :, bi * 512:(bi + 1) * 512], ("xbT", bi), d_, mcar2[:, d_:d_ + 1], True, bi)
            c.op(V, lambda e, d_=d_: e.tensor_copy(out=mcar2[:, d_:d_ + 1], in_=mblk[:, 8:9]), reads=["mblk"], writes=["mcar"])
    c.release(gm)

    wst = c.alloc([8, 128], F32)
    wq_b = c.alloc([8, 128], BF16)
    wk_b = c.alloc([8, 128], BF16)
    wv_b = c.alloc([8, 256], BF16)
    wout_b = c.alloc([2, D], BF16)
    xh_b = c.alloc([8, 4], BF16)
    pre = c.alloc([516], F32)
    kf = c.alloc([512], F32)
    qT = c.alloc([NT], BF16)
    kT = c.alloc([NT], BF16)
    ktm = c.alloc([16, 128], BF16)
    kaw = c.alloc([128], BF16)
    vau = c.alloc([16, 258], BF16)
    hacc = c.alloc([16, 256], F32)
    dg = [c.alloc([128], F32) for _ in range(2)]
    Ub = [c.alloc([128], F32) for _ in range(2)]
    DwT = [c.alloc([128], F32) for _ in range(2)]
    SDT = [c.alloc([128], BF16) for _ in range(2)]
    hB = [c.alloc([257], F32) for _ in range(2)]
    hN = c.alloc([257], F32)
    Cf = c.alloc([257], F32)
    Cb = [c.alloc([258], BF16) for _ in range(2)]
    dcol = c.alloc([4], F32)
    ng = c.alloc([256], F32)
    ysb = c.alloc([256], F32)
    og = c.alloc([256], F32)
    yT = c.alloc([2, 128], BF16)
    c.op("sync", lambda e: e.dma_start(out=xh_b[:, :, 0:2], in_=ex[0][:, :, NT - 2:NT]), reads=[("ex_out", 0)], writes=["xh_b"], dkey="xh_b")
    c.op("sync", lambda e: e.dma_start(out=xh_b[:, :, 2:4], in_=ex[1][:, :, 0:2]), reads=[("ex_out", 1)], writes=["xh_b"], dkey="xh_b")
    c.op(G_, lambda e: e.tensor_scalar(out=xh_b[:, :, 0:2], in0=xh_b[:, :, 0:2], scalar1=cinfo[:, 1:2], scalar2=None, op0=ALU.mult), reads=["xh_b", "cinfo"], writes=["xh_b"])
    c.op(G_, lambda e: e.tensor_scalar(out=xh_b[:, :, 2:4], in0=xh_b[:, :, 2:4], scalar1=cinfo[:, 2:3], scalar2=None, op0=ALU.mult), reads=["xh_b", "cinfo"], writes=["xh_b"])
    c.op(G_, lambda e: e.memset(vau[:, :, 256:257], 1.0), writes=["vau1"])

    def load_w(src, dst, n, key):
        for hf in range(n // 128):
            c.op("sync", lambda e, hf=hf: e.dma_start(out=wst, in_=src[:, :, hf * 128:(hf + 1) * 128]), writes=["wst"], dkey="wst")
            c.op(G_, lambda e, hf=hf: e.tensor_copy(out=dst[:, :, hf * 128:(hf + 1) * 128], in_=wst), reads=["wst"], writes=[key])

    def proj_fm(wb, wkey, ci, dst_bf, dkey, tm):
        for qt in range(4):
            lo = qt * 512 - 2
            for kc in range(8):
                c.op(T, lambda e, kc=kc, qt=qt: e.matmul(ps[:, 0, :], lhsT=wb[:, kc, :], rhs=c.xbT[:, kc, qt * 512:(qt + 1) * 512], start=(kc == 0), stop=(kc == 7)),
                     reads=[wkey, ("xbT", qt)], writes=[("ps", 0)])
            c.op(A, lambda e: e.copy(out=pre[:, 2:514], in_=ps[:, 0, :]), reads=[("ps", 0)], writes=["pre"])
            for side, (pc, tok0) in enumerate(((0, lo), (514, lo + 514))):
                if tok0 < 0 or tok0 >= NT:
                    rhs = xh_b[:, :, 0:2] if tok0 < 0 else xh_b[:, :, 2:4]
                    rk = "xh_b"
                else:
                    rhs = c.xbT[:, :, tok0:tok0 + 2]
                    rk = ("xbT", tok0 // 512)
                for kc in range(8):
                    c.op(T, lambda e, kc=kc, rhs=rhs, side=side: e.matmul(ps[:, 1, side * 2:side * 2 + 2], lhsT=wb[:, kc, :], rhs=rhs[:, kc, :], start=(kc == 0), stop=(kc == 7)),
                         reads=[wkey, rk], writes=[("ps", 1)])
            c.op(A, lambda e: e.copy(out=pre[:, 0:2], in_=ps[:, 1, 0:2]), reads=[("ps", 1)], writes=["pre"])
            c.op(A, lambda e: e.copy(out=pre[:, 514:516], in_=ps[:, 1, 2:4]), reads=[("ps", 1)], writes=["pre"])
            conv_silu(pre, "pre", ci, 512, kf, "kf")
            c.op(G_, lambda e, qt=qt: e.tensor_copy(out=dst_bf[:, qt * 512:(qt + 1) * 512], in_=kf), reads=["kf"], writes=[dkey])
            if tm:
                for t4 in range(4):
                    c.op(T, lambda e, t4=t4: e.transpose(out=ps[:, 2, t4 * 128:(t4 + 1) * 128], in_=kf[:, t4 * 128:(t4 + 1) * 128], identity=c.ident),
                         reads=["kf", "ident"], writes=[("ps", 2)])
                c.op(A, lambda e, qt=qt: e.copy(out=ktm[:, qt * 4:(qt + 1) * 4, :], in_=ps[:, 2, :].rearrange("p (a b) -> p a b", b=128)),
                     reads=[("ps", 2)], writes=["ktm"])

    for h in range(ML_H):
        load_w(W["wq"][h], wq_b, 128, "wq")
        load_w(W["wk"][h], wk_b, 128, "wk")
        load_w(W["wv"][h], wv_b, 256, "wv")
        for cc_ in range(2):
            for q8 in range(8):
                c.op("sync", lambda e, h=h, cc_=cc_, q8=q8: e.dma_start(out=wst[:, 0, :], in_=W["wout"][h, :, cc_, q8 * 128:(q8 + 1) * 128]), writes=["wst"], dkey="wst")
                c.op(G_, lambda e, cc_=cc_, q8=q8: e.tensor_copy(out=wout_b[:, cc_, q8 * 128:(q8 + 1) * 128], in_=wst[:, 0, :]), reads=["wst"], writes=["wout"])
        c.op("sync", lambda e, h=h: e.dma_start(out=ng, in_=W["normg"][h * 256:(h + 1) * 256].partition_broadcast(128)), writes=["ng"], dkey="ng")
        proj_fm(wq_b, "wq", h, qT, "qT", False)
        proj_fm(wk_b, "wk", 4 + h, kT, "kT", True)
        for t4 in range(16):
            ts = slice(t4 * 128, (t4 + 1) * 128)
            for kc in range(8):
                c.op(T, lambda e, kc=kc, ts=ts: e.matmul(ps[:, 3, 0:256], lhsT=c.xbT[:, kc, ts], rhs=wv_b[:, kc, :], start=(kc == 0), stop=(kc == 7)),
                     reads=["wv", ("xbT", t4 // 4)], writes=[("ps", 3)])
            c.op(A, lambda e, t4=t4: e.copy(out=vau[:, t4, 0:256], in_=ps[:, 3, 0:256]), reads=[("ps", 3)], writes=["vau"])
        load_w(W["wog"][h], wv_b, 256, "wv")
        for d_ in range(2):
            c.op(V, lambda e, d_=d_, h=h: e.tensor_copy(out=Cf, in_=Cfin[:, d_, h, :]), reads=[("Cfin", d_)], writes=["Cf"])
            c.op(A, lambda e: e.copy(out=Cb[0][:, 0:257], in_=Cf), reads=["Cf"], writes=[("Cb", 0)])
            tiles = list(range(16)) if d_ == 0 else list(range(15, -1, -1))
            cbi = 0
            for ti, tile in enumerate(tiles):
                ts = slice(tile * 128, (tile + 1) * 128)
                k2 = ti % 2
                ccol = lambda qi, tile=tile, d_=d_, h=h: colq[:, tile, d_, qi * 4 + h:qi * 4 + h + 1]
                c.op(V, lambda e, k2=k2, ccol=ccol: e.tensor_scalar(out=dg[k2], in0=c.ident, scalar1=ccol(4), scalar2=None, op0=ALU.mult),
                     reads=["ident", "colq"], writes=[("dg", k2)])
                c.op(T, lambda e, k2=k2: e.matmul(ps[:, 4 + k2, 0:128], lhsT=c.ones, rhs=dg[k2], start=True, stop=True),
                     reads=["ones", ("dg", k2)], writes=[("ps", 4 + k2)])
                c.op(V, lambda e, k2=k2, d_=d_: e.tensor_tensor(out=Ub[k2], in0=ps[:, 4 + k2, 0:128], in1=maskP[d_], op=ALU.add),
                     reads=[("ps", 4 + k2), ("maskP", d_)], writes=[("Ub", k2)])
                c.op(T, lambda e, ts=ts, k2=k2: e.matmul(ps[:, k2, 0:128], lhsT=kT[:, ts], rhs=qT[:, ts], start=True, stop=True),
                     reads=["kT", "qT"], writes=[("ps", k2)])
                c.op(A, lambda e, k2=k2, ccol=ccol: e.activation(out=DwT[k2], in_=Ub[k2], func=AF.Exp, scale=-1.0, bias=ccol(0)),
                     reads=[("Ub", k2), "colq"], writes=[("DwT", k2)])
                c.op(V, lambda e, k2=k2: e.tensor_tensor(out=SDT[k2], in0=ps[:, k2, 0:128], in1=DwT[k2], op=ALU.mult),
                     reads=[("ps", k2), ("DwT", k2)], writes=[("SDT", k2)])
                c.op(T, lambda e, k2=k2, tile=tile: e.matmul(ps[:, 2 + k2, 0:257], lhsT=SDT[k2], rhs=vau[:, tile, 0:257], start=True, stop=True),
                     reads=[("SDT", k2), "vau", "vau1"], writes=[("ps", 2 + k2)])
                c.op(A, lambda e, k2=k2: e.copy(out=hB[k2], in_=ps[:, 2 + k2, 0:257]), reads=[("ps", 2 + k2)], writes=[("hB", k2)])
                c.op(V, lambda e, tile=tile, ccol=ccol: e.tensor_scalar(out=kaw, in0=ktm[:, tile, :], scalar1=ccol(3), scalar2=None, op0=ALU.mult),
                     reads=["ktm", "colq"], writes=["kaw"])
                for hb in ((0, 1) if d_ == 0 else (1, 0)):
                    rs = slice(64 * hb, 64 * hb + 64)
                    ch = tile * 2 + hb
                    cur = Cb[cbi % 2]
                    c.op(T, lambda e, ts=ts, cur=cur: e.matmul(ps[:, 6, 0:257], lhsT=qT[:, ts], rhs=cur[:, 0:257], start=True, stop=True),
                         reads=["qT", ("Cb", cbi % 2)], writes=[("ps", 6)])
                    c.op(V, lambda e, rs=rs, k2=k2, ccol=ccol: e.scalar_tensor_tensor(out=hN[rs, :], in0=ps[rs, 6, 0:257], scalar=ccol(1)[rs, :], in1=hB[k2][rs, :],
                                                                                     op0=ALU.mult, op1=ALU.add),
                         reads=[("ps", 6), "colq", ("hB", k2)], writes=["hN"])
                    c.op(T, lambda e, rs=rs, tile=tile: e.matmul(ps[:, 7, 0:257], lhsT=kaw[rs, :], rhs=vau[rs, tile, 0:257], start=True, stop=True),
                         reads=["kaw", "vau", "vau1"], writes=[("ps", 7)])
                    c.op(V, lambda e, ch=ch, d_=d_, h=h: e.scalar_tensor_tensor(out=Cf, in0=Cf, scalar=cwB[:, d_, ch, h:h + 1], in1=ps[:, 7, 0:257], op0=ALU.mult, op1=ALU.add),
                         reads=["Cf", ("cwB", d_), ("ps", 7)], writes=["Cf"])
                    cbi += 1
                    nxt = Cb[cbi % 2]
                    c.op(A, lambda e, nxt=nxt: e.copy(out=nxt[:, 0:257], in_=Cf), reads=["Cf"], writes=[("Cb", cbi % 2)])
                c.op(V, lambda e: e.scalar_tensor_tensor(out=dcol[:, 0:1], in0=hN[:, 256:257], scalar=-1.0, in1=hN[:, 256:257], op0=ALU.mult, op1=ALU.max), reads=["hN"], writes=["dcol"])
                c.op(V, lambda e, ccol=ccol: e.tensor_tensor(out=dcol[:, 0:1], in0=dcol[:, 0:1], in1=ccol(2), op=ALU.max), reads=["dcol", "colq"], writes=["dcol"])
                c.op(V, lambda e: e.reciprocal(out=dcol[:, 1:2], in_=dcol[:, 0:1]), reads=["dcol"], writes=["dcol"])
                if d_ == 0:
                    c.op(V, lambda e, tile=tile: e.tensor_scalar(out=hacc[:, tile, :], in0=hN[:, 0:256], scalar1=dcol[:, 1:2], scalar2=None, op0=ALU.mult),
                         reads=["hN", "dcol"], writes=[("hacc", tile)])
                else:
                    c.op(V, lambda e, tile=tile: e.scalar_tensor_tensor(out=hacc[:, tile, :], in0=hN[:, 0:256], scalar=dcol[:, 1:2], in1=hacc[:, tile, :], op0=ALU.mult, op1=ALU.add),
                         reads=["hN", "dcol", ("hacc", tile)], writes=[("hacc", tile)])
        for tile in range(16):
            ha = hacc[:, tile, :]
            hk = ("hacc", tile)
            ts = slice(tile * 128, (tile + 1) * 128)
            for kc in range(8):
                c.op(T, lambda e, kc=kc, ts=ts: e.matmul(ps[:, 4, 0:256], lhsT=c.xbT[:, kc, ts], rhs=wv_b[:, kc, :], start=(kc == 0), stop=(kc == 7)),
                     reads=["wv", ("xbT", tile // 4)], writes=[("ps", 4)])
            c.op(A, lambda e: e.activation(out=og, in_=ps[:, 4, 0:256], func=AF.Sigmoid), reads=[("ps", 4)], writes=["og"])
            c.op(A, lambda e, ha=ha: e.activation(out=ysb, in_=ha, func=AF.Identity, accum_out=dcol[:, 0:1]), reads=[hk, "dcol"], writes=["ysb", "dcol"])
            c.op(A, lambda e, ha=ha: e.activation(out=ysb, in_=ha, func=AF.Square, accum_out=dcol[:, 1:2]), reads=[hk, "dcol"], writes=["ysb", "dcol"])
            c.op(V, lambda e: e.tensor_scalar(out=dcol[:, 0:2], in0=dcol[:, 0:2], scalar1=1.0 / 256, scalar2=None, op0=ALU.mult), reads=["dcol"], writes=["dcol"])
            c.op(V, lambda e: e.tensor_tensor(out=dcol[:, 2:3], in0=dcol[:, 0:1], in1=dcol[:, 0:1], op=ALU.mult), reads=["dcol"], writes=["dcol"])
            c.op(V, lambda e: e.tensor_tensor(out=dcol[:, 2:3], in0=dcol[:, 1:2], in1=dcol[:, 2:3], op=ALU.subtract), reads=["dcol"], writes=["dcol"])
            c.op(A, lambda e: e.activation(out=dcol[:, 2:3], in_=dcol[:, 2:3], func=AF.Sqrt, bias=float(LN_EPS), scale=1.0), reads=["dcol"], writes=["dcol"])
            c.op(V, lambda e: e.reciprocal(out=dcol[:, 2:3], in_=dcol[:, 2:3]), reads=["dcol"], writes=["dcol"])
            c.op(V, lambda e: e.scalar_tensor_tensor(out=dcol[:, 3:4], in0=dcol[:, 0:1], scalar=-1.0, in1=dcol[:, 2:3], op0=ALU.mult, op1=ALU.mult), reads=["dcol"], writes=["dcol"])
            c.op(A, lambda e, ha=ha: e.activation(out=ysb, in_=ha, func=AF.Identity, scale=dcol[:, 2:3], bias=dcol[:, 3:4]), reads=[hk, "dcol"], writes=["ysb"])
            c.op(V, lambda e: e.tensor_tensor(out=ysb, in0=ysb, in1=ng, op=ALU.mult), reads=["ysb", "ng"], writes=["ysb"])
            c.op(V, lambda e: e.tensor_tensor(out=ysb, in0=ysb, in1=og, op=ALU.mult), reads=["ysb", "og"], writes=["ysb"])
            for cc_ in range(2):
                c.op(T, lambda e, cc_=cc_: e.transpose(out=ps[:, 0, cc_ * 128:(cc_ + 1) * 128], in_=ysb[:, cc_ * 128:(cc_ + 1) * 128], identity=c.ident),
                     reads=["ysb", "ident"], writes=[("ps", 0)])
            c.op(A, lambda e: e.copy(out=yT, in_=ps[:, 0, 0:256].rearrange("p (a b) -> p a b", b=128)), reads=[("ps", 0)], writes=["yT"])
            for dh in range(2):
                for cc_ in range(2):
                    c.op(T, lambda e, cc_=cc_, dh=dh: e.matmul(ps[:, 1, :], lhsT=yT[:, cc_, :], rhs=wout_b[:, cc_, dh * 512:(dh + 1) * 512], start=(cc_ == 0), stop=(cc_ == 1)),
                         reads=["yT", "wout"], writes=[("ps", 1)])
                xs = c.x_tm[:, tile, dh * 512:(dh + 1) * 512]
                c.op(V, lambda e, xs=xs: e.tensor_tensor(out=xs, in0=ps[:, 1, :], in1=xs, op=ALU.add), reads=[("ps", 1), ("x", tile)], writes=[("x", tile)])
    c.release(m0)


def lay_mlstm(w_in, conv, b_gate, norm_g, w_out):
    kc = lambda w: np.ascontiguousarray(w.reshape(-1, 128, w.shape[1]).transpose(1, 0, 2))
    wq = np.stack([kc(w_in[:, h * 128:(h + 1) * 128]) for h in range(4)])
    wk = np.stack([kc(w_in[:, 512 + h * 128:512 + (h + 1) * 128]) for h in range(4)])
    wv = np.stack([kc(w_in[:, 1024 + h * 256:1024 + (h + 1) * 256]) for h in range(4)])
    wog = np.stack([kc(w_in[:, 2048 + h * 256:2048 + (h + 1) * 256]) for h in range(4)])
    wgate = kc(w_in[:, 3072:3088])
    bgate = np.ascontiguousarray(b_gate.reshape(4, 4).T)
    convl = np.ascontiguousarray(conv.T.reshape(8, 128, 5).transpose(1, 0, 2))
    wout = np.ascontiguousarray(w_out.reshape(4, 2, 128, 1024).transpose(0, 2, 1, 3))
    return dict(wq=wq, wk=wk, wv=wv, wog=wog, wgate=wgate, bgate=bgate, conv=convl, normg=np.ascontiguousarray(norm_g), wout=wout)


G_DENSE = 2
G_MOE = 2


def _fm(a):
    return np.ascontiguousarray(a.T.reshape(8, 128, a.shape[0]).transpose(1, 0, 2))


def layer_weights(i, inp):
    w = {}
    mix = i % 3
    if mix == 0:
        j = i // 3
        L = lay_mla(inp["mla_w_dqkv"][j], inp["mla_q_norm"][j], inp["mla_w_uq"][j], inp["mla_kv_norm"][j], inp["mla_w_ukv"][j], inp["mla_w_o"][j])
        w.update({"mla_" + k: v for k, v in L.items()})
    elif mix == 1:
        j = i // 3
        w["pool_w"] = np.ascontiguousarray(inp["pool_w"][j].reshape(4, 2, 128, 256).transpose(0, 2, 1, 3))
        w["pool_scale"] = np.ascontiguousarray(inp["pool_scale"][j])
    else:
        j = i // 3
        L = lay_mlstm(inp["mlstm_w_in"][j], inp["mlstm_conv"][j], inp["mlstm_b_gate"][j], inp["mlstm_norm"][j], inp["mlstm_w_out"][j])
        w.update({"ml_" + k: v for k, v in L.items()})
    cidx = i // 2
    if i % 2 == 0:
        w["wg"] = lay_gu(inp["ffn_w_gate"][cidx], G_DENSE)[None]
        w["wu"] = lay_gu(inp["ffn_w_up"][cidx], G_DENSE)[None]
        w["wd"] = lay_d(inp["ffn_w_down"][cidx], G_DENSE)[None]
    else:
        w["wg"] = np.stack([lay_gu(inp["moe_w_gate"][cidx, e], G_MOE) for e in range(NE)])
        w["wu"] = np.stack([lay_gu(inp["moe_w_up"][cidx, e], G_MOE) for e in range(NE)])
        w["wd"] = np.stack([lay_d(inp["moe_w_down"][cidx, e], G_MOE) for e in range(NE)])
        w["wr"] = np.ascontiguousarray(inp["moe_router"][cidx].reshape(8, 128, NE).transpose(1, 0, 2))
    w["ln_g"] = np.ascontiguousarray(inp["ln_g"][i])
    w["ln_b"] = np.ascontiguousarray(inp["ln_b"][i])
    return w


PAIRS = [[0, 1], [2, 3], [4, 5], [6, 7]]


def exchange_x(c, exin, exout, cinfo):
    m = c.mark()
    tmp = [c.alloc([8, 512], BF16) for _ in range(2)]
    k = 0
    for h in range(2):
        fl = c_flag(cinfo, h)
        v_in = exin[h].ap().rearrange("(k p) t -> p k t", p=128)
        for q in range(4):
            tb = tmp[k % 2]
            key = ("extmp", k % 2)
            c.op("gpsimd", lambda e, tb=tb, q=q, fl=fl: e.tensor_scalar(out=tb, in0=c.xbT[:, :, q * 512:(q + 1) * 512], scalar1=fl, scalar2=None, op0=ALU.mult),
                 reads=[("xbT", q), "cinfo"], writes=[key])
            c.op("sync", lambda e, tb=tb, q=q, v_in=v_in: e.dma_start(out=v_in[:, :, q * 512:(q + 1) * 512], in_=tb),
                 reads=[key], writes=[("ex_in", h)], dkey=f"exin{k % 2}")
            k += 1
        c.op("gpsimd", lambda e, h=h: e.collective_compute("AllReduce", ALU.add, replica_groups=PAIRS,
                                                           ins=[exin[h].ap().opt()], outs=[exout[h].ap().opt()]),
             reads=[("ex_in", h)], writes=[("ex_out", h)], dkey=f"cc{h}", dinc=1)
    c.release(m)


def c_flag(cinfo, h):
    return cinfo[:, 2:3] if h == 0 else cinfo[:, 1:2]


def exchange_pool_halo(c, pxin, pxout, cinfo):
    m = c.mark()
    ta = c.alloc([D], F32)
    tb = c.alloc([D], F32)
    pi = pxin.ap()
    for h in range(2):
        fl = c_flag(cinfo, h)
        c.op("gpsimd", lambda e, fl=fl: e.tensor_scalar(out=ta[0:32, :], in0=c.x_tm[0:32, 0, :], scalar1=fl[0:32, :], scalar2=None, op0=ALU.mult),
             reads=[("x", 0), "cinfo"], writes=["pxa"])
        c.op("gpsimd", lambda e, fl=fl: e.tensor_scalar(out=tb[96:128, :], in0=c.x_tm[96:128, 15, :], scalar1=fl[96:128, :], scalar2=None, op0=ALU.mult),
             reads=[("x", 15), "cinfo"], writes=["pxb"])
        c.op("sync", lambda e, h=h: e.dma_start(out=pi[h * 16:h * 16 + 8, :], in_=ta[0:8, :]), reads=["pxa"], writes=["px_in"], dkey="pxin")
        c.op("sync", lambda e, h=h: e.dma_start(out=pi[h * 16 + 8:h * 16 + 16, :], in_=tb[120:128, :]), reads=["pxb"], writes=["px_in"], dkey="pxin")
    c.op("gpsimd", lambda e: e.collective_compute("AllReduce", ALU.add, replica_groups=PAIRS, ins=[pxin.ap().opt()], outs=[pxout.ap().opt()]),
         reads=["px_in"], writes=["px_out"], dkey="ccp", dinc=1)
    c.release(m)


DEBUG_DUMP = False
FORCE_NONLAST = False
STOP_STAGE = None
N_LAYERS = DEPTH


def build_fused(wshapes):
    nc = bass.Bass("TRN2", target_bir_lowering=False)
    with ExitStack() as st:
        c = Ctx(nc, st)
        c.init_consts()
        Wl = [{k[3:]: c.dram_in(k, shp) for k, shp in wshapes.items() if k.startswith(f"L{i}_")} for i in range(DEPTH)]
        x_own = c.dram_in("x_own", [NT, D])
        xT_seq = c.dram_in("xT_seq", [128, 8, SEQ])
        pos_seq = c.dram_in("pos_seq", [SEQ], I32)
        pos_own = c.dram_in("pos_own", [NT], I32)
        ci_d = c.dram_in("cinfo", [128, 4])
        out = c.dram_out("out", [NT, D])
        exin = [nc.dram_tensor(f"exin{h}", [D, NT], BF16) for h in range(2)]
        exout = [nc.dram_tensor(f"exout{h}", [D, NT], BF16) for h in range(2)]
        pxin = nc.dram_tensor("pxin", [32, D], F32)
        pxout = nc.dram_tensor("pxout", [32, D], F32)
        exv = [t.ap().rearrange("(k p) t -> p k t", p=128) for t in exout]
        cinfo = c.alloc([4], F32)
        c.op("sync", lambda e: e.dma_start(out=cinfo, in_=ci_d), writes=["cinfo"], dkey="cinfo")
        load_x(c, x_own)
        for i in range(N_LAYERS):
            W = Wl[i]
            mk = c.mark()
            moe = (i % 2 == 1)
            router = None
            if moe:
                wr = c.alloc([8, NE], F32)
                logits = c.alloc([16, NE], F32)
                comb = c.alloc([16, NE], F32)
                c.op("sync", lambda e, wr=wr, W=W: e.dma_start(out=wr, in_=W["wr"]), writes=["wr"], dkey="wr")
                router = (wr, logits)
            mix = i % 3
            if mix == 0:
                if i == 0:
                    make_xbT(c)
                    src = ("f32", xT_seq)
                else:
                    src = ("bf16", exv)
                scale_x(c)
                mla_phase(c, src, pos_seq, pos_own, W["mla_wlq"], W["mla_wlkv"], W["mla_qng"], W["mla_kvg"], W["mla_wuq"], W["mla_wukv"], W["mla_wo"])
            elif mix == 1:
                exchange_pool_halo(c, pxin, pxout, cinfo)
                po = pxout.ap()

                def halo_fill(hprev, hnext):
                    c.op("gpsimd", lambda e: e.memset(hprev, 0.0), writes=["hprev"])
                    c.op("gpsimd", lambda e: e.memset(hnext, 0.0), writes=["hnext"])
                    c.op("sync", lambda e: e.dma_start(out=hprev[120:128, :], in_=po[8:16, :]), reads=["px_out"], writes=["hprev"], dkey="hprev")
                    c.op("sync", lambda e: e.dma_start(out=hnext[0:8, :], in_=po[16:24, :]), reads=["px_out"], writes=["hnext"], dkey="hnext")
                    c.op("gpsimd", lambda e: e.tensor_scalar(out=hprev[96:128, :], in0=hprev[96:128, :], scalar1=cinfo[96:128, 1:2], scalar2=None, op0=ALU.mult),
                         reads=["hprev", "cinfo"], writes=["hprev"])
                    c.op("gpsimd", lambda e: e.tensor_scalar(out=hnext[0:32, :], in0=hnext[0:32, :], scalar1=cinfo[0:32, 2:3], scalar2=None, op0=ALU.mult),
                         reads=["hnext", "cinfo"], writes=["hnext"])

                pool_phase(c, halo_fill, cinfo, W["pool_w"], W["pool_scale"])
            else:
                scale_x(c)
                mlstm_phase(c, exv, cinfo, {k[3:]: v for k, v in W.items() if k.startswith("ml_")})
            if STOP_STAGE == (i, "mixer"):
                ov = out.rearrange("(t p) d -> p t d", p=128)
                for t in range(16):
                    c.op("sync", lambda e, t=t, ov=ov: e.dma_start(out=ov[:, t, :], in_=c.x_tm[:, t, :]), reads=[("x", t)], dkey=f"dbg{t % 4}")
                break
            layernorm(c, W["ln_g"][0], W["ln_b"][0], router=router)
            if STOP_STAGE == (i, "ln1"):
                ov = out.rearrange("(t p) d -> p t d", p=128)
                for t in range(16):
                    c.op("sync", lambda e, t=t, ov=ov: e.dma_start(out=ov[:, t, :], in_=c.x_tm[:, t, :]), reads=[("x", t)], dkey=f"dbg{t % 4}")
                break
            if moe:
                moe_route(c, logits, comb)
                scale_x(c)
                ffn_phase(c, W["wg"], W["wu"], W["wd"], NE, D_FFE, G_MOE, moe=comb)
            else:
                scale_x(c)
                ffn_phase(c, W["wg"], W["wu"], W["wd"], 1, D_FF, G_DENSE)
            last = (i == N_LAYERS - 1)
            if FORCE_NONLAST and last:
                layernorm(c, W["ln_g"][1], W["ln_b"][1], out_dram=None)
                ov = out.rearrange("(t p) d -> p t d", p=128)
                for t in range(16):
                    c.op("sync", lambda e, t=t, ov=ov: e.dma_start(out=ov[:, t, :], in_=c.x_tm[:, t, :]), reads=[("x", t)], dkey=f"dbg{t % 4}")
                c.release(mk)
                continue
            layernorm(c, W["ln_g"][1], W["ln_b"][1], out_dram=out if last else None)
            if DEBUG_DUMP and not last:
                dbg = c.dram_out(f"dbg{i}", [NT, D]).rearrange("(t p) d -> p t d", p=128)
                for t in range(16):
                    c.op("sync", lambda e, t=t, dbg=dbg: e.dma_start(out=dbg[:, t, :], in_=c.x_tm[:, t, :]), reads=[("x", t)], dkey=f"dbg{t % 4}")
            if not last and (i + 1) % 3 != 1:
                exchange_x(c, exin, exout, cinfo)
            c.release(mk)
        c.P.emit()
    return nc


def kernel(**inputs):
    inp = {k: np.asarray(v) for k, v in inputs.items()}
    x = np.ascontiguousarray(inp["x"], dtype=np.float32)
    positions = np.asarray(inp["positions"]).astype(np.int32)
    weights = {}
    for i in range(N_LAYERS):
        if STOP_STAGE is not None and STOP_STAGE[0] == i and i % 2 == 1:
            inp = dict(inp)
            for kk in ("moe_w_gate", "moe_w_up", "moe_w_down"):
                inp[kk] = inp[kk][:, :, :, :512] if kk != "moe_w_down" else inp[kk][:, :, :512, :]
        for k, v in layer_weights(i, inp).items():
            weights[f"L{i}_{k}"] = v
    nc = build_fused({k: list(v.shape) for k, v in weights.items()})
    xT = [_fm(x[b]) for b in range(4)]
    in_maps = []
    for core in range(8):
        bi, hf = core // 2, core % 2
        ci = np.zeros((128, 4), np.float32)
        ci[:, 0] = hf * NT
        ci[:, 1] = float(hf == 1)
        ci[:, 2] = float(hf == 0)
        m = dict(weights)
        m["x_own"] = np.ascontiguousarray(x[bi, hf * NT:(hf + 1) * NT])
        m["xT_seq"] = xT[bi]
        m["pos_seq"] = np.ascontiguousarray(positions[bi])
        m["pos_own"] = np.ascontiguousarray(positions[bi, hf * NT:(hf + 1) * NT])
        m["cinfo"] = ci
        in_maps.append(m)
    res = run_bass_kernel_spmd(nc, in_maps, core_ids=list(range(8)))
    outs = [np.asarray(r["out"]) for r in res.results]
    return np.stack([np.concatenate([outs[2 * b], outs[2 * b + 1]], axis=0) for b in range(4)]).astype(np.float32)
```

```python
import math
from contextlib import ExitStack

import numpy as np
import concourse.bass as bass
import concourse.mybir as mybir
from concourse.bass_utils import run_bass_kernel_spmd

F32 = mybir.dt.float32
BF16 = mybir.dt.bfloat16
I32 = mybir.dt.int32
AF = mybir.ActivationFunctionType
ALU = mybir.AluOpType
AX = mybir.AxisListType

D = 1024
NT = 2048
SEQ = 4096
DEPTH = 4
ALPHA = (2.0 * DEPTH) ** 0.25
LN_EPS = 1e-5
RMS_EPS = 1e-6
D_FF = 2816
D_FFE = 3584
NE = 8

ENGS = ("sync", "scalar", "vector", "gpsimd", "tensor")


class Op:
    __slots__ = ("eng", "fn", "waits", "dkey", "dval", "sig", "idx", "epoch", "dinc")

    def __init__(self, eng, fn):
        self.eng = eng
        self.fn = fn
        self.waits = []
        self.dkey = None
        self.dval = 0
        self.sig = None
        self.idx = None
        self.epoch = 0
        self.dinc = 16


class Prog:
    def __init__(self, nc):
        self.nc = nc
        self.ops = {e: [] for e in ENGS}
        self.last_write = {}
        self.readers = {}
        self.dcount = {}
        self.epoch = 0
        self.waited_e = {}
        self.waited_d = {}
        self.n_epochs = 1
        self.epoch_dkeys = set()

    def op(self, eng, fn, reads=(), writes=(), dkey=None, dinc=16):
        o = Op(eng, fn)
        o.dinc = dinc
        o.epoch = self.epoch
        o.idx = len(self.ops[eng])
        psr = [k for k in reads if isinstance(k, tuple) and k[0] == "ps"]
        if psr:
            writes = list(writes) + [k for k in psr if k not in writes]
        deps = []
        for k in reads:
            w = self.last_write.get(k)
            if w is not None:
                deps.append(w)
        for k in writes:
            w = self.last_write.get(k)
            if w is not None:
                deps.append(w)
            deps.extend(self.readers.get(k, ()))
        for d in deps:
            self._add_wait(o, d)
        if dkey is not None:
            key = dkey
            self.epoch_dkeys.add(key)
            self.dcount[key] = self.dcount.get(key, 0) + dinc
            o.dkey = key
            o.dval = self.dcount[key]
            tok = ("d", key, o.dval)
        else:
            tok = ("e", eng, o.idx)
        for k in writes:
            self.last_write[k] = tok
            self.readers[k] = []
        for k in reads:
            self.readers.setdefault(k, []).append(tok)
        self.ops[eng].append(o)
        return o

    def _add_wait(self, o, tok):
        eng = o.eng
        if tok[0] == "d":
            _, key, val = tok
            pk = (eng, key)
            if self.waited_d.get(pk, 0) >= val:
                return
            self.waited_d[pk] = val
            o.waits.append(tok)
        else:
            _, peng, pidx = tok
            if peng == eng and eng in ("tensor", "sync"):
                return
            pk = (eng, peng)
            if self.waited_e.get(pk, -1) >= pidx:
                return
            self.waited_e[pk] = pidx
            o.waits.append(tok)

    def barrier(self):
        lasts = {}
        for e in ENGS:
            li = -1
            for o in reversed(self.ops[e]):
                if o.fn is not None and o.dkey is None:
                    li = o.idx
                    break
            lasts[e] = li
        dk = [(k, self.dcount[k]) for k in sorted(self.epoch_dkeys, key=str)]
        self.epoch_dkeys = set()
        for e in ENGS:
            o = Op(e, None)
            o.epoch = self.epoch
            o.idx = len(self.ops[e])
            for pe in ENGS:
                if pe != e and lasts[pe] >= 0:
                    self._add_wait(o, ("e", pe, lasts[pe]))
            for k, v in dk:
                self._add_wait(o, ("d", k, v))
            self.ops[e].append(o)
        self.last_write = {}
        self.readers = {}
        self.epoch += 1
        self.n_epochs = self.epoch + 1
        self.waited_e = {}
        self.waited_d = {}

    def emit(self):
        nc = self.nc
        self.barrier()
        need = {e: set() for e in ENGS}
        for e in ENGS:
            for o in self.ops[e]:
                for w in o.waits:
                    if w[0] == "e":
                        need[w[1]].add(w[2])
        K_ROT = 4
        sigval = {}
        self.maxsig = 0
        for e in ENGS:
            cnt = {}
            for o in self.ops[e]:
                if o.idx in need[e]:
                    r = o.epoch % K_ROT
                    c = cnt.get(r, 0) + 1
                    cnt[r] = c
                    o.sig = c
                    sigval[(e, o.idx)] = (r, c)
                    self.maxsig = max(self.maxsig, c)
        dkeys = sorted(self.dcount.keys(), key=str)
        self.n_sems = 5 * K_ROT + len(dkeys)
        with ExitStack() as st:
            for i in range(getattr(self, "pad_sems", 0)):
                st.enter_context(nc.semaphore(f"pad_{i}"))
            esem = {}
            for e in ENGS:
                for ep in range(K_ROT):
                    esem[(e, ep)] = st.enter_context(nc.semaphore(f"s_{e}_{ep}"))
            dsem = {}
            for i, k in enumerate(dkeys):
                dsem[k] = st.enter_context(nc.semaphore(f"d_{i}"))
            block = st.enter_context(nc.Block())

            def run(ename):
                def body(eng):
                    for o in self.ops[ename]:
                        for w in o.waits:
                            if w[0] == "d":
                                eng.wait_ge(dsem[w[1]], w[2])
                            else:
                                ep, c = sigval[(w[1], w[2])]
                                eng.wait_ge(esem[(w[1], ep)], c)
                        if o.fn is None:
                            continue
                        ins = o.fn(eng)
                        if o.dkey is not None:
                            ins.then_inc(dsem[o.dkey], o.dinc)
                        elif o.sig is not None:
                            ins.then_inc(esem[(ename, o.epoch % K_ROT)], 1)
                return body

            block.sync(run("sync"))
            block.scalar(run("scalar"))
            block.vector(run("vector"))
            block.gpsimd(run("gpsimd"))
            block.tensor(run("tensor"))


ARENA_W = 49000


class Ctx:
    def __init__(self, nc, st):
        self.nc = nc
        self.P = Prog(nc)
        self.st = st
        self.arena = st.enter_context(nc.sbuf_tensor("arena", [128, ARENA_W], F32))
        self.top = 0
        self.ps = st.enter_context(nc.psum_tensor("ps", [128, 8, 512], F32))
        self.x_tm = self.alloc([16, D], F32)
        self.xbT = self.alloc([8, NT], BF16)
        self.ident = self.alloc([128], F32)
        self.ones = self.alloc([128], F32)
        self.stat = self.alloc([16, 8], F32)
        self.n_in = 0

    def alloc(self, shape, dt, parts=128):
        n = 1
        for v in shape:
            n *= v
        words = n if dt in (F32, I32) else (n + 1) // 2
        off = self.top
        self.top += words
        assert self.top <= ARENA_W, f"arena overflow {self.top}"
        v = self.arena[0:parts, off:off + words]
        if dt == BF16:
            v = v.bitcast(BF16)
        elif dt == I32:
            v = v.bitcast(I32)
        if len(shape) == 2:
            v = v.rearrange("p (a b) -> p a b", a=shape[0])
        elif len(shape) == 3:
            v = v.rearrange("p (a b c) -> p a b c", a=shape[0], b=shape[1])
        return v

    def mark(self):
        return self.top

    def release(self, m):
        self.P.barrier()
        self.top = m

    def dram_in(self, name, shape, dt=F32):
        return self.nc.dram_tensor(name, list(shape), dt, kind="ExternalInput").ap()

    def dram_out(self, name, shape, dt=F32):
        return self.nc.dram_tensor(name, list(shape), dt, kind="ExternalOutput").ap()

    def op(self, *a, **k):
        return self.P.op(*a, **k)

    def init_consts(self):
        P = self.P
        ident, ones = self.ident, self.ones
        P.op("gpsimd", lambda e: e.memset(ones, 1.0), writes=["ones"])
        P.op("gpsimd", lambda e: e.memset(ident, 1.0), writes=["ident"])
        P.op("gpsimd", lambda e: e.affine_select(
            out=ident, in_=ident, pattern=[[-1, 128]], compare_op=ALU.is_equal,
            fill=0.0, base=0, channel_multiplier=1), reads=["ident"], writes=["ident"])


def load_x(c, x_own):
    xv = x_own.rearrange("(t p) d -> p t d", p=128)
    for q in range(4):
        c.op("sync", lambda e, q=q: e.dma_start(out=c.x_tm[:, 4 * q:4 * q + 4, :], in_=xv[:, 4 * q:4 * q + 4, :]),
             writes=[("x", t) for t in range(4 * q, 4 * q + 4)], dkey=f"xin{q}")


def make_xbT(c, tiles=range(16), router=None):
    ps = c.ps
    for t in tiles:
        b0 = 6
        for kc in range(8):
            c.op("tensor", lambda e, t=t, kc=kc: e.transpose(
                out=ps[:, b0 + kc // 4, (kc % 4) * 128:(kc % 4 + 1) * 128],
                in_=c.x_tm[:, t, kc * 128:(kc + 1) * 128], identity=c.ident),
                reads=[("x", t), "ident"], writes=[("ps", b0 + kc // 4)])
        src = ps[:, 6:8, :].rearrange("p b (k j) -> p (b k) j", j=128)
        c.op("scalar", lambda e, t=t, src=src: e.copy(out=c.xbT[:, :, t * 128:(t + 1) * 128], in_=src),
             reads=[("ps", 6), ("ps", 7)], writes=[("xbT", t // 4)])
        if router is not None:
            wr, logits, xtf = router
            c.op("vector", lambda e, src=src: e.tensor_copy(out=xtf, in_=src),
                 reads=[("ps", 6), ("ps", 7)], writes=["xtf"])
            for kc in range(8):
                c.op("tensor", lambda e, t=t, kc=kc: e.matmul(
                    ps[:, 5, 0:8], lhsT=xtf[:, kc, :], rhs=wr[:, kc, :], start=(kc == 0), stop=(kc == 7)),
                    reads=["xtf", "wr"], writes=[("ps", 5)])
            c.op("vector", lambda e, t=t: e.tensor_copy(out=logits[:, t, :], in_=ps[:, 5, 0:8]),
                 reads=[("ps", 5)], writes=["logits"])


def scale_x(c):
    for t in range(16):
        c.op("gpsimd", lambda e, t=t: e.tensor_scalar(
            out=c.x_tm[:, t, :], in0=c.x_tm[:, t, :], scalar1=float(ALPHA), scalar2=None, op0=ALU.mult),
            reads=[("x", t)], writes=[("x", t)])


def layernorm(c, g_ap, b_ap, router=None, out_dram=None):
    stat = c.stat
    m = c.mark()
    gb = c.alloc([2, D], F32)
    c.gb = gb
    if router is not None:
        router = (router[0], router[1], c.alloc([8, 128], F32))
    c.op("sync", lambda e: e.dma_start(out=gb[:, 0, :], in_=g_ap.partition_broadcast(128)), writes=["gb"], dkey="gb")
    c.op("sync", lambda e: e.dma_start(out=gb[:, 1, :], in_=b_ap.partition_broadcast(128)), writes=["gb"], dkey="gb")
    junk = c.alloc([D], BF16)
    xn = [c.alloc([D], F32) for _ in range(2)]
    col = lambda i: stat[:, :, i]
    for t in range(16):
        xt = c.x_tm[:, t, :]
        c.op("scalar", lambda e, xt=xt, t=t: e.activation(out=junk, in_=xt, func=AF.Identity, accum_out=stat[:, t, 0:1]),
             reads=[("x", t)], writes=[("stA", t)])
        c.op("scalar", lambda e, xt=xt, t=t: e.activation(out=junk, in_=xt, func=AF.Square, accum_out=stat[:, t, 1:2]),
             reads=[("x", t)], writes=[("stB", t)])
    V = "vector"
    c.op(V, lambda e: e.tensor_scalar(out=stat[:, :, 2:4], in0=stat[:, :, 0:2], scalar1=1.0 / D, scalar2=None, op0=ALU.mult),
         reads=[("stA", t) for t in range(16)] + [("stB", t) for t in range(16)], writes=["stat"])
    c.op(V, lambda e: e.tensor_tensor(out=col(4), in0=col(2), in1=col(2), op=ALU.mult), reads=["stat"], writes=["stat"])
    c.op(V, lambda e: e.tensor_tensor(out=col(5), in0=col(3), in1=col(4), op=ALU.subtract), reads=["stat"], writes=["stat"])
    c.op("scalar", lambda e: e.activation(out=col(6), in_=col(5), func=AF.Sqrt, bias=float(LN_EPS), scale=1.0), reads=["stat"], writes=["stat"])
    c.op(V, lambda e: e.reciprocal(out=col(6), in_=col(6)), reads=["stat"], writes=["stat"])
    c.op(V, lambda e: e.scalar_tensor_tensor(out=col(7), in0=col(2), scalar=-1.0, in1=col(6), op0=ALU.mult, op1=ALU.mult),
         reads=["stat"], writes=["stat"])
    ov = out_dram.rearrange("(t p) d -> p t d", p=128) if out_dram is not None else None
    for t in range(16):
        xt = c.x_tm[:, t, :]
        xb = xn[t % 2]
        kx = ("xn", t % 2)
        c.op("scalar", lambda e, xt=xt, t=t, xb=xb: e.activation(out=xb, in_=xt, func=AF.Identity, scale=stat[:, t, 6:7], bias=stat[:, t, 7:8]),
             reads=[("x", t), "stat"], writes=[kx])
        c.op("vector", lambda e, xb=xb: e.tensor_tensor(out=xb, in0=xb, in1=gb[:, 0, :], op=ALU.mult),
             reads=[kx, "gb"], writes=[kx])
        c.op("gpsimd", lambda e, xt=xt, xb=xb: e.tensor_tensor(out=xt, in0=xb, in1=gb[:, 1, :], op=ALU.add),
             reads=[kx, "gb"], writes=[("x", t)])
        if ov is not None:
            c.op("sync", lambda e, t=t: e.dma_start(out=ov[:, t, :], in_=c.x_tm[:, t, :]),
                 reads=[("x", t)], dkey=f"out{t % 4}")
        else:
            make_xbT(c, tiles=[t], router=router)
    c.release(m)


def ffn_phase(c, wg, wu, wd, n_exp, n_f, G, moe=None):
    ps = c.ps
    n_grp = n_f // (G * 128)
    GW = G * 128
    NSTG = 2
    m = c.mark()
    stg_g = [c.alloc([8, GW], F32) for i in range(NSTG)]
    stg_u = [c.alloc([8, GW], F32) for i in range(NSTG)]
    stg_d = [c.alloc([G, D], F32) for i in range(NSTG)]
    wgb = [c.alloc([8, GW], BF16) for i in range(2)]
    wub = [c.alloc([8, GW], BF16) for i in range(2)]
    wdb = [c.alloc([G, D], BF16) for i in range(2)]
    sg = [c.alloc([512], F32) for i in range(2)]
    hb = [c.alloc([G, 512], BF16) for i in range(2)]
    it = 0
    hcnt = 0
    pending = None

    def emit_gateup(b_, tt, hs, fc):
        pg = (2 * fc) % 4
        pu = (2 * fc + 1) % 4
        for kc in range(8):
            c.op("tensor", lambda e, kc=kc: e.matmul(
                ps[:, pg, :], lhsT=wgb[b_][:, kc, fc * 128:(fc + 1) * 128],
                rhs=c.xbT[:, kc, tt * 512:(tt + 1) * 512], start=(kc == 0), stop=(kc == 7)),
                reads=[("wgb", b_), ("xbT", tt)], writes=[("ps", pg)])
        for kc in range(8):
            c.op("tensor", lambda e, kc=kc: e.matmul(
                ps[:, pu, :], lhsT=wub[b_][:, kc, fc * 128:(fc + 1) * 128],
                rhs=c.xbT[:, kc, tt * 512:(tt + 1) * 512], start=(kc == 0), stop=(kc == 7)),
                reads=[("wub", b_), ("xbT", tt)], writes=[("ps", pu)])
        si = fc % 2
        c.op("scalar", lambda e: e.activation(out=sg[si], in_=ps[:, pg, :], func=AF.Silu),
             reads=[("ps", pg)], writes=[("sg", si)])
        c.op("vector", lambda e: e.tensor_tensor(out=hb[hs][:, fc, :], in0=ps[:, pu, :], in1=sg[si], op=ALU.mult),
             reads=[("ps", pu), ("sg", si)], writes=[("hb", hs)])

    def emit_down(ex, b_, tt, hs):
        for ts in range(4):
            tile = tt * 4 + ts
            for dh in range(2):
                py = 4 + (ts * 2 + dh) % 4
                for fc in range(G):
                    c.op("tensor", lambda e, fc=fc, ts=ts, dh=dh, py=py: e.matmul(
                        ps[:, py, :], lhsT=hb[hs][:, fc, ts * 128:(ts + 1) * 128],
                        rhs=wdb[b_][:, fc, dh * 512:(dh + 1) * 512], start=(fc == 0), stop=(fc == G - 1)),
                        reads=[("hb", hs), ("wdb", b_)], writes=[("ps", py)])
                xs = c.x_tm[:, tile, dh * 512:(dh + 1) * 512]
                if moe is None:
                    c.op("vector", lambda e, xs=xs, py=py: e.tensor_tensor(out=xs, in0=ps[:, py, :], in1=xs, op=ALU.add),
                         reads=[("ps", py), ("x", tile)], writes=[("x", tile)])
                else:
                    c.op("vector", lambda e, xs=xs, py=py, tile=tile: e.scalar_tensor_tensor(
                        out=xs, in0=ps[:, py, :], scalar=moe[:, tile, ex:ex + 1], in1=xs, op0=ALU.mult, op1=ALU.add),
                        reads=[("ps", py), ("x", tile), "comb"], writes=[("x", tile)])

    for ex in range(n_exp):
        for g in range(n_grp):
            s_ = it % NSTG
            b_ = it % 2
            c.op("sync", lambda e, ex=ex, g=g, s_=s_: e.dma_start(out=stg_g[s_], in_=wg[ex, g]),
                 writes=[("stg_g", s_)], dkey=f"stg_g{s_}")
            c.op("sync", lambda e, ex=ex, g=g, s_=s_: e.dma_start(out=stg_u[s_], in_=wu[ex, g]),
                 writes=[("stg_u", s_)], dkey=f"stg_u{s_}")
            c.op("sync", lambda e, ex=ex, g=g, s_=s_: e.dma_start(out=stg_d[s_], in_=wd[ex, g]),
                 writes=[("stg_d", s_)], dkey=f"stg_d{s_}")
            c.op("gpsimd", lambda e, s_=s_, b_=b_: e.tensor_copy(out=wgb[b_], in_=stg_g[s_]),
                 reads=[("stg_g", s_)], writes=[("wgb", b_)])
            c.op("gpsimd", lambda e, s_=s_, b_=b_: e.tensor_copy(out=wub[b_], in_=stg_u[s_]),
                 reads=[("stg_u", s_)], writes=[("wub", b_)])
            c.op("gpsimd", lambda e, s_=s_, b_=b_: e.tensor_copy(out=wdb[b_], in_=stg_d[s_]),
                 reads=[("stg_d", s_)], writes=[("wdb", b_)])
            for tt in range(4):
                hs = hcnt % 2
                hcnt += 1
                for fc in range(G):
                    emit_gateup(b_, tt, hs, fc)
                    if fc == 0 and pending is not None:
                        emit_down(*pending)
                        pending = None
                pending = (ex, b_, tt, hs)
            it += 1
    if pending is not None:
        emit_down(*pending)
    c.release(m)


def moe_route(c, logits, comb):
    m = c.mark()
    m1 = c.alloc([16], F32)
    m2 = c.alloc([16], F32)
    t1 = c.alloc([16, 8], F32)
    l2 = c.alloc([16, 8], F32)
    sel = c.alloc([16, 8], F32)
    w1 = c.alloc([16], F32)
    w2 = c.alloc([16], F32)
    V = "vector"
    bc = lambda a: a.unsqueeze(2).to_broadcast([128, 16, 8])
    c.op(V, lambda e: e.tensor_reduce(out=m1, in_=logits, axis=AX.X, op=ALU.max), reads=["logits"], writes=["rt_m1"])
    c.op(V, lambda e: e.tensor_tensor(out=t1, in0=logits, in1=bc(m1), op=ALU.is_equal), reads=["logits", "rt_m1"], writes=["rt_t1"])
    c.op(V, lambda e: e.scalar_tensor_tensor(out=l2, in0=t1, scalar=-1e30, in1=logits, op0=ALU.mult, op1=ALU.add),
         reads=["rt_t1", "logits"], writes=["rt_l2"])
    c.op(V, lambda e: e.tensor_reduce(out=m2, in_=l2, axis=AX.X, op=ALU.max), reads=["rt_l2"], writes=["rt_m2"])
    c.op(V, lambda e: e.tensor_tensor(out=sel, in0=l2, in1=bc(m2), op=ALU.is_equal), reads=["rt_l2", "rt_m2"], writes=["rt_sel"])
    c.op(V, lambda e: e.tensor_tensor(out=w2, in0=m2, in1=m1, op=ALU.subtract), reads=["rt_m1", "rt_m2"], writes=["rt_w2"])
    c.op("scalar", lambda e: e.activation(out=w2, in_=w2, func=AF.Sigmoid), reads=["rt_w2"], writes=["rt_w2"])
    c.op(V, lambda e: e.tensor_scalar(out=w1, in0=w2, scalar1=-1.0, scalar2=1.0, op0=ALU.mult, op1=ALU.add), reads=["rt_w2"], writes=["rt_w1"])
    c.op(V, lambda e: e.tensor_tensor(out=t1, in0=t1, in1=bc(w1), op=ALU.mult), reads=["rt_t1", "rt_w1"], writes=["rt_t1"])
    c.op(V, lambda e: e.tensor_tensor(out=sel, in0=sel, in1=bc(w2), op=ALU.mult), reads=["rt_sel", "rt_w2"], writes=["rt_sel"])
    c.op(V, lambda e: e.tensor_tensor(out=comb, in0=t1, in1=sel, op=ALU.add), reads=["rt_t1", "rt_sel"], writes=["comb"])
    c.release(m)


def lay_gu(w, G):
    K, F = w.shape
    GW = G * 128
    return np.ascontiguousarray(w.reshape(8, 128, F // GW, GW).transpose(2, 1, 0, 3))


def lay_d(w, G):
    F, Dm = w.shape
    return np.ascontiguousarray(w.reshape(F // (G * 128), G, 128, Dm).transpose(0, 2, 1, 3))


POOL_W = (2, 4, 8, 16)


def pool_phase(c, halo_fill, cinfo, pw_d, pscale_d):
    ps = c.ps
    m = c.mark()
    hprev = c.alloc([D], F32)
    hnext = c.alloc([D], F32)
    Bp = c.alloc([4, 128], F32)
    Bm = c.alloc([4, 128], F32)
    Bn = c.alloc([4, 128], F32)
    pos = c.alloc([NT], F32)
    posi = c.alloc([NT], I32)
    cntt = [c.alloc([128], F32) for _ in range(2)]
    rct = [c.alloc([128], F32) for _ in range(2)]
    tmpt = [c.alloc([128], F32) for _ in range(2)]
    wst = c.alloc([4, 2, 256], F32)
    wpb = c.alloc([4, 2, 256], BF16)
    scb = c.alloc([D], F32)
    tB = [c.alloc([128], F32) for _ in range(2)]
    zb = [c.alloc([2, 128], BF16) for _ in range(2)]
    ybuf = [c.alloc([D], F32) for _ in range(3)]
    G_ = "gpsimd"
    V = "vector"
    halo_fill(hprev, hnext)
    c.op("sync", lambda e: e.dma_start(out=wst, in_=pw_d.rearrange("g p c j -> p g c j")), writes=["wst"], dkey="wst")
    c.op("sync", lambda e: e.dma_start(out=scb, in_=pscale_d.partition_broadcast(128)), writes=["scb"], dkey="scb")
    c.op(V, lambda e: e.tensor_copy(out=wpb, in_=wst), reads=["wst"], writes=["wpb"])
    for gi, w in enumerate(POOL_W):
        h = w // 2
        c.op(G_, lambda e, gi=gi: e.memset(Bp[:, gi, :], 1.0), writes=[("Bp", gi)])
        c.op(G_, lambda e, gi=gi, h=h: e.affine_select(out=Bp[:, gi, :], in_=Bp[:, gi, :], pattern=[[-1, 128]],
                                                       compare_op=ALU.is_ge, fill=0.0, base=-(128 - h), channel_multiplier=1),
             reads=[("Bp", gi)], writes=[("Bp", gi)])
        c.op(G_, lambda e, gi=gi: e.memset(Bn[:, gi, :], 1.0), writes=[("Bn", gi)])
        c.op(G_, lambda e, gi=gi, h=h: e.affine_select(out=Bn[:, gi, :], in_=Bn[:, gi, :], pattern=[[1, 128]],
                                                       compare_op=ALU.is_ge, fill=0.0, base=-(129 - h), channel_multiplier=-1),
             reads=[("Bn", gi)], writes=[("Bn", gi)])
        c.op(G_, lambda e, gi=gi: e.memset(Bm[:, gi, :], 1.0), writes=[("Bm", gi)])
        c.op(G_, lambda e, gi=gi, h=h: e.affine_select(out=Bm[:, gi, :], in_=Bm[:, gi, :], pattern=[[-1, 128]],
                                                       compare_op=ALU.is_ge, fill=0.0, base=h, channel_multiplier=1),
             reads=[("Bm", gi)], writes=[("Bm", gi)])
        c.op(G_, lambda e, gi=gi, h=h: e.affine_select(out=Bm[:, gi, :], in_=Bm[:, gi, :], pattern=[[1, 128]],
                                                       compare_op=ALU.is_ge, fill=0.0, base=h - 1, channel_multiplier=-1),
             reads=[("Bm", gi)], writes=[("Bm", gi)])
    c.op(G_, lambda e: e.iota(posi, pattern=[[1, NT]], base=0, channel_multiplier=0), writes=["posi"])
    c.op(V, lambda e: e.tensor_copy(out=pos, in_=posi), reads=["posi"], writes=["pos"])
    c.op(V, lambda e: e.tensor_scalar(out=pos, in0=pos, scalar1=cinfo[:, 0:1], scalar2=None, op0=ALU.add),
         reads=["pos", "cinfo"], writes=["pos"])
    def src_tile(j):
        if j < 0:
            return hprev, "hprev"
        if j > 15:
            return hnext, "hnext"
        return c.x_tm[:, j, :], ("x", j)

    def compute(j):
        yb = ybuf[j % 3]
        ky = ("ybuf", j % 3)
        for gi in range(4):
            k = (j * 4 + gi) % 2
            tok = slice(j * 128, (j + 1) * 128)
            h = POOL_W[gi] // 2
            c.op(V, lambda e, k=k, h=h, tok=tok: e.tensor_scalar(out=cntt[k], in0=pos[:, tok], scalar1=float(h), scalar2=float(SEQ), op0=ALU.add, op1=ALU.min),
                 reads=["pos"], writes=[("cntt", k)])
            c.op(V, lambda e, k=k, h=h, tok=tok: e.tensor_scalar(out=tmpt[k], in0=pos[:, tok], scalar1=float(-h), scalar2=0.0, op0=ALU.add, op1=ALU.max),
                 reads=["pos"], writes=[("tmpt", k)])
            c.op(V, lambda e, k=k: e.tensor_tensor(out=cntt[k], in0=cntt[k], in1=tmpt[k], op=ALU.subtract),
                 reads=[("tmpt", k), ("cntt", k)], writes=[("cntt", k)])
            c.op(V, lambda e, k=k: e.reciprocal(out=rct[k], in_=cntt[k]), reads=[("cntt", k)], writes=[("rct", k)])
            c.op(V, lambda e, k=k: e.scalar_tensor_tensor(out=tB[k], in0=cntt[k], scalar=-1.0, in1=c.ident, op0=ALU.mult, op1=ALU.mult),
                 reads=[("cntt", k), "ident"], writes=[("tB", k)])
            c.op(G_, lambda e, k=k, gi=gi: e.tensor_tensor(out=tB[k], in0=tB[k], in1=Bm[:, gi, :], op=ALU.add),
                 reads=[("tB", k), ("Bm", gi)], writes=[("tB", k)])
            pb = k
            for cc in range(2):
                fs = slice(gi * 256 + cc * 128, gi * 256 + (cc + 1) * 128)
                srcs = [(src_tile(j - 1), Bp[:, gi, :], ("Bp", gi)), (src_tile(j), tB[k], ("tB", k)), (src_tile(j + 1), Bn[:, gi, :], ("Bn", gi))]
                for si, ((sap, skey), bap, bkey) in enumerate(srcs):
                    c.op("tensor", lambda e, sap=sap, fs=fs, bap=bap, pb=pb, cc=cc, si=si: e.matmul(
                        ps[:, pb, cc * 128:(cc + 1) * 128], lhsT=sap[:, fs], rhs=bap, start=(si == 0), stop=(si == 2)),
                        reads=[skey, bkey], writes=[("ps", pb)])
            c.op(V, lambda e, k=k, pb=pb, gi=gi, tok=tok: e.tensor_tensor(
                out=zb[k], in0=ps[:, pb, 0:256].rearrange("p (c t) -> p c t", c=2),
                in1=rct[k].unsqueeze(1).to_broadcast([128, 2, 128]), op=ALU.mult),
                reads=[("ps", pb), ("rct", k)], writes=[("zb", k)])
            py = 2 + (j % 2) * 2 + gi // 2
            for cc in range(2):
                c.op("tensor", lambda e, k=k, cc=cc, gi=gi, py=py: e.matmul(
                    ps[:, py, (gi % 2) * 256:(gi % 2 + 1) * 256], lhsT=zb[k][:, cc, :], rhs=wpb[:, gi, cc, :],
                    start=(cc == 0), stop=(cc == 1)),
                    reads=[("zb", k), "wpb"], writes=[("ps", py)])
        pbase = 2 + (j % 2) * 2
        c.op(V, lambda e, yb=yb, pbase=pbase: e.tensor_tensor(
            out=yb, in0=ps[:, pbase:pbase + 2, :].rearrange("p b j -> p (b j)"), in1=scb, op=ALU.mult),
            reads=[("ps", pbase), ("ps", pbase + 1), "scb"], writes=[ky])

    def update(j):
        yb = ybuf[j % 3]
        ky = ("ybuf", j % 3)
        c.op(V, lambda e, yb=yb, j=j: e.scalar_tensor_tensor(out=c.x_tm[:, j, :], in0=c.x_tm[:, j, :], scalar=float(ALPHA), in1=yb, op0=ALU.mult, op1=ALU.add),
             reads=[ky, ("x", j)], writes=[("x", j)])

    compute(0)
    for j in range(1, 16):
        compute(j)
        update(j - 1)
    update(15)
    c.release(m)


MLA_H = 8
Q_RANK = 384
KV_RANK = 256
ATT_SCALE = (128 + 64) ** -0.5
TWO_PI = 2.0 * math.pi


def mla_phase(c, src, pos_keys, pos_own, wlq_d, wlkv_d, qng_d, kvg_d, wuq_d, wukv_d, wo_d):
    ps = c.ps
    V, G_, A, T = "vector", "gpsimd", "scalar", "tensor"
    m0 = c.mark()
    cos_own = c.alloc([NT], BF16, parts=64)
    ss_own = c.alloc([NT], BF16, parts=64)
    cqnT = c.alloc([3, NT], BF16)
    ckvnT = c.alloc([2, SEQ], BF16)
    krT = c.alloc([SEQ], BF16)
    kmr = c.alloc([16], F32)
    invf = c.alloc([1], F32, parts=64)
    ones_bf = c.alloc([128], BF16)
    qng = c.alloc([3], F32)
    kvg = c.alloc([2], F32)
    m1 = c.mark()
    wlq = c.alloc([8, 384], BF16)
    wlkv = c.alloc([8, 384], BF16)
    xst = c.alloc([8, 256], F32)
    wl_st = xst[:, :, 0:192]
    xob = c.alloc([8, 512], BF16)
    sqr0 = c.alloc([512], BF16, parts=64)
    sq = c.alloc([3, 512], F32)
    rstd = c.alloc([512], F32)
    posi = c.alloc([512], I32, parts=64)
    ang = c.alloc([512], F32, parts=64)
    ang2 = c.alloc([512], F32, parts=64)
    cosb = c.alloc([512], BF16, parts=64)
    ssb = c.alloc([512], BF16, parts=64)
    tmpr = c.alloc([512], F32, parts=64)
    frow = c.alloc([64], F32, parts=1)

    for i in range(64):
        val = float(np.float32(10000.0) ** np.float32(-(2 * (i % 32)) / 64.0))
        c.op(G_, lambda e, i=i, val=val: e.memset(frow[0:1, i:i + 1], val), writes=["frow"])
    c.op(T, lambda e: e.matmul(ps[0:64, 7, 0:1], lhsT=frow[0:1, :], rhs=c.ones[0:1, 0:1], start=True, stop=True),
         reads=["frow", "ones"], writes=[("ps", 7)])
    c.op(V, lambda e: e.tensor_copy(out=invf, in_=ps[0:64, 7, 0:1]), reads=[("ps", 7)], writes=["invf"])
    c.op(G_, lambda e: e.memset(ones_bf, 1.0), writes=["ones_bf"])
    c.op(G_, lambda e: e.memset(krT[64:65, :], 1.0), writes=["krT_ones"])
    c.op("sync", lambda e: e.dma_start(out=qng, in_=qng_d), writes=["qng"], dkey="qng")
    c.op("sync", lambda e: e.dma_start(out=kvg, in_=kvg_d), writes=["kvg"], dkey="kvg")
    for (wd_, wb_, wk_) in ((wlq_d, wlq, "wlq"), (wlkv_d, wlkv, "wlkv")):
        for hf in range(2):
            cs = slice(hf * 192, (hf + 1) * 192)
            c.op("sync", lambda e, wd_=wd_, cs=cs: e.dma_start(out=wl_st, in_=wd_[:, :, cs]), writes=["xst"], dkey="xst")
            c.op(G_, lambda e, wb_=wb_, cs=cs: e.tensor_copy(out=wb_[:, :, cs], in_=wl_st), reads=["xst"], writes=[wk_])

    def rms_norm(banks, nch, rank, gcol, dst, tok, tag, gkey):
        for cc in range(nch):
            c.op(A, lambda e, cc=cc: e.activation(out=sq[:, cc, :], in_=ps[:, banks[cc], :], func=AF.Square),
                 reads=[("ps", banks[cc])], writes=[("sq", cc)])
        for cc in range(nch):
            c.op(T, lambda e, cc=cc: e.matmul(ps[:, 7, :], lhsT=c.ones, rhs=sq[:, cc, :], start=(cc == 0), stop=(cc == nch - 1)),
                 reads=[("sq", cc), "ones"], writes=[("ps", 7)])
        c.op(A, lambda e: e.activation(out=rstd, in_=ps[:, 7, :], func=AF.Sqrt, bias=float(RMS_EPS), scale=1.0 / rank),
             reads=[("ps", 7)], writes=["rstd"])
        c.op(V, lambda e: e.reciprocal(out=rstd, in_=rstd), reads=["rstd"], writes=["rstd"])
        for cc in range(nch):
            c.op(V, lambda e, cc=cc: e.scalar_tensor_tensor(out=dst[:, cc, tok], in0=ps[:, banks[cc], :], scalar=gcol[:, cc:cc + 1],
                                                            in1=rstd, op0=ALU.mult, op1=ALU.mult),
                 reads=[("ps", banks[cc]), "rstd", gkey], writes=[tag])

    def rope_tables(pos_ap, cdst, sdst, ck, sk):
        c.op("sync", lambda e: e.dma_start(out=posi, in_=pos_ap.partition_broadcast(64)), writes=["posi"], dkey="posi")
        c.op(V, lambda e: e.tensor_copy(out=ang, in_=posi), reads=["posi"], writes=["ang"])
        c.op(V, lambda e: e.tensor_scalar(out=ang, in0=ang, scalar1=invf[:, 0:1], scalar2=None, op0=ALU.mult), reads=["ang", "invf"], writes=["ang"])
        c.op(V, lambda e: e.tensor_scalar(out=ang2, in0=ang, scalar1=float(0.5 * math.pi), scalar2=None, op0=ALU.add),
             reads=["ang"], writes=["ang2"])
        for (a_, ak) in ((ang, "ang"), (ang2, "ang2")):
            c.op(V, lambda e, a_=a_: e.tensor_scalar(out=tmpr, in0=a_, scalar1=float(1.0 / TWO_PI), scalar2=None, op0=ALU.mult), reads=[ak], writes=["tmpr"])
            c.op(V, lambda e: e.tensor_copy(out=posi, in_=tmpr), reads=["tmpr"], writes=["posi"])
            c.op(V, lambda e: e.tensor_copy(out=tmpr, in_=posi), reads=["posi"], writes=["tmpr"])
            c.op(V, lambda e, a_=a_: e.scalar_tensor_tensor(out=a_, in0=tmpr, scalar=float(-TWO_PI), in1=a_, op0=ALU.mult, op1=ALU.add),
                 reads=["tmpr", ak], writes=[ak])
            c.op(V, lambda e, a_=a_: e.tensor_scalar(out=tmpr, in0=a_, scalar1=float(math.pi), scalar2=float(-TWO_PI), op0=ALU.is_gt, op1=ALU.mult),
                 reads=[ak], writes=["tmpr"])
            c.op(V, lambda e, a_=a_: e.tensor_tensor(out=a_, in0=a_, in1=tmpr, op=ALU.add), reads=[ak, "tmpr"], writes=[ak])
        c.op(A, lambda e: e.activation(out=cdst, in_=ang2, func=AF.Sin), reads=["ang2"], writes=[ck])
        c.op(A, lambda e: e.activation(out=sdst, in_=ang, func=AF.Sin), reads=["ang"], writes=[sk])
        c.op(G_, lambda e: e.tensor_scalar(out=sdst[0:32, :], in0=sdst[0:32, :], scalar1=-1.0, scalar2=None, op0=ALU.mult), reads=[sk], writes=[sk])

    for i in range(8):
        tok = slice(i * 512, (i + 1) * 512)
        if src[0] == "f32":
            for hf in range(2):
                c.op("sync", lambda e, i=i, hf=hf: e.dma_start(out=xst, in_=src[1][:, :, i * 512 + hf * 256:i * 512 + (hf + 1) * 256]),
                     writes=["xst"], dkey="xst")
                c.op(G_, lambda e, hf=hf: e.tensor_copy(out=xob[:, :, hf * 256:(hf + 1) * 256], in_=xst), reads=["xst"], writes=["xob"])
        else:
            sap = src[1][i // 4]
            c.op("sync", lambda e, sap=sap, i=i: e.dma_start(out=xob, in_=sap[:, :, (i % 4) * 512:(i % 4 + 1) * 512]),
                 reads=[("ex_out", i // 4)], writes=["xob"], dkey="xob")
        rope_tables(pos_keys[tok], cosb, ssb, "cosb", "ssb")
        for cc in range(2):
            for kc in range(8):
                c.op(T, lambda e, cc=cc, kc=kc: e.matmul(ps[:, 3 + cc, :], lhsT=wlkv[:, kc, cc * 128:(cc + 1) * 128], rhs=xob[:, kc, :],
                                                         start=(kc == 0), stop=(kc == 7)),
                     reads=["wlkv", "xob"], writes=[("ps", 3 + cc)])
        for r in range(2):
            for kc in range(8):
                c.op(T, lambda e, r=r, kc=kc: e.matmul(ps[0:64, 5 + r, :], lhsT=wlkv[:, kc, 256 + r * 64:256 + (r + 1) * 64], rhs=xob[:, kc, :],
                                                       start=(kc == 0), stop=(kc == 7)),
                     reads=["wlkv", "xob"], writes=[("ps", 5 + r)])
        rms_norm([3, 4], 2, KV_RANK, kvg, ckvnT, tok, "ckvnT", "kvg")
        t1, t2 = ang, ang2
        c.op(V, lambda e: e.tensor_tensor(out=t1, in0=ps[0:64, 5, :], in1=cosb, op=ALU.mult), reads=[("ps", 5), "cosb"], writes=["ang"])
        c.op(V, lambda e: e.tensor_tensor(out=t2, in0=ps[0:64, 6, :], in1=ssb, op=ALU.mult), reads=[("ps", 6), "ssb"], writes=["ang2"])
        c.op(G_, lambda e: e.tensor_tensor(out=t1, in0=t1, in1=t2, op=ALU.add), reads=["ang", "ang2"], writes=["ang"])
        c.op(G_, lambda e, tok=tok: e.tensor_copy(out=krT[0:64, tok], in_=t1), reads=["ang"], writes=["krT"])
        c.op(A, lambda e: e.activation(out=sqr0, in_=t1, func=AF.Square), reads=["ang"], writes=["sqr0"])
        c.op(T, lambda e: e.matmul(ps[0:65, 7, :], lhsT=ones_bf[0:64, 0:65], rhs=sqr0, start=True, stop=True),
             reads=["sqr0", "ones_bf"], writes=[("ps", 7)])
        c.op(V, lambda e, i=i: e.tensor_reduce(out=kmr[64:65, i:i + 1], in_=ps[64:65, 7, :], axis=AX.X, op=ALU.max),
             reads=[("ps", 7)], writes=["kmr"])
    c.op(V, lambda e: e.tensor_reduce(out=kmr[64:65, 8:9], in_=kmr[64:65, 0:8], axis=AX.X, op=ALU.max), reads=["kmr"], writes=["kmr2"])
    for qt in range(4):
        tok = slice(qt * 512, (qt + 1) * 512)
        rope_tables(pos_own[tok], cos_own[:, tok], ss_own[:, tok], ("cos_own", qt), ("ss_own", qt))
        for cc in range(3):
            for kc in range(8):
                c.op(T, lambda e, cc=cc, kc=kc, tok=tok: e.matmul(ps[:, cc, :], lhsT=wlq[:, kc, cc * 128:(cc + 1) * 128], rhs=c.xbT[:, kc, tok],
                                                                  start=(kc == 0), stop=(kc == 7)),
                     reads=["wlq", ("xbT", qt)], writes=[("ps", cc)])
        rms_norm([0, 1, 2], 3, Q_RANK, qng, cqnT, tok, "cqnT", "qng")
    c.release(m1)

    wuq_st = c.alloc([3, 256], F32)
    wuq_b = c.alloc([3, 256], BF16)
    wukv_st = c.alloc([2, 256], F32)
    wukv_b = c.alloc([2, 256], BF16)
    wo_st = c.alloc([D], F32)
    wo_b = c.alloc([D], BF16)
    PT = [c.alloc([512], BF16) for _ in range(2)]
    onf = [c.alloc([128], F32) for _ in range(2)]
    rcp = c.alloc([4], F32)
    sqk = c.alloc([512], BF16)
    sqr = c.alloc([512], BF16, parts=64)
    qrf = c.alloc([512], F32, parts=64)
    q2 = c.alloc([512], F32, parts=64)
    kmx = c.alloc([16], F32)
    brow = c.alloc([512], F32)
    al = c.xbT.rearrange("p a b -> p (a b)")
    knT = al[:, 0:4096]
    Vaug = al[:, 4096:4096 + 32 * 129].rearrange("p (a b) -> p a b", a=32)
    o0 = 4096 + 32 * 129
    qnT = al[:, o0:o0 + 2048]
    qrT = al[:, o0 + 2048:o0 + 4096]
    oT = al[:, o0 + 4096:o0 + 6144]
    c.op(G_, lambda e: e.memset(Vaug[:, :, 128:129], 1.0), writes=["Vones"])

    for h in range(MLA_H):
        c.op("sync", lambda e, h=h: e.dma_start(out=wuq_st, in_=wuq_d[h]), writes=["wuq_st"], dkey="wuq_st")
        c.op("sync", lambda e, h=h: e.dma_start(out=wukv_st, in_=wukv_d[h]), writes=["wukv_st"], dkey="wukv_st")
        c.op("sync", lambda e, h=h: e.dma_start(out=wo_st, in_=wo_d[h]), writes=["wo_st"], dkey="wo_st")
        c.op(G_, lambda e: e.tensor_copy(out=wuq_b, in_=wuq_st), reads=["wuq_st"], writes=["wuq_b"])
        c.op(G_, lambda e: e.tensor_copy(out=wukv_b, in_=wukv_st), reads=["wukv_st"], writes=["wukv_b"])
        c.op(G_, lambda e: e.tensor_copy(out=wo_b, in_=wo_st), reads=["wo_st"], writes=["wo_b"])
        for i in range(8):
            tok = slice(i * 512, (i + 1) * 512)
            for kc in range(2):
                c.op(T, lambda e, kc=kc, tok=tok: e.matmul(ps[:, 6, :], lhsT=wukv_b[:, kc, 0:128], rhs=ckvnT[:, kc, tok], start=(kc == 0), stop=(kc == 1)),
                     reads=["wukv_b", "ckvnT"], writes=[("ps", 6)])
            c.op(A, lambda e, tok=tok: e.copy(out=knT[:, tok], in_=ps[:, 6, :]), reads=[("ps", 6)], writes=["knT"])
            c.op(A, lambda e: e.activation(out=sqk, in_=ps[:, 6, :], func=AF.Square), reads=[("ps", 6)], writes=["sqk"])
            c.op(T, lambda e: e.matmul(ps[0:65, 7, :], lhsT=ones_bf[:, 0:65], rhs=sqk, start=True, stop=True),
                 reads=["sqk", "ones_bf"], writes=[("ps", 7)])
            c.op(V, lambda e, i=i: e.tensor_reduce(out=kmx[64:65, i:i + 1], in_=ps[64:65, 7, :], axis=AX.X, op=ALU.max),
                 reads=[("ps", 7)], writes=["kmx"])
            for j4 in range(4):
                kch = i * 4 + j4
                ks = slice(kch * 128, (kch + 1) * 128)
                for kc in range(2):
                    c.op(T, lambda e, kc=kc, ks=ks, j4=j4: e.matmul(ps[:, 5, j4 * 128:(j4 + 1) * 128], lhsT=ckvnT[:, kc, ks], rhs=wukv_b[:, kc, 128:256],
                                                                    start=(kc == 0), stop=(kc == 1)),
                         reads=["wukv_b", "ckvnT"], writes=[("ps", 5)])
            c.op(V, lambda e, i=i: e.tensor_copy(out=Vaug[:, i * 4:(i + 1) * 4, 0:128], in_=ps[:, 5, :].rearrange("p (a b) -> p a b", a=4)),
                 reads=[("ps", 5)], writes=["Vaug"])
        c.op(V, lambda e: e.tensor_reduce(out=kmx[64:65, 9:10], in_=kmx[64:65, 0:8], axis=AX.X, op=ALU.max), reads=["kmx"], writes=["kmx1"])
        c.op(V, lambda e: e.tensor_tensor(out=kmx[64:65, 8:9], in0=kmx[64:65, 9:10], in1=kmr[64:65, 8:9], op=ALU.add), reads=["kmx1", "kmr2"], writes=["kmx2"])
        for qt in range(4):
            tok = slice(qt * 512, (qt + 1) * 512)
            for kc in range(3):
                c.op(T, lambda e, kc=kc, tok=tok: e.matmul(ps[:, 6, :], lhsT=wuq_b[:, kc, 0:128], rhs=cqnT[:, kc, tok], start=(kc == 0), stop=(kc == 2)),
                     reads=["wuq_b", "cqnT"], writes=[("ps", 6)])
            c.op(A, lambda e, tok=tok: e.copy(out=qnT[:, tok], in_=ps[:, 6, :]), reads=[("ps", 6)], writes=["qnT"])
            c.op(A, lambda e: e.activation(out=sqk, in_=ps[:, 6, :], func=AF.Square), reads=[("ps", 6)], writes=["sqk"])
            for r in range(2):
                for kc in range(3):
                    c.op(T, lambda e, kc=kc, tok=tok, r=r: e.matmul(ps[0:64, r, :], lhsT=wuq_b[:, kc, 128 + r * 64:192 + r * 64], rhs=cqnT[:, kc, tok],
                                                                    start=(kc == 0), stop=(kc == 2)),
                         reads=["wuq_b", "cqnT"], writes=[("ps", r)])
            c.op(V, lambda e, tok=tok: e.tensor_tensor(out=qrf, in0=ps[0:64, 0, :], in1=cos_own[:, tok], op=ALU.mult),
                 reads=[("ps", 0), ("cos_own", qt)], writes=["qrf"])
            c.op(V, lambda e, tok=tok: e.tensor_tensor(out=q2, in0=ps[0:64, 1, :], in1=ss_own[:, tok], op=ALU.mult),
                 reads=[("ps", 1), ("ss_own", qt)], writes=["q2"])
            c.op(G_, lambda e: e.tensor_tensor(out=qrf, in0=qrf, in1=q2, op=ALU.add), reads=["qrf", "q2"], writes=["qrf"])
            c.op(G_, lambda e, tok=tok: e.tensor_copy(out=qrT[0:64, tok], in_=qrf), reads=["qrf"], writes=["qrT"])
            c.op(A, lambda e: e.activation(out=sqr, in_=qrf, func=AF.Square), reads=["qrf"], writes=["sqr"])
            c.op(T, lambda e: e.matmul(ps[0:65, 7, :], lhsT=ones_bf[:, 0:65], rhs=sqk, start=True, stop=False),
                 reads=["sqk", "ones_bf"], writes=[("ps", 7)])
            c.op(T, lambda e: e.matmul(ps[0:65, 7, :], lhsT=ones_bf[0:64, 0:65], rhs=sqr, start=False, stop=True),
                 reads=["sqr", "ones_bf"], writes=[("ps", 7)])
            c.op(A, lambda e: e.activation(out=brow[64:65, :], in_=ps[64:65, 7, :], func=AF.Sqrt, scale=kmx[64:65, 8:9]),
                 reads=[("ps", 7), "kmx2"], writes=["brow"])
            c.op(V, lambda e, tok=tok: e.tensor_scalar(out=qrT[64:65, tok], in0=brow[64:65, :], scalar1=-1.0, scalar2=None, op0=ALU.mult),
                 reads=["brow"], writes=["qrT"])
        for qb in range(4):
            qs_ = slice(qb * 512, (qb + 1) * 512)
            def emit_scores(kc, qs_=qs_):
                sb_ = kc % 2
                ks = slice(kc * 128, (kc + 1) * 128)
                c.op(T, lambda e: e.matmul(ps[:, sb_, :], lhsT=knT[:, ks], rhs=qnT[:, qs_], start=True, stop=False),
                     reads=["knT", "qnT"], writes=[("ps", sb_)])
                c.op(T, lambda e: e.matmul(ps[:, sb_, :], lhsT=krT[0:65, ks], rhs=qrT[0:65, qs_], start=False, stop=True),
                     reads=["krT", "krT_ones", "qrT"], writes=[("ps", sb_)])

            emit_scores(0)
            for kc in range(32):
                sb_ = kc % 2
                if kc + 1 < 32:
                    emit_scores(kc + 1)
                c.op(A, lambda e, sb_=sb_: e.activation(out=PT[sb_], in_=ps[:, sb_, :], func=AF.Exp, scale=float(ATT_SCALE)),
                     reads=[("ps", sb_)], writes=[("PT", sb_)])
                for q4 in range(4):
                    c.op(T, lambda e, q4=q4, sb_=sb_, kc=kc: e.matmul(ps[:, 2 + q4, 0:129], lhsT=PT[sb_][:, q4 * 128:(q4 + 1) * 128], rhs=Vaug[:, kc, :],
                                                                      start=(kc == 0), stop=(kc == 31)),
                         reads=[("PT", sb_), "Vaug", "Vones"], writes=[("ps", 2 + q4)])
            for q4 in range(4):
                k2 = q4 % 2
                c.op(V, lambda e, q4=q4: e.reciprocal(out=rcp[:, q4:q4 + 1], in_=ps[:, 2 + q4, 128:129]), reads=[("ps", 2 + q4)], writes=[("rcp", q4)])
                c.op(V, lambda e, q4=q4, k2=k2: e.tensor_scalar(out=onf[k2], in0=ps[:, 2 + q4, 0:128], scalar1=rcp[:, q4:q4 + 1], scalar2=None, op0=ALU.mult),
                     reads=[("ps", 2 + q4), ("rcp", q4)], writes=[("onf", k2)])
                c.op(T, lambda e, q4=q4, k2=k2: e.transpose(out=ps[:, 6, q4 * 128:(q4 + 1) * 128], in_=onf[k2], identity=c.ident),
                     reads=[("onf", k2), "ident"], writes=[("ps", 6)])
            c.op(A, lambda e, qs_=qs_: e.copy(out=oT[:, qs_], in_=ps[:, 6, :]), reads=[("ps", 6)], writes=["oT"])
        for tile in range(16):
            for dh in range(2):
                c.op(T, lambda e, tile=tile, dh=dh: e.matmul(ps[:, 7, :], lhsT=oT[:, tile * 128:(tile + 1) * 128], rhs=wo_b[:, dh * 512:(dh + 1) * 512],
                                                             start=True, stop=True),
                     reads=["oT", "wo_b"], writes=[("ps", 7)])
                xs = c.x_tm[:, tile, dh * 512:(dh + 1) * 512]
                c.op(V, lambda e, xs=xs: e.tensor_tensor(out=xs, in0=ps[:, 7, :], in1=xs, op=ALU.add),
                     reads=[("ps", 7), ("x", tile)], writes=[("x", tile)])
    c.release(m0)


def lay_mla(w_dqkv, q_norm, w_uq, kv_norm, w_ukv, w_o):
    kc = lambda w: np.ascontiguousarray(w.reshape(-1, 128, w.shape[1]).transpose(1, 0, 2))
    wlq = kc(w_dqkv[:, :384])
    kr = w_dqkv[:, 640:704]
    kr_sw = np.concatenate([kr[:, 32:], kr[:, :32]], axis=1)
    wlkv = kc(np.concatenate([w_dqkv[:, 384:640], kr, kr_sw], axis=1))
    qng = np.ascontiguousarray(q_norm.reshape(3, 128).T)
    kvg = np.ascontiguousarray(kv_norm.reshape(2, 128).T)
    wuq = []
    for h in range(8):
        blk = w_uq[:, h * 192:(h + 1) * 192]
        r = blk[:, 128:]
        wuq.append(kc(np.concatenate([blk[:, :128], r, r[:, 32:], r[:, :32]], axis=1)))
    wukv = [kc(w_ukv[:, h * 256:(h + 1) * 256]) for h in range(8)]
    wo = np.ascontiguousarray(w_o.reshape(8, 128, 1024))
    return dict(wlq=wlq, wlkv=wlkv, qng=qng, kvg=kvg, wuq=np.stack(wuq), wukv=np.stack(wukv), wo=wo)


ML_H = 4
LN_KS = math.log(128 ** -0.5)
BIG = 1.0e30


def mlstm_phase(c, ex, cinfo, W):
    ps = c.ps
    V, G_, A, T = "vector", "gpsimd", "scalar", "tensor"
    m0 = c.mark()
    wgate = c.alloc([8, 16], BF16)
    bg = c.alloc([4], F32, parts=4)
    convw = c.alloc([8, 5], F32)
    id4 = c.ident[0:4, 0:4]
    rst = c.alloc([512], F32, parts=4)
    rstn = c.alloc([512], F32, parts=4)
    maskP = [c.alloc([128], F32) for _ in range(2)]
    colq = c.alloc([16, 2, 20], F32)
    cwB = c.alloc([2, 32, 4], F32)
    Cfin = c.alloc([2, 4, 257], F32)
    mfin = c.alloc([2], F32, parts=4)
    ones4 = c.alloc([128], F32, parts=4)
    st4 = c.alloc([8, 16], F32)
    c.op("sync", lambda e: e.dma_start(out=st4, in_=W["wgate"]), writes=["st4"], dkey="st4")
    c.op(G_, lambda e: e.tensor_copy(out=wgate, in_=st4), reads=["st4"], writes=["wgate"])
    c.op("sync", lambda e: e.dma_start(out=bg, in_=W["bgate"]), writes=["bg"], dkey="bg")
    c.op("sync", lambda e: e.dma_start(out=convw, in_=W["conv"]), writes=["convw"], dkey="convw")
    c.op(G_, lambda e: e.memset(ones4, 1.0), writes=["ones4"])
    c.op(G_, lambda e: e.memset(rst, 1.0), writes=["rst"])
    c.op(G_, lambda e: e.memset(rst.rearrange("p (a b) -> p a b", b=64)[:, :, 0:1], 0.0), reads=["rst"], writes=["rst"])
    c.op(G_, lambda e: e.memset(rstn, 0.0), writes=["rstn"])
    c.op(G_, lambda e: e.memset(rstn.rearrange("p (a b) -> p a b", b=64)[:, :, 0:1], -BIG), reads=["rstn"], writes=["rstn"])
    for d_ in range(2):
        mk = maskP[d_]
        c.op(G_, lambda e, mk=mk: e.memset(mk, BIG), writes=[("maskP", d_)])
        for hb in range(2):
            blk = mk[64 * hb:64 * hb + 64, 64 * hb:64 * hb + 64]
            c.op(G_, lambda e, blk=blk: e.memset(blk, 0.0), reads=[("maskP", d_)], writes=[("maskP", d_)])
            if d_ == 0:
                c.op(G_, lambda e, blk=blk: e.affine_select(out=blk, in_=blk, pattern=[[1, 64]], compare_op=ALU.is_ge, fill=BIG, base=0, channel_multiplier=-1),
                     reads=[("maskP", d_)], writes=[("maskP", d_)])
            else:
                c.op(G_, lambda e, blk=blk: e.affine_select(out=blk, in_=blk, pattern=[[-1, 64]], compare_op=ALU.is_ge, fill=BIG, base=0, channel_multiplier=1),
                     reads=[("maskP", d_)], writes=[("maskP", d_)])

    gm = c.mark()
    R = {n: c.alloc([512], F32, parts=4) for n in ("li", "xf", "t0", "t1", "b", "cc", "pm", "pm2", "aw")}
    mblk = c.alloc([16], F32, parts=4)
    uL = c.alloc([8], F32, parts=4)
    cwr = c.alloc([8], F32, parts=4)
    cwx = c.alloc([8, 4], F32, parts=4)
    awall = c.alloc([16, 4], F32)

    def v3(a):
        return a.rearrange("p (a b) -> p a b", b=64)

    def gate_rows(xb, xkey, d_, m_in, own, blk_i):
        for gsel, bank in ((d_, 6), (2 + d_, 7)):
            for kc in range(8):
                c.op(T, lambda e, kc=kc, gsel=gsel, bank=bank: e.matmul(ps[0:4, bank, :], lhsT=wgate[:, kc, 4 * gsel:4 * gsel + 4], rhs=xb[:, kc, :],
                                                                          start=(kc == 0), stop=(kc == 7)),
                     reads=["wgate", xkey], writes=[("ps", bank)])
        c.op(V, lambda e: e.tensor_scalar(out=R["li"], in0=ps[0:4, 6, :], scalar1=bg[:, d_:d_ + 1], scalar2=None, op0=ALU.add),
             reads=[("ps", 6), "bg"], writes=["r_li"])
        c.op(V, lambda e: e.tensor_scalar(out=R["xf"], in0=ps[0:4, 7, :], scalar1=bg[:, 2 + d_:3 + d_], scalar2=None, op0=ALU.add),
             reads=[("ps", 7), "bg"], writes=["r_xf"])
        c.op(V, lambda e: e.scalar_tensor_tensor(out=R["t0"], in0=R["xf"], scalar=-1.0, in1=R["xf"], op0=ALU.mult, op1=ALU.max), reads=["r_xf"], writes=["r_t0"])
        c.op(A, lambda e: e.activation(out=R["t0"], in_=R["t0"], func=AF.Exp, scale=-1.0), reads=["r_t0"], writes=["r_t0"])
        c.op(A, lambda e: e.activation(out=R["t0"], in_=R["t0"], func=AF.Ln, bias=1.0, scale=1.0), reads=["r_t0"], writes=["r_t0"])
        c.op(V, lambda e: e.tensor_scalar(out=R["t1"], in0=R["xf"], scalar1=0.0, scalar2=None, op0=ALU.min), reads=["r_xf"], writes=["r_t1"])
        c.op(V, lambda e: e.tensor_tensor(out=R["t1"], in0=R["t1"], in1=R["t0"], op=ALU.subtract), reads=["r_t1", "r_t0"], writes=["r_t1"])
        c.op(V, lambda e: e.tensor_tensor_scan(out=R["b"], data0=rst, data1=R["t1"], initial=0.0, op0=ALU.mult, op1=ALU.add),
             reads=["rst", "r_t1"], writes=["r_b"])
        if d_ == 1:
            c.op(V, lambda e: e.tensor_tensor(out=v3(R["t0"]), in0=v3(R["b"])[:, :, 63:64].to_broadcast([4, 8, 64]), in1=v3(R["b"]), op=ALU.subtract),
                 reads=["r_b"], writes=["r_t0"])
            c.op(V, lambda e: e.tensor_tensor(out=R["b"], in0=R["t0"], in1=R["t1"], op=ALU.add), reads=["r_t0", "r_t1", "r_b"], writes=["r_b"])
        c.op(V, lambda e: e.tensor_tensor(out=R["cc"], in0=R["li"], in1=R["b"], op=ALU.subtract), reads=["r_li", "r_b"], writes=["r_cc"])
        if d_ == 0:
            c.op(V, lambda e: e.tensor_tensor_scan(out=R["pm"], data0=rstn, data1=R["cc"], initial=-BIG, op0=ALU.add, op1=ALU.max),
                 reads=["rstn", "r_cc"], writes=["r_pm"])
        else:
            seq = [("cc", "pm"), ("pm", "pm2"), ("pm2", "pm"), ("pm", "pm2"), ("pm2", "pm"), ("pm", "pm2")]
            for k, (sn, dn) in zip((1, 2, 4, 8, 16, 32), seq):
                src, dst = R[sn], R[dn]
                c.op(V, lambda e, src=src, dst=dst, k=k: e.tensor_tensor(out=v3(dst)[:, :, 0:64 - k], in0=v3(src)[:, :, 0:64 - k], in1=v3(src)[:, :, k:64], op=ALU.max),
                     reads=["r_" + sn], writes=["r_" + dn])
                c.op(V, lambda e, src=src, dst=dst, k=k: e.tensor_copy(out=v3(dst)[:, :, 64 - k:64], in_=v3(src)[:, :, 64 - k:64]),
                     reads=["r_" + sn, "r_" + dn], writes=["r_" + dn])
            c.op(V, lambda e: e.tensor_copy(out=R["pm"], in_=R["pm2"]), reads=["r_pm2"], writes=["r_pm"])
        last = 63 if d_ == 0 else 0
        bL = v3(R["b"])[:, :, last]
        pmL = v3(R["pm"])[:, :, last]
        order = list(range(8)) if d_ == 0 else list(range(7, -1, -1))
        c.op(V, lambda e: e.tensor_copy(out=mblk[:, order[0]:order[0] + 1], in_=m_in), reads=["mcar", "r_b", "r_pm"], writes=["mblk"])
        for i, ch in enumerate(order):
            nxt = order[i + 1] if i < 7 else 8
            c.op(V, lambda e, ch=ch, nxt=nxt: e.scalar_tensor_tensor(out=mblk[:, nxt:nxt + 1], in0=mblk[:, ch:ch + 1], scalar=pmL[:, ch:ch + 1],
                                                                     in1=bL[:, ch:ch + 1], op0=ALU.max, op1=ALU.add),
                 reads=["mblk", "r_b", "r_pm"], writes=["mblk"])
        c.op(V, lambda e: e.tensor_tensor(out=uL, in0=mblk[:, 0:8], in1=pmL, op=ALU.max), reads=["mblk", "r_pm"], writes=["uL"])
        c.op(V, lambda e: e.tensor_tensor(out=cwr, in0=mblk[:, 0:8], in1=uL, op=ALU.subtract), reads=["mblk", "uL"], writes=["cwr"])
        c.op(A, lambda e: e.activation(out=cwr, in_=cwr, func=AF.Exp), reads=["cwr"], writes=["cwr"])
        c.op(V, lambda e: e.tensor_scalar(out=R["cc"], in0=R["cc"], scalar1=float(LN_KS), scalar2=None, op0=ALU.add), reads=["r_cc", "r_pm"], writes=["r_cc"])
        c.op(V, lambda e: e.tensor_tensor(out=v3(R["aw"]), in0=v3(R["cc"]), in1=uL.unsqueeze(2).to_broadcast([4, 8, 64]), op=ALU.subtract),
             reads=["r_cc", "uL"], writes=["r_aw"])
        c.op(A, lambda e: e.activation(out=R["aw"], in_=R["aw"], func=AF.Exp), reads=["r_aw"], writes=["r_aw"])
        c.op(V, lambda e: e.tensor_tensor(out=cwx, in0=cwr.unsqueeze(2).to_broadcast([4, 8, 4]), in1=id4.unsqueeze(1).to_broadcast([4, 8, 4]), op=ALU.mult),
             reads=["cwr", "ident"], writes=["cwx"])
        c.op(T, lambda e: e.matmul(ps[:, 6, 0:32], lhsT=ones4, rhs=cwx.rearrange("p a b -> p (a b)"), start=True, stop=True),
             reads=["ones4", "cwx"], writes=[("ps", 6)])
        c.op(V, lambda e: e.tensor_copy(out=cwB[:, d_, blk_i * 8:(blk_i + 1) * 8, :], in_=ps[:, 6, 0:32].rearrange("p (a b) -> p a b", b=4)),
             reads=[("ps", 6)], writes=[("cwB", d_)])
        if own:
            c.op(V, lambda e: e.tensor_tensor(out=v3(R["pm2"]), in0=v3(R["pm"]), in1=mblk[:, 0:8].unsqueeze(2).to_broadcast([4, 8, 64]), op=ALU.max),
                 reads=["r_pm", "mblk"], writes=["r_pm2"])
            c.op(V, lambda e: e.tensor_tensor(out=v3(R["t0"]), in0=mblk[:, 0:8].unsqueeze(2).to_broadcast([4, 8, 64]), in1=v3(R["pm2"]), op=ALU.subtract),
                 reads=["r_pm2", "mblk"], writes=["r_t0"])
            c.op(A, lambda e: e.activation(out=R["t0"], in_=R["t0"], func=AF.Exp), reads=["r_t0"], writes=["r_t0"])
            c.op(V, lambda e: e.tensor_tensor(out=R["t1"], in0=R["b"], in1=R["pm2"], op=ALU.add), reads=["r_b", "r_pm2"], writes=["r_t1"])
            c.op(A, lambda e: e.activation(out=R["t1"], in_=R["t1"], func=AF.Exp, scale=-1.0), reads=["r_t1"], writes=["r_t1"])
            qs = (("cc", "r_cc"), ("t0", "r_t0"), ("t1", "r_t1"), ("aw", "r_aw"), ("pm2", "r_pm2"))
        else:
            qs = (("aw", "r_aw"),)
        for t4 in range(4):
            tile = blk_i * 4 + t4
            ts = slice(t4 * 128, (t4 + 1) * 128)
            for qi, (rn, rk) in enumerate(qs):
                c.op(T, lambda e, rn=rn, ts=ts, qi=qi: e.matmul(ps[:, 5, qi * 4:(qi + 1) * 4], lhsT=R[rn][:, ts], rhs=id4, start=True, stop=True),
                     reads=[rk, "ident"], writes=[("ps", 5)])
            if own:
                c.op(V, lambda e, tile=tile: e.tensor_copy(out=colq[:, tile, d_, :], in_=ps[:, 5, 0:20]), reads=[("ps", 5)], writes=["colq"])
            else:
                c.op(V, lambda e, tile=tile: e.tensor_copy(out=awall[:, tile, :], in_=ps[:, 5, 0:4]), reads=[("ps", 5)], writes=["awall"])

    def conv_silu(pre, prekey, ci, n, dst_f, dkey):
        c.op(V, lambda e: e.tensor_scalar(out=dst_f, in0=pre[:, 0:n], scalar1=convw[:, ci, 0:1], scalar2=None, op0=ALU.mult),
             reads=[prekey, "convw"], writes=[dkey])
        for j in range(1, 5):
            c.op(V, lambda e, j=j: e.scalar_tensor_tensor(out=dst_f, in0=pre[:, j:n + j], scalar=convw[:, ci, j:j + 1], in1=dst_f, op0=ALU.mult, op1=ALU.add),
                 reads=[prekey, "convw", dkey], writes=[dkey])
        c.op(A, lambda e: e.activation(out=dst_f, in_=dst_f, func=AF.Silu), reads=[dkey], writes=[dkey])

    p1 = c.mark()
    xst_p1 = c.alloc([8, 171], F32)
    xob_p1 = c.alloc([8, NT + 4], BF16)
    wk_b_p1 = c.alloc([8, 128], BF16)
    wv_b_p1 = c.alloc([8, 256], BF16)
    pre_p1 = c.alloc([516], F32)
    kf_p1 = c.alloc([512], F32)
    ktm_p1 = c.alloc([4, 128], BF16)
    vau_p1 = c.alloc([4, 258], BF16)
    Cst_p1 = c.alloc([257], F32)
    zcol_p1 = c.alloc([1], F32, parts=4)
    c.op(G_, lambda e: e.memset(zcol_p1, 0.0), writes=["zcol"])
    c.op(G_, lambda e: e.memset(vau_p1[:, :, 256:257], 1.0), writes=["vau1"])
    wstg_p1 = xst_p1[:, :, 0:128]
    for d_ in range(2):
        if d_ == 0:
            c.op(G_, lambda e: e.memset(xob_p1[:, :, 0:2], 0.0), writes=["xob"])
            c.op("sync", lambda e: e.dma_start(out=xob_p1[:, :, 2:NT + 2], in_=ex[0]), reads=[("ex_out", 0)], writes=["xob"], dkey="xob")
            c.op("sync", lambda e: e.dma_start(out=xob_p1[:, :, NT + 2:NT + 4], in_=ex[1][:, :, 0:2]), reads=[("ex_out", 1)], writes=["xob"], dkey="xob")
        else:
            c.op(G_, lambda e: e.memset(xob_p1[:, :, NT + 2:NT + 4], 0.0), writes=["xob"])
            c.op("sync", lambda e: e.dma_start(out=xob_p1[:, :, 2:NT + 2], in_=ex[1]), reads=[("ex_out", 1)], writes=["xob"], dkey="xob")
            c.op("sync", lambda e: e.dma_start(out=xob_p1[:, :, 0:2], in_=ex[0][:, :, NT - 2:NT]), reads=[("ex_out", 0)], writes=["xob"], dkey="xob")
        c.op(V, lambda e, d_=d_: e.tensor_copy(out=mfin[:, d_:d_ + 1], in_=zcol_p1), reads=["zcol"], writes=["mcar"])
        blocks = list(range(4)) if d_ == 0 else list(range(3, -1, -1))
        for bi in blocks:
            gate_rows(xob_p1[:, :, 2 + bi * 512:2 + (bi + 1) * 512], "xob", d_, mfin[:, d_:d_ + 1], False, bi)
            c.op(V, lambda e, d_=d_: e.tensor_copy(out=mfin[:, d_:d_ + 1], in_=mblk[:, 8:9]), reads=["mblk"], writes=["mcar"])
        for h in range(ML_H):
            c.op("sync", lambda e, h=h: e.dma_start(out=wstg_p1, in_=W["wk"][h]), writes=["xst"], dkey="xst")
            c.op(G_, lambda e: e.tensor_copy(out=wk_b_p1, in_=wstg_p1), reads=["xst"], writes=["wk_b"])
            for hf in range(2):
                c.op("sync", lambda e, h=h, hf=hf: e.dma_start(out=wstg_p1, in_=W["wv"][h, :, :, hf * 128:(hf + 1) * 128]), writes=["xst"], dkey="xst")
                c.op(G_, lambda e, hf=hf: e.tensor_copy(out=wv_b_p1[:, :, hf * 128:(hf + 1) * 128], in_=wstg_p1), reads=["xst"], writes=["wv_b"])
            c.op(G_, lambda e: e.memset(Cst_p1, 0.0), writes=["Cst"])
            for bi in blocks:
                x0 = bi * 512
                for (bank, c0, n) in ((0, 0, 512), (1, 512, 4)):
                    for kc in range(8):
                        c.op(T, lambda e, kc=kc, bank=bank, c0=c0, n=n, x0=x0: e.matmul(ps[:, bank, 0:n], lhsT=wk_b_p1[:, kc, :], rhs=xob_p1[:, kc, x0 + c0:x0 + c0 + n],
                                                                                      start=(kc == 0), stop=(kc == 7)),
                             reads=["wk_b", "xob"], writes=[("ps", bank)])
                    c.op(A, lambda e, bank=bank, c0=c0, n=n: e.copy(out=pre_p1[:, c0:c0 + n], in_=ps[:, bank, 0:n]), reads=[("ps", bank)], writes=["pre"])
                conv_silu(pre_p1, "pre", 4 + h, 512, kf_p1, "kf")
                for t4 in range(4):
                    c.op(T, lambda e, t4=t4: e.transpose(out=ps[:, 2, t4 * 128:(t4 + 1) * 128], in_=kf_p1[:, t4 * 128:(t4 + 1) * 128], identity=c.ident),
                         reads=["kf", "ident"], writes=[("ps", 2)])
                c.op(V, lambda e, h=h, bi=bi: e.tensor_tensor(out=ktm_p1, in0=ps[:, 2, :].rearrange("p (a b) -> p a b", b=128),
                                                              in1=awall[:, bi * 4:(bi + 1) * 4, h:h + 1].to_broadcast([128, 4, 128]), op=ALU.mult),
                     reads=[("ps", 2), "awall"], writes=["ktm"])
                for t4 in range(4):
                    for kc in range(8):
                        c.op(T, lambda e, kc=kc, t4=t4, x0=x0: e.matmul(ps[:, 3, 0:256], lhsT=xob_p1[:, kc, 2 + x0 + t4 * 128:2 + x0 + (t4 + 1) * 128], rhs=wv_b_p1[:, kc, :],
                                                                        start=(kc == 0), stop=(kc == 7)),
                             reads=["wv_b", "xob"], writes=[("ps", 3)])
                    c.op(A, lambda e, t4=t4: e.copy(out=vau_p1[:, t4, 0:256], in_=ps[:, 3, 0:256]), reads=[("ps", 3)], writes=["vau"])
                chunks = list(range(8)) if d_ == 0 else list(range(7, -1, -1))
                for ch in chunks:
                    t4, hb = ch // 2, ch % 2
                    rs = slice(64 * hb, 64 * hb + 64)
                    c.op(T, lambda e, t4=t4, rs=rs: e.matmul(ps[:, 4, 0:257], lhsT=ktm_p1[rs, t4, :], rhs=vau_p1[rs, t4, 0:257], start=True, stop=True),
                         reads=["ktm", "vau", "vau1"], writes=[("ps", 4)])
                    c.op(V, lambda e, h=h, ch=ch, bi=bi, d_=d_: e.scalar_tensor_tensor(out=Cst_p1, in0=Cst_p1, scalar=cwB[:, d_, bi * 8 + ch, h:h + 1],
                                                                                      in1=ps[:, 4, 0:257], op0=ALU.mult, op1=ALU.add),
                         reads=["Cst", ("cwB", d_), ("ps", 4)], writes=["Cst"])
            c.op(V, lambda e, d_=d_, h=h: e.tensor_scalar(out=Cfin[:, d_, h, :], in0=Cst_p1, scalar1=cinfo[:, 1 + d_:2 + d_], scalar2=None, op0=ALU.mult),
                 reads=["Cst", "cinfo"], writes=[("Cfin", d_)])
        c.op(V, lambda e, d_=d_: e.tensor_scalar(out=mfin[:, d_:d_ + 1], in0=mfin[:, d_:d_ + 1], scalar1=cinfo[0:4, 1 + d_:2 + d_], scalar2=None, op0=ALU.mult),
             reads=["mcar", "cinfo"], writes=["mcar"])
    c.release(p1)

    mcar2 = c.alloc([2], F32, parts=4)
    for d_ in range(2):
        c.op(V, lambda e, d_=d_: e.tensor_copy(out=mcar2[:, d_:d_ + 1], in_=mfin[:, d_:d_ + 1]), reads=["mcar"], writes=["mcar"])
        blocks = list(range(4)) if d_ == 0 else list(range(3, -1, -1))
        for bi in blocks:
            gate_rows(c.xbT[:, :, bi * 512:(bi + 1) * 512], ("xbT", bi), d_, mcar2[:, d_:d_ + 1], True, bi)
            c.op(V, lambda e, d_=d_: e.tensor_copy(out=mcar2[:, d_:d_ + 1], in_=mblk[:, 8:9]), reads=["mblk"], writes=["mcar"])
    c.release(gm)

    wst = c.alloc([8, 128], F32)
    wq_b = c.alloc([8, 128], BF16)
    wk_b = c.alloc([8, 128], BF16)
    wv_b = c.alloc([8, 256], BF16)
    wout_b = c.alloc([2, D], BF16)
    xh_b = c.alloc([8, 4], BF16)
    pre = c.alloc([516], F32)
    kf = c.alloc([512], F32)
    qT = c.alloc([NT], BF16)
    kT = c.alloc([NT], BF16)
    ktm = c.alloc([16, 128], BF16)
    kaw = c.alloc([128], BF16)
    vau = c.alloc([16, 258], BF16)
    hacc = c.alloc([16, 256], F32)
    dg = [c.alloc([128], F32) for _ in range(2)]
    Ub = [c.alloc([128], F32) for _ in range(2)]
    DwT = [c.alloc([128], F32) for _ in range(2)]
    SDT = [c.alloc([128], BF16) for _ in range(2)]
    hB = [c.alloc([257], F32) for _ in range(2)]
    hN = c.alloc([257], F32)
    Cf = c.alloc([257], F32)
    Cb = [c.alloc([258], BF16) for _ in range(2)]
    dcol = c.alloc([4], F32)
    ng = c.alloc([256], F32)
    ysb = c.alloc([256], F32)
    og = c.alloc([256], F32)
    yT = c.alloc([2, 128], BF16)
    c.op("sync", lambda e: e.dma_start(out=xh_b[:, :, 0:2], in_=ex[0][:, :, NT - 2:NT]), reads=[("ex_out", 0)], writes=["xh_b"], dkey="xh_b")
    c.op("sync", lambda e: e.dma_start(out=xh_b[:, :, 2:4], in_=ex[1][:, :, 0:2]), reads=[("ex_out", 1)], writes=["xh_b"], dkey="xh_b")
    c.op(G_, lambda e: e.tensor_scalar(out=xh_b[:, :, 0:2], in0=xh_b[:, :, 0:2], scalar1=cinfo[:, 1:2], scalar2=None, op0=ALU.mult), reads=["xh_b", "cinfo"], writes=["xh_b"])
    c.op(G_, lambda e: e.tensor_scalar(out=xh_b[:, :, 2:4], in0=xh_b[:, :, 2:4], scalar1=cinfo[:, 2:3], scalar2=None, op0=ALU.mult), reads=["xh_b", "cinfo"], writes=["xh_b"])
    c.op(G_, lambda e: e.memset(vau[:, :, 256:257], 1.0), writes=["vau1"])

    def load_w(src, dst, n, key):
        for hf in range(n // 128):
            c.op("sync", lambda e, hf=hf: e.dma_start(out=wst, in_=src[:, :, hf * 128:(hf + 1) * 128]), writes=["wst"], dkey="wst")
            c.op(G_, lambda e, hf=hf: e.tensor_copy(out=dst[:, :, hf * 128:(hf + 1) * 128], in_=wst), reads=["wst"], writes=[key])

    def proj_fm(wb, wkey, ci, dst_bf, dkey, tm):
        for qt in range(4):
            lo = qt * 512 - 2
            for kc in range(8):
                c.op(T, lambda e, kc=kc, qt=qt: e.matmul(ps[:, 0, :], lhsT=wb[:, kc, :], rhs=c.xbT[:, kc, qt * 512:(qt + 1) * 512], start=(kc == 0), stop=(kc == 7)),
                     reads=[wkey, ("xbT", qt)], writes=[("ps", 0)])
            c.op(A, lambda e: e.copy(out=pre[:, 2:514], in_=ps[:, 0, :]), reads=[("ps", 0)], writes=["pre"])
            for side, (pc, tok0) in enumerate(((0, lo), (514, lo + 514))):
                if tok0 < 0 or tok0 >= NT:
                    rhs = xh_b[:, :, 0:2] if tok0 < 0 else xh_b[:, :, 2:4]
                    rk = "xh_b"
                else:
                    rhs = c.xbT[:, :, tok0:tok0 + 2]
                    rk = ("xbT", tok0 // 512)
                for kc in range(8):
                    c.op(T, lambda e, kc=kc, rhs=rhs, side=side: e.matmul(ps[:, 1, side * 2:side * 2 + 2], lhsT=wb[:, kc, :], rhs=rhs[:, kc, :], start=(kc == 0), stop=(kc == 7)),
                         reads=[wkey, rk], writes=[("ps", 1)])
            c.op(A, lambda e: e.copy(out=pre[:, 0:2], in_=ps[:, 1, 0:2]), reads=[("ps", 1)], writes=["pre"])
            c.op(A, lambda e: e.copy(out=pre[:, 514:516], in_=ps[:, 1, 2:4]), reads=[("ps", 1)], writes=["pre"])
            conv_silu(pre, "pre", ci, 512, kf, "kf")
            c.op(G_, lambda e, qt=qt: e.tensor_copy(out=dst_bf[:, qt * 512:(qt + 1) * 512], in_=kf), reads=["kf"], writes=[dkey])
            if tm:
                for t4 in range(4):
                    c.op(T, lambda e, t4=t4: e.transpose(out=ps[:, 2, t4 * 128:(t4 + 1) * 128], in_=kf[:, t4 * 128:(t4 + 1) * 128], identity=c.ident),
                         reads=["kf", "ident"], writes=[("ps", 2)])
                c.op(A, lambda e, qt=qt: e.copy(out=ktm[:, qt * 4:(qt + 1) * 4, :], in_=ps[:, 2, :].rearrange("p (a b) -> p a b", b=128)),
                     reads=[("ps", 2)], writes=["ktm"])

    for h in range(ML_H):
        load_w(W["wq"][h], wq_b, 128, "wq")
        load_w(W["wk"][h], wk_b, 128, "wk")
        load_w(W["wv"][h], wv_b, 256, "wv")
        for cc_ in range(2):
            for q8 in range(8):
                c.op("sync", lambda e, h=h, cc_=cc_, q8=q8: e.dma_start(out=wst[:, 0, :], in_=W["wout"][h, :, cc_, q8 * 128:(q8 + 1) * 128]), writes=["wst"], dkey="wst")
                c.op(G_, lambda e, cc_=cc_, q8=q8: e.tensor_copy(out=wout_b[:, cc_, q8 * 128:(q8 + 1) * 128], in_=wst[:, 0, :]), reads=["wst"], writes=["wout"])
        c.op("sync", lambda e, h=h: e.dma_start(out=ng, in_=W["normg"][h * 256:(h + 1) * 256].partition_broadcast(128)), writes=["ng"], dkey="ng")
        proj_fm(wq_b, "wq", h, qT, "qT", False)
        proj_fm(wk_b, "wk", 4 + h, kT, "kT", True)
        for t4 in range(16):
            ts = slice(t4 * 128, (t4 + 1) * 128)
            for kc in range(8):
                c.op(T, lambda e, kc=kc, ts=ts: e.matmul(ps[:, 3, 0:256], lhsT=c.xbT[:, kc, ts], rhs=wv_b[:, kc, :], start=(kc == 0), stop=(kc == 7)),
                     reads=["wv", ("xbT", t4 // 4)], writes=[("ps", 3)])
            c.op(A, lambda e, t4=t4: e.copy(out=vau[:, t4, 0:256], in_=ps[:, 3, 0:256]), reads=[("ps", 3)], writes=["vau"])
        load_w(W["wog"][h], wv_b, 256, "wv")
        for d_ in range(2):
            c.op(V, lambda e, d_=d_, h=h: e.tensor_copy(out=Cf, in_=Cfin[:, d_, h, :]), reads=[("Cfin", d_)], writes=["Cf"])
            c.op(A, lambda e: e.copy(out=Cb[0][:, 0:257], in_=Cf), reads=["Cf"], writes=[("Cb", 0)])
            tiles = list(range(16)) if d_ == 0 else list(range(15, -1, -1))
            cbi = 0
            for ti, tile in enumerate(tiles):
                ts = slice(tile * 128, (tile + 1) * 128)
                k2 = ti % 2
                ccol = lambda qi, tile=tile, d_=d_, h=h: colq[:, tile, d_, qi * 4 + h:qi * 4 + h + 1]
                c.op(V, lambda e, k2=k2, ccol=ccol: e.tensor_scalar(out=dg[k2], in0=c.ident, scalar1=ccol(4), scalar2=None, op0=ALU.mult),
                     reads=["ident", "colq"], writes=[("dg", k2)])
                c.op(T, lambda e, k2=k2: e.matmul(ps[:, 4 + k2, 0:128], lhsT=c.ones, rhs=dg[k2], start=True, stop=True),
                     reads=["ones", ("dg", k2)], writes=[("ps", 4 + k2)])
                c.op(V, lambda e, k2=k2, d_=d_: e.tensor_tensor(out=Ub[k2], in0=ps[:, 4 + k2, 0:128], in1=maskP[d_], op=ALU.add),
                     reads=[("ps", 4 + k2), ("maskP", d_)], writes=[("Ub", k2)])
                c.op(T, lambda e, ts=ts, k2=k2: e.matmul(ps[:, k2, 0:128], lhsT=kT[:, ts], rhs=qT[:, ts], start=True, stop=True),
                     reads=["kT", "qT"], writes=[("ps", k2)])
                c.op(A, lambda e, k2=k2, ccol=ccol: e.activation(out=DwT[k2], in_=Ub[k2], func=AF.Exp, scale=-1.0, bias=ccol(0)),
                     reads=[("Ub", k2), "colq"], writes=[("DwT", k2)])
                c.op(V, lambda e, k2=k2: e.tensor_tensor(out=SDT[k2], in0=ps[:, k2, 0:128], in1=DwT[k2], op=ALU.mult),
                     reads=[("ps", k2), ("DwT", k2)], writes=[("SDT", k2)])
                c.op(T, lambda e, k2=k2, tile=tile: e.matmul(ps[:, 2 + k2, 0:257], lhsT=SDT[k2], rhs=vau[:, tile, 0:257], start=True, stop=True),
                     reads=[("SDT", k2), "vau", "vau1"], writes=[("ps", 2 + k2)])
                c.op(A, lambda e, k2=k2: e.copy(out=hB[k2], in_=ps[:, 2 + k2, 0:257]), reads=[("ps", 2 + k2)], writes=[("hB", k2)])
                c.op(V, lambda e, tile=tile, ccol=ccol: e.tensor_scalar(out=kaw, in0=ktm[:, tile, :], scalar1=ccol(3), scalar2=None, op0=ALU.mult),
                     reads=["ktm", "colq"], writes=["kaw"])
                for hb in ((0, 1) if d_ == 0 else (1, 0)):
                    rs = slice(64 * hb, 64 * hb + 64)
                    ch = tile * 2 + hb
                    cur = Cb[cbi % 2]
                    c.op(T, lambda e, ts=ts, cur=cur: e.matmul(ps[:, 6, 0:257], lhsT=qT[:, ts], rhs=cur[:, 0:257], start=True, stop=True),
                         reads=["qT", ("Cb", cbi % 2)], writes=[("ps", 6)])
                    c.op(V, lambda e, rs=rs, k2=k2, ccol=ccol: e.scalar_tensor_tensor(out=hN[rs, :], in0=ps[rs, 6, 0:257], scalar=ccol(1)[rs, :], in1=hB[k2][rs, :],
                                                                                     op0=ALU.mult, op1=ALU.add),
                         reads=[("ps", 6), "colq", ("hB", k2)], writes=["hN"])
                    c.op(T, lambda e, rs=rs, tile=tile: e.matmul(ps[:, 7, 0:257], lhsT=kaw[rs, :], rhs=vau[rs, tile, 0:257], start=True, stop=True),
                         reads=["kaw", "vau", "vau1"], writes=[("ps", 7)])
                    c.op(V, lambda e, ch=ch, d_=d_, h=h: e.scalar_tensor_tensor(out=Cf, in0=Cf, scalar=cwB[:, d_, ch, h:h + 1], in1=ps[:, 7, 0:257], op0=ALU.mult, op1=ALU.add),
                         reads=["Cf", ("cwB", d_), ("ps", 7)], writes=["Cf"])
                    cbi += 1
                    nxt = Cb[cbi % 2]
                    c.op(A, lambda e, nxt=nxt: e.copy(out=nxt[:, 0:257], in_=Cf), reads=["Cf"], writes=[("Cb", cbi % 2)])
                c.op(V, lambda e: e.scalar_tensor_tensor(out=dcol[:, 0:1], in0=hN[:, 256:257], scalar=-1.0, in1=hN[:, 256:257], op0=ALU.mult, op1=ALU.max), reads=["hN"], writes=["dcol"])
                c.op(V, lambda e, ccol=ccol: e.tensor_tensor(out=dcol[:, 0:1], in0=dcol[:, 0:1], in1=ccol(2), op=ALU.max), reads=["dcol", "colq"], writes=["dcol"])
                c.op(V, lambda e: e.reciprocal(out=dcol[:, 1:2], in_=dcol[:, 0:1]), reads=["dcol"], writes=["dcol"])
                if d_ == 0:
                    c.op(V, lambda e, tile=tile: e.tensor_scalar(out=hacc[:, tile, :], in0=hN[:, 0:256], scalar1=dcol[:, 1:2], scalar2=None, op0=ALU.mult),
                         reads=["hN", "dcol"], writes=[("hacc", tile)])
                else:
                    c.op(V, lambda e, tile=tile: e.scalar_tensor_tensor(out=hacc[:, tile, :], in0=hN[:, 0:256], scalar=dcol[:, 1:2], in1=hacc[:, tile, :], op0=ALU.mult, op1=ALU.add),
                         reads=["hN", "dcol", ("hacc", tile)], writes=[("hacc", tile)])
        for tile in range(16):
            ha = hacc[:, tile, :]
            hk = ("hacc", tile)
            ts = slice(tile * 128, (tile + 1) * 128)
            for kc in range(8):
                c.op(T, lambda e, kc=kc, ts=ts: e.matmul(ps[:, 4, 0:256], lhsT=c.xbT[:, kc, ts], rhs=wv_b[:, kc, :], start=(kc == 0), stop=(kc == 7)),
                     reads=["wv", ("xbT", tile // 4)], writes=[("ps", 4)])
            c.op(A, lambda e: e.activation(out=og, in_=ps[:, 4, 0:256], func=AF.Sigmoid), reads=[("ps", 4)], writes=["og"])
            c.op(A, lambda e, ha=ha: e.activation(out=ysb, in_=ha, func=AF.Identity, accum_out=dcol[:, 0:1]), reads=[hk, "dcol"], writes=["ysb", "dcol"])
            c.op(A, lambda e, ha=ha: e.activation(out=ysb, in_=ha, func=AF.Square, accum_out=dcol[:, 1:2]), reads=[hk, "dcol"], writes=["ysb", "dcol"])
            c.op(V, lambda e: e.tensor_scalar(out=dcol[:, 0:2], in0=dcol[:, 0:2], scalar1=1.0 / 256, scalar2=None, op0=ALU.mult), reads=["dcol"], writes=["dcol"])
            c.op(V, lambda e: e.tensor_tensor(out=dcol[:, 2:3], in0=dcol[:, 0:1], in1=dcol[:, 0:1], op=ALU.mult), reads=["dcol"], writes=["dcol"])
            c.op(V, lambda e: e.tensor_tensor(out=dcol[:, 2:3], in0=dcol[:, 1:2], in1=dcol[:, 2:3], op=ALU.subtract), reads=["dcol"], writes=["dcol"])
            c.op(A, lambda e: e.activation(out=dcol[:, 2:3], in_=dcol[:, 2:3], func=AF.Sqrt, bias=float(LN_EPS), scale=1.0), reads=["dcol"], writes=["dcol"])
            c.op(V, lambda e: e.reciprocal(out=dcol[:, 2:3], in_=dcol[:, 2:3]), reads=["dcol"], writes=["dcol"])
            c.op(V, lambda e: e.scalar_tensor_tensor(out=dcol[:, 3:4], in0=dcol[:, 0:1], scalar=-1.0, in1=dcol[:, 2:3], op0=ALU.mult, op1=ALU.mult), reads=["dcol"], writes=["dcol"])
            c.op(A, lambda e, ha=ha: e.activation(out=ysb, in_=ha, func=AF.Identity, scale=dcol[:, 2:3], bias=dcol[:, 3:4]), reads=[hk, "dcol"], writes=["ysb"])
            c.op(V, lambda e: e.tensor_tensor(out=ysb, in0=ysb, in1=ng, op=ALU.mult), reads=["ysb", "ng"], writes=["ysb"])
            c.op(V, lambda e: e.tensor_tensor(out=ysb, in0=ysb, in1=og, op=ALU.mult), reads=["ysb", "og"], writes=["ysb"])
            for cc_ in range(2):
                c.op(T, lambda e, cc_=cc_: e.transpose(out=ps[:, 0, cc_ * 128:(cc_ + 1) * 128], in_=ysb[:, cc_ * 128:(cc_ + 1) * 128], identity=c.ident),
                     reads=["ysb", "ident"], writes=[("ps", 0)])
            c.op(A, lambda e: e.copy(out=yT, in_=ps[:, 0, 0:256].rearrange("p (a b) -> p a b", b=128)), reads=[("ps", 0)], writes=["yT"])
            for dh in range(2):
                for cc_ in range(2):
                    c.op(T, lambda e, cc_=cc_, dh=dh: e.matmul(ps[:, 1, :], lhsT=yT[:, cc_, :], rhs=wout_b[:, cc_, dh * 512:(dh + 1) * 512], start=(cc_ == 0), stop=(cc_ == 1)),
                         reads=["yT", "wout"], writes=[("ps", 1)])
                xs = c.x_tm[:, tile, dh * 512:(dh + 1) * 512]
                c.op(V, lambda e, xs=xs: e.tensor_tensor(out=xs, in0=ps[:, 1, :], in1=xs, op=ALU.add), reads=[("ps", 1), ("x", tile)], writes=[("x", tile)])
    c.release(m0)


def lay_mlstm(w_in, conv, b_gate, norm_g, w_out):
    kc = lambda w: np.ascontiguousarray(w.reshape(-1, 128, w.shape[1]).transpose(1, 0, 2))
    wq = np.stack([kc(w_in[:, h * 128:(h + 1) * 128]) for h in range(4)])
    wk = np.stack([kc(w_in[:, 512 + h * 128:512 + (h + 1) * 128]) for h in range(4)])
    wv = np.stack([kc(w_in[:, 1024 + h * 256:1024 + (h + 1) * 256]) for h in range(4)])
    wog = np.stack([kc(w_in[:, 2048 + h * 256:2048 + (h + 1) * 256]) for h in range(4)])
    wgate = kc(w_in[:, 3072:3088])
    bgate = np.ascontiguousarray(b_gate.reshape(4, 4).T)
    convl = np.ascontiguousarray(conv.T.reshape(8, 128, 5).transpose(1, 0, 2))
    wout = np.ascontiguousarray(w_out.reshape(4, 2, 128, 1024).transpose(0, 2, 1, 3))
    return dict(wq=wq, wk=wk, wv=wv, wog=wog, wgate=wgate, bgate=bgate, conv=convl, normg=np.ascontiguousarray(norm_g), wout=wout)


G_DENSE = 2
G_MOE = 2


def _fm(a):
    return np.ascontiguousarray(a.T.reshape(8, 128, a.shape[0]).transpose(1, 0, 2))


def layer_weights(i, inp):
    w = {}
    mix = i % 3
    if mix == 0:
        j = i // 3
        L = lay_mla(inp["mla_w_dqkv"][j], inp["mla_q_norm"][j], inp["mla_w_uq"][j], inp["mla_kv_norm"][j], inp["mla_w_ukv"][j], inp["mla_w_o"][j])
        w.update({"mla_" + k: v for k, v in L.items()})
    elif mix == 1:
        j = i // 3
        w["pool_w"] = np.ascontiguousarray(inp["pool_w"][j].reshape(4, 2, 128, 256).transpose(0, 2, 1, 3))
        w["pool_scale"] = np.ascontiguousarray(inp["pool_scale"][j])
    else:
        j = i // 3
        L = lay_mlstm(inp["mlstm_w_in"][j], inp["mlstm_conv"][j], inp["mlstm_b_gate"][j], inp["mlstm_norm"][j], inp["mlstm_w_out"][j])
        w.update({"ml_" + k: v for k, v in L.items()})
    cidx = i // 2
    if i % 2 == 0:
        w["wg"] = lay_gu(inp["ffn_w_gate"][cidx], G_DENSE)[None]
        w["wu"] = lay_gu(inp["ffn_w_up"][cidx], G_DENSE)[None]
        w["wd"] = lay_d(inp["ffn_w_down"][cidx], G_DENSE)[None]
    else:
        w["wg"] = np.stack([lay_gu(inp["moe_w_gate"][cidx, e], G_MOE) for e in range(NE)])
        w["wu"] = np.stack([lay_gu(inp["moe_w_up"][cidx, e], G_MOE) for e in range(NE)])
        w["wd"] = np.stack([lay_d(inp["moe_w_down"][cidx, e], G_MOE) for e in range(NE)])
        w["wr"] = np.ascontiguousarray(inp["moe_router"][cidx].reshape(8, 128, NE).transpose(1, 0, 2))
    w["ln_g"] = np.ascontiguousarray(inp["ln_g"][i])
    w["ln_b"] = np.ascontiguousarray(inp["ln_b"][i])
    return w


PAIRS = [[0, 1], [2, 3], [4, 5], [6, 7]]


def exchange_x(c, exin, exout, cinfo):
    m = c.mark()
    tmp = [c.alloc([8, 512], BF16) for _ in range(2)]
    k = 0
    for h in range(2):
        fl = c_flag(cinfo, h)
        v_in = exin[h].ap().rearrange("(k p) t -> p k t", p=128)
        for q in range(4):
            tb = tmp[k % 2]
            key = ("extmp", k % 2)
            c.op("gpsimd", lambda e, tb=tb, q=q, fl=fl: e.tensor_scalar(out=tb, in0=c.xbT[:, :, q * 512:(q + 1) * 512], scalar1=fl, scalar2=None, op0=ALU.mult),
                 reads=[("xbT", q), "cinfo"], writes=[key])
            c.op("sync", lambda e, tb=tb, q=q, v_in=v_in: e.dma_start(out=v_in[:, :, q * 512:(q + 1) * 512], in_=tb),
                 reads=[key], writes=[("ex_in", h)], dkey=f"exin{k % 2}")
            k += 1
        c.op("gpsimd", lambda e, h=h: e.collective_compute("AllReduce", ALU.add, replica_groups=PAIRS,
                                                           ins=[exin[h].ap().opt()], outs=[exout[h].ap().opt()]),
             reads=[("ex_in", h)], writes=[("ex_out", h)], dkey=f"cc{h}", dinc=1)
    c.release(m)


def c_flag(cinfo, h):
    return cinfo[:, 2:3] if h == 0 else cinfo[:, 1:2]


def exchange_pool_halo(c, pxin, pxout, cinfo):
    m = c.mark()
    ta = c.alloc([D], F32)
    tb = c.alloc([D], F32)
    pi = pxin.ap()
    for h in range(2):
        fl = c_flag(cinfo, h)
        c.op("gpsimd", lambda e, fl=fl: e.tensor_scalar(out=ta[0:32, :], in0=c.x_tm[0:32, 0, :], scalar1=fl[0:32, :], scalar2=None, op0=ALU.mult),
             reads=[("x", 0), "cinfo"], writes=["pxa"])
        c.op("gpsimd", lambda e, fl=fl: e.tensor_scalar(out=tb[96:128, :], in0=c.x_tm[96:128, 15, :], scalar1=fl[96:128, :], scalar2=None, op0=ALU.mult),
             reads=[("x", 15), "cinfo"], writes=["pxb"])
        c.op("sync", lambda e, h=h: e.dma_start(out=pi[h * 16:h * 16 + 8, :], in_=ta[0:8, :]), reads=["pxa"], writes=["px_in"], dkey="pxin")
        c.op("sync", lambda e, h=h: e.dma_start(out=pi[h * 16 + 8:h * 16 + 16, :], in_=tb[120:128, :]), reads=["pxb"], writes=["px_in"], dkey="pxin")
    c.op("gpsimd", lambda e: e.collective_compute("AllReduce", ALU.add, replica_groups=PAIRS, ins=[pxin.ap().opt()], outs=[pxout.ap().opt()]),
         reads=["px_in"], writes=["px_out"], dkey="ccp", dinc=1)
    c.release(m)


DEBUG_DUMP = False
FORCE_NONLAST = False
STOP_STAGE = None
N_LAYERS = DEPTH


def build_fused(wshapes):
    nc = bass.Bass("TRN2", target_bir_lowering=False)
    with ExitStack() as st:
        c = Ctx(nc, st)
        c.init_consts()
        Wl = [{k[3:]: c.dram_in(k, shp) for k, shp in wshapes.items() if k.startswith(f"L{i}_")} for i in range(DEPTH)]
        x_own = c.dram_in("x_own", [NT, D])
        xT_seq = c.dram_in("xT_seq", [128, 8, SEQ])
        pos_seq = c.dram_in("pos_seq", [SEQ], I32)
        pos_own = c.dram_in("pos_own", [NT], I32)
        ci_d = c.dram_in("cinfo", [128, 4])
        out = c.dram_out("out", [NT, D])
        exin = [nc.dram_tensor(f"exin{h}", [D, NT], BF16) for h in range(2)]
        exout = [nc.dram_tensor(f"exout{h}", [D, NT], BF16) for h in range(2)]
        pxin = nc.dram_tensor("pxin", [32, D], F32)
        pxout = nc.dram_tensor("pxout", [32, D], F32)
        exv = [t.ap().rearrange("(k p) t -> p k t", p=128) for t in exout]
        cinfo = c.alloc([4], F32)
        c.op("sync", lambda e: e.dma_start(out=cinfo, in_=ci_d), writes=["cinfo"], dkey="cinfo")
        load_x(c, x_own)
        for i in range(N_LAYERS):
            W = Wl[i]
            mk = c.mark()
            moe = (i % 2 == 1)
            router = None
            if moe:
                wr = c.alloc([8, NE], F32)
                logits = c.alloc([16, NE], F32)
                comb = c.alloc([16, NE], F32)
                c.op("sync", lambda e, wr=wr, W=W: e.dma_start(out=wr, in_=W["wr"]), writes=["wr"], dkey="wr")
                router = (wr, logits)
            mix = i % 3
            if mix == 0:
                if i == 0:
                    make_xbT(c)
                    src = ("f32", xT_seq)
                else:
                    src = ("bf16", exv)
                scale_x(c)
                mla_phase(c, src, pos_seq, pos_own, W["mla_wlq"], W["mla_wlkv"], W["mla_qng"], W["mla_kvg"], W["mla_wuq"], W["mla_wukv"], W["mla_wo"])
            elif mix == 1:
                exchange_pool_halo(c, pxin, pxout, cinfo)
                po = pxout.ap()

                def halo_fill(hprev, hnext):
                    c.op("gpsimd", lambda e: e.memset(hprev, 0.0), writes=["hprev"])
                    c.op("gpsimd", lambda e: e.memset(hnext, 0.0), writes=["hnext"])
                    c.op("sync", lambda e: e.dma_start(out=hprev[120:128, :], in_=po[8:16, :]), reads=["px_out"], writes=["hprev"], dkey="hprev")
                    c.op("sync", lambda e: e.dma_start(out=hnext[0:8, :], in_=po[16:24, :]), reads=["px_out"], writes=["hnext"], dkey="hnext")
                    c.op("gpsimd", lambda e: e.tensor_scalar(out=hprev[96:128, :], in0=hprev[96:128, :], scalar1=cinfo[96:128, 1:2], scalar2=None, op0=ALU.mult),
                         reads=["hprev", "cinfo"], writes=["hprev"])
                    c.op("gpsimd", lambda e: e.tensor_scalar(out=hnext[0:32, :], in0=hnext[0:32, :], scalar1=cinfo[0:32, 2:3], scalar2=None, op0=ALU.mult),
                         reads=["hnext", "cinfo"], writes=["hnext"])

                pool_phase(c, halo_fill, cinfo, W["pool_w"], W["pool_scale"])
            else:
                scale_x(c)
                mlstm_phase(c, exv, cinfo, {k[3:]: v for k, v in W.items() if k.startswith("ml_")})
            if STOP_STAGE == (i, "mixer"):
                ov = out.rearrange("(t p) d -> p t d", p=128)
                for t in range(16):
                    c.op("sync", lambda e, t=t, ov=ov: e.dma_start(out=ov[:, t, :], in_=c.x_tm[:, t, :]), reads=[("x", t)], dkey=f"dbg{t % 4}")
                break
            layernorm(c, W["ln_g"][0], W["ln_b"][0], router=router)
            if STOP_STAGE == (i, "ln1"):
                ov = out.rearrange("(t p) d -> p t d", p=128)
                for t in range(16):
                    c.op("sync", lambda e, t=t, ov=ov: e.dma_start(out=ov[:, t, :], in_=c.x_tm[:, t, :]), reads=[("x", t)], dkey=f"dbg{t % 4}")
                break
            if moe:
                moe_route(c, logits, comb)
                scale_x(c)
                ffn_phase(c, W["wg"], W["wu"], W["wd"], NE, D_FFE, G_MOE, moe=comb)
            else:
                scale_x(c)
                ffn_phase(c, W["wg"], W["wu"], W["wd"], 1, D_FF, G_DENSE)
            last = (i == N_LAYERS - 1)
            if FORCE_NONLAST and last:
                layernorm(c, W["ln_g"][1], W["ln_b"][1], out_dram=None)
                ov = out.rearrange("(t p) d -> p t d", p=128)
                for t in range(16):
                    c.op("sync", lambda e, t=t, ov=ov: e.dma_start(out=ov[:, t, :], in_=c.x_tm[:, t, :]), reads=[("x", t)], dkey=f"dbg{t % 4}")
                c.release(mk)
                continue
            layernorm(c, W["ln_g"][1], W["ln_b"][1], out_dram=out if last else None)
            if DEBUG_DUMP and not last:
                dbg = c.dram_out(f"dbg{i}", [NT, D]).rearrange("(t p) d -> p t d", p=128)
                for t in range(16):
                    c.op("sync", lambda e, t=t, dbg=dbg: e.dma_start(out=dbg[:, t, :], in_=c.x_tm[:, t, :]), reads=[("x", t)], dkey=f"dbg{t % 4}")
            if not last and (i + 1) % 3 != 1:
                exchange_x(c, exin, exout, cinfo)
            c.release(mk)
        c.P.emit()
    return nc


def kernel(**inputs):
    inp = {k: np.asarray(v) for k, v in inputs.items()}
    x = np.ascontiguousarray(inp["x"], dtype=np.float32)
    positions = np.asarray(inp["positions"]).astype(np.int32)
    weights = {}
    for i in range(N_LAYERS):
        if STOP_STAGE is not None and STOP_STAGE[0] == i and i % 2 == 1:
            inp = dict(inp)
            for kk in ("moe_w_gate", "moe_w_up", "moe_w_down"):
                inp[kk] = inp[kk][:, :, :, :512] if kk != "moe_w_down" else inp[kk][:, :, :512, :]
        for k, v in layer_weights(i, inp).items():
            weights[f"L{i}_{k}"] = v
    nc = build_fused({k: list(v.shape) for k, v in weights.items()})
    xT = [_fm(x[b]) for b in range(4)]
    in_maps = []
    for core in range(8):
        bi, hf = core // 2, core % 2
        ci = np.zeros((128, 4), np.float32)
        ci[:, 0] = hf * NT
        ci[:, 1] = float(hf == 1)
        ci[:, 2] = float(hf == 0)
        m = dict(weights)
        m["x_own"] = np.ascontiguousarray(x[bi, hf * NT:(hf + 1) * NT])
        m["xT_seq"] = xT[bi]
        m["pos_seq"] = np.ascontiguousarray(positions[bi])
        m["pos_own"] = np.ascontiguousarray(positions[bi, hf * NT:(hf + 1) * NT])
        m["cinfo"] = ci
        in_maps.append(m)
    res = run_bass_kernel_spmd(nc, in_maps, core_ids=list(range(8)))
    outs = [np.asarray(r["out"]) for r in res.results]
    return np.stack([np.concatenate([outs[2 * b], outs[2 * b + 1]], axis=0) for b in range(4)]).astype(np.float32)
```
